# Optimizing a Trainium2 kernel written in Bass

```python
import math
import jax
import jax.numpy as jnp
from jax import lax
import numpy as np

D_MODEL = 1024
BATCH = 8
SEQ = 4096
DEPTH = 1

D_SSM = 512
SSM_CH = 16
SSM_GROUPS = D_SSM // SSM_CH
SSM_STATE = 64
N_HEADS = 8
N_KV = 2
HEAD_DIM = 64
GROUP = N_HEADS // N_KV
ATT_W = N_HEADS * HEAD_DIM
WINDOW = 128
BLOCK = 128
NUM_BUCKETS = 32
MAX_DIST = 128
D_IN = D_SSM + ATT_W + 2 * N_KV * HEAD_DIM
N_GROUPS = 4
EXPERTS_PER_GROUP = 8
N_EXPERTS = N_GROUPS * EXPERTS_PER_GROUP
TOP_K = 2
D_FF_EXPERT = 512
MOE_BLK = 256
PLE_DIM = 256
EPS = 1e-6

kernel_name = 'hybrid_s5_swa_sink_hiermoe_block'


def rmsnorm(x, g):
    xf = x.astype(jnp.float32)
    y = xf * lax.rsqrt(jnp.mean(xf * xf, axis=-1, keepdims=True) + EPS)
    return (y * g.astype(jnp.float32)).astype(x.dtype)


def t5_bucket(rel):
    max_exact = NUM_BUCKETS // 2
    relf = jnp.maximum(rel, 1).astype(jnp.float32)
    large = max_exact + (jnp.log(relf / max_exact) / math.log(MAX_DIST / max_exact)
                         * (NUM_BUCKETS - max_exact)).astype(jnp.int32)
    large = jnp.minimum(large, NUM_BUCKETS - 1)
    return jnp.where(rel < max_exact, rel, large)


def s5_mixer(u, a_re, a_im, log_dt, b_re, b_im, c_re, c_im, d_skip, w_glu, b_glu):
    bsz, seq, _ = u.shape
    f32 = jnp.float32
    uf = u.astype(f32).reshape(bsz, seq, SSM_GROUPS, SSM_CH)
    lam = lax.complex(a_re.astype(f32), a_im.astype(f32))
    dt = jnp.exp(log_dt.astype(f32))[:, None]
    a_bar = jnp.exp(lam * dt)
    b_mat = lax.complex(b_re.astype(f32), b_im.astype(f32))
    b_bar = ((a_bar - 1.0) / lam)[..., None] * b_mat
    bu = jnp.einsum('gpc,bsgc->bsgp', b_bar, uf.astype(jnp.complex64))
    a_elems = jnp.broadcast_to(a_bar, (1, seq, SSM_GROUPS, SSM_STATE))

    def combine(e1, e2):
        a1, s1 = e1
        a2, s2 = e2
        return a2 * a1, a2 * s1 + s2

    _, states = lax.associative_scan(combine, (a_elems, bu), axis=1)
    c_mat = lax.complex(c_re.astype(f32), c_im.astype(f32))
    y = jnp.real(jnp.einsum('gcp,bsgp->bsgc', c_mat, states))
    y = y + d_skip.astype(f32).reshape(SSM_GROUPS, SSM_CH) * uf
    y = jax.nn.gelu(y.reshape(bsz, seq, D_SSM))
    y = y * jax.nn.sigmoid(y @ w_glu.astype(f32) + b_glu.astype(f32))
    return y.astype(u.dtype)


def window_attention(q, k, v, rel_bias, sinks):
    bsz, seq = q.shape[0], q.shape[1]
    nb = seq // BLOCK
    qb = q.reshape(bsz, nb, BLOCK, N_KV, GROUP, HEAD_DIM)
    kb = k.reshape(bsz, nb, BLOCK, N_KV, HEAD_DIM)
    vb = v.reshape(bsz, nb, BLOCK, N_KV, HEAD_DIM)
    pad = ((0, 0), (1, 0), (0, 0), (0, 0), (0, 0))
    kwin = jnp.concatenate([jnp.pad(kb, pad)[:, :-1], kb], axis=2)
    vwin = jnp.concatenate([jnp.pad(vb, pad)[:, :-1], vb], axis=2)
    s = jnp.einsum('bnqkgd,bnckd->bnkgqc', qb, kwin).astype(jnp.float32) * (HEAD_DIM ** -0.5)
    q_loc = jnp.arange(BLOCK)[:, None]
    c_loc = jnp.arange(2 * BLOCK)[None, :]
    rel = q_loc + BLOCK - c_loc
    bias = rel_bias.astype(jnp.float32)[t5_bucket(jnp.maximum(rel, 0))]
    bias = bias.transpose(2, 0, 1).reshape(N_KV, GROUP, BLOCK, 2 * BLOCK)
    valid = (rel >= 0) & (rel < WINDOW)
    blk = jnp.arange(nb)[:, None, None]
    mask = valid[None] & ((blk > 0) | (c_loc[None] >= BLOCK))
    s = jnp.where(mask[None, :, None, None], s + bias, -jnp.inf)
    sink = sinks.astype(jnp.float32).reshape(N_KV, GROUP)[None, None, :, :, None, None]
    m = jnp.maximum(jnp.max(s, axis=-1, keepdims=True), sink)
    e = jnp.exp(s - m)
    pr = e / (jnp.sum(e, axis=-1, keepdims=True) + jnp.exp(sink - m))
    o = jnp.einsum('bnkgqc,bnckd->bnqkgd', pr.astype(v.dtype), vwin)
    return o.reshape(bsz, seq, ATT_W)


def hier_moe(h, w_rg, b_rg, w_re, b_re, w_eg, w_eu, w_ed):
    bsz, seq, dm = h.shape
    n_tok = bsz * seq
    ht = h.reshape(n_tok, dm)
    g_prob = jax.nn.softmax((ht @ w_rg + b_rg).astype(jnp.float32), axis=-1)
    g_top_p, g_idx = lax.top_k(g_prob, 1)
    e_logits = (ht @ w_re + b_re).astype(jnp.float32).reshape(n_tok, N_GROUPS, EXPERTS_PER_GROUP)
    idx = jnp.broadcast_to(g_idx[:, :, None], (n_tok, 1, EXPERTS_PER_GROUP))
    e_in = jnp.take_along_axis(e_logits, idx, axis=1)[:, 0]
    e_top_l, e_top_i = lax.top_k(e_in, TOP_K)
    e_w = jax.nn.softmax(e_top_l, axis=-1) * g_top_p
    expert = g_idx * EXPERTS_PER_GROUP + e_top_i
    n_slots = n_tok * TOP_K
    flat_e = expert.reshape(-1)
    flat_w = e_w.reshape(-1)
    flat_tok = jnp.arange(n_slots) // TOP_K
    order = jnp.argsort(flat_e)
    se = flat_e[order]
    tok_sorted = flat_tok[order]
    counts = jnp.bincount(flat_e, length=N_EXPERTS)
    padded = ((counts + MOE_BLK - 1) // MOE_BLK) * MOE_BLK
    pad_end = jnp.cumsum(padded)
    pad_start = pad_end - padded
    start = jnp.cumsum(counts) - counts
    dest = pad_start[se] + (jnp.arange(n_slots) - start[se])
    n_rows = n_slots + N_EXPERTS * MOE_BLK
    n_blk = n_rows // MOE_BLK
    xd = jnp.zeros((n_rows, dm), ht.dtype).at[dest].set(ht[tok_sorted])
    blk_e = jnp.minimum(jnp.searchsorted(pad_end, jnp.arange(n_blk) * MOE_BLK, side='right'),
                        N_EXPERTS - 1)

    def expert_block(args):
        xb, e = args
        return (jax.nn.silu(xb @ w_eg[e]) * (xb @ w_eu[e])) @ w_ed[e]

    yd = lax.map(expert_block, (xd.reshape(n_blk, MOE_BLK, dm), blk_e)).reshape(n_rows, dm)
    y_slots = yd[dest] * flat_w[order][:, None].astype(yd.dtype)
    out = jax.ops.segment_sum(y_slots, tok_sorted, num_segments=n_tok)
    return out.reshape(bsz, seq, dm).astype(h.dtype)


def setup_inputs(seed: int = 0) -> dict:
    key = jax.random.key(seed)
    ks = iter(jax.random.split(key, 48))
    L = DEPTH

    def nrm(shape, scale):
        return jax.random.normal(next(ks), shape, jnp.float32) * scale

    n_idx = jnp.arange(SSM_STATE, dtype=jnp.float32)
    return {
        'x': nrm((BATCH, SEQ, D_MODEL), 1.0),
        'p': nrm((DEPTH, BATCH, SEQ, PLE_DIM), 1.0),
        'rel_bias': nrm((NUM_BUCKETS, N_HEADS), 0.5),
        'g_mix': 1.0 + nrm((L, D_MODEL), 0.02),
        'w_in': nrm((L, D_MODEL, D_IN), D_MODEL ** -0.5),
        'w_gate': nrm((L, D_MODEL, 2 * D_MODEL), D_MODEL ** -0.5),
        'b_gate': nrm((L, 2 * D_MODEL), 0.02),
        'ssm_a_re': -0.5 + nrm((L, SSM_GROUPS, SSM_STATE), 0.01),
        'ssm_a_im': math.pi * n_idx + nrm((L, SSM_GROUPS, SSM_STATE), 0.01),
        'ssm_log_dt': jax.random.uniform(next(ks), (L, SSM_GROUPS), jnp.float32,
                                         minval=math.log(1e-3), maxval=math.log(1e-1)),
        'ssm_b_re': nrm((L, SSM_GROUPS, SSM_STATE, SSM_CH), (2 * SSM_CH) ** -0.5),
        'ssm_b_im': nrm((L, SSM_GROUPS, SSM_STATE, SSM_CH), (2 * SSM_CH) ** -0.5),
        'ssm_c_re': nrm((L, SSM_GROUPS, SSM_CH, SSM_STATE), (2 * SSM_STATE) ** -0.5),
        'ssm_c_im': nrm((L, SSM_GROUPS, SSM_CH, SSM_STATE), (2 * SSM_STATE) ** -0.5),
        'ssm_d': nrm((L, D_SSM), 1.0),
        'w_glu': nrm((L, D_SSM, D_SSM), D_SSM ** -0.5),
        'b_glu': nrm((L, D_SSM), 0.02),
        'sinks': nrm((L, N_HEADS), 0.5),
        'w_br_ssm': nrm((L, D_SSM, D_MODEL), D_SSM ** -0.5),
        'w_br_attn': nrm((L, ATT_W, D_MODEL), ATT_W ** -0.5),
        'w_out': nrm((L, D_MODEL, D_MODEL), D_MODEL ** -0.5),
        'g_ffn': 1.0 + nrm((L, D_MODEL), 0.02),
        'w_router_group': nrm((L, D_MODEL, N_GROUPS), D_MODEL ** -0.5),
        'b_router_group': nrm((L, N_GROUPS), 0.01),
        'w_router_expert': nrm((L, D_MODEL, N_EXPERTS), D_MODEL ** -0.5),
        'b_router_expert': nrm((L, N_EXPERTS), 0.01),
        'w_e_gate': nrm((L, N_EXPERTS, D_MODEL, D_FF_EXPERT), D_MODEL ** -0.5),
        'w_e_up': nrm((L, N_EXPERTS, D_MODEL, D_FF_EXPERT), D_MODEL ** -0.5),
        'w_e_down': nrm((L, N_EXPERTS, D_FF_EXPERT, D_MODEL), D_FF_EXPERT ** -0.5),
        'g_ple': 1.0 + nrm((L, D_MODEL), 0.02),
        'w_ple_gate': nrm((L, D_MODEL, D_MODEL), D_MODEL ** -0.5),
        'b_ple_gate': nrm((L, D_MODEL), 0.02),
        'w_ple_proj': nrm((L, PLE_DIM, D_MODEL), PLE_DIM ** -0.5),
        'g_final': 1.0 + nrm((D_MODEL,), 0.02),
    }


def reference(x, p, rel_bias, g_mix, w_in, w_gate, b_gate, ssm_a_re, ssm_a_im, ssm_log_dt,
              ssm_b_re, ssm_b_im, ssm_c_re, ssm_c_im, ssm_d, w_glu, b_glu, sinks,
              w_br_ssm, w_br_attn, w_out, g_ffn, w_router_group, b_router_group,
              w_router_expert, b_router_expert, w_e_gate, w_e_up, w_e_down,
              g_ple, w_ple_gate, b_ple_gate, w_ple_proj, g_final):
    bsz, seq, _ = x.shape
    for i in range(DEPTH):
        h = rmsnorm(x, g_mix[i])
        proj = h @ w_in[i]
        u = proj[..., :D_SSM]
        q = proj[..., D_SSM:D_SSM + ATT_W].reshape(bsz, seq, N_HEADS, HEAD_DIM)
        k = proj[..., D_SSM + ATT_W:D_SSM + ATT_W + N_KV * HEAD_DIM].reshape(bsz, seq, N_KV, HEAD_DIM)
        v = proj[..., D_SSM + ATT_W + N_KV * HEAD_DIM:].reshape(bsz, seq, N_KV, HEAD_DIM)
        y_ssm = s5_mixer(u, ssm_a_re[i], ssm_a_im[i], ssm_log_dt[i], ssm_b_re[i], ssm_b_im[i],
                         ssm_c_re[i], ssm_c_im[i], ssm_d[i], w_glu[i], b_glu[i])
        y_att = window_attention(q, k, v, rel_bias, sinks[i])
        gates = jax.nn.sigmoid(h @ w_gate[i] + b_gate[i]).reshape(bsz, seq, 2, D_MODEL)
        merged = gates[:, :, 0] * (y_ssm @ w_br_ssm[i]) + gates[:, :, 1] * (y_att @ w_br_attn[i])
        x = x + merged @ w_out[i]
        h2 = rmsnorm(x, g_ffn[i])
        x = x + hier_moe(h2, w_router_group[i], b_router_group[i], w_router_expert[i],
                         b_router_expert[i], w_e_gate[i], w_e_up[i], w_e_down[i])
        h3 = rmsnorm(x, g_ple[i])
        x = x + (p[i] @ w_ple_proj[i]) * jax.nn.sigmoid(h3 @ w_ple_gate[i] + b_ple_gate[i])
    return rmsnorm(x, g_final)
```

```python
import math
import os
from contextlib import ExitStack
import numpy as np
import concourse.bass as bass
import concourse.mybir as mybir
from concourse.bass_utils import run_bass_kernel_spmd

F32 = mybir.dt.float32
BF16 = mybir.dt.bfloat16
I32 = mybir.dt.int32
AF = mybir.ActivationFunctionType
ALU = mybir.AluOpType
AX = mybir.AxisListType

D = 1024
DC = 8
D_SSM = 512
NG = 32
NST = 64
NH = 8
NKV = 2
HD = 64
DIN_L = 1408
NE = 32
DFF = 512
PLE = 256
EPS = 1e-6
NEG = -30000.0
TWO_PI = 2.0 * math.pi


class Res:
    __slots__ = ("w", "r")

    def __init__(self):
        self.w = None
        self.r = {}


class Prog:
    NDS = 24

    def __init__(self, nc, stack):
        self.nc = nc
        self.names = ["pe", "dve", "act", "pool", "sp"]
        self.q = {k: [] for k in self.names}
        self.cnt = {k: 0 for k in self.names}
        self.sem = {k: stack.enter_context(nc.semaphore("s_" + k)) for k in self.names}
        self.seen = {k: {} for k in self.names}
        for j in range(self.NDS):
            self.sem[("d", j)] = stack.enter_context(nc.semaphore("s_d%d" % j))
        self.dcnt = [0] * self.NDS
        self.rr = 0
        self.res = {}

    def R(self, *key):
        r = self.res.get(key)
        if r is None:
            r = self.res[key] = Res()
        return r

    def _deps(self, eng, reads, writes):
        deps = {}

        def add(t):
            if t is None:
                return
            k, v = t
            if deps.get(k, 0) < v:
                deps[k] = v

        for r in reads:
            add(r.w)
        for w in writes:
            add(w.w)
            for k, v in w.r.items():
                add((k, v))
        return deps

    def _wait(self, eng, deps, skip_same=False):
        seen = self.seen[eng]
        for k, v in deps.items():
            if skip_same and k == eng:
                continue
            if seen.get(k, 0) < v:
                seen[k] = v
                sem = self.sem[k]
                self.q[eng].append(lambda e, sem=sem, v=v: e.wait_ge(sem, v))

    def _mark(self, ticket, reads, writes):
        k, v = ticket
        for r in reads:
            if r.r.get(k, 0) < v:
                r.r[k] = v
        for w in writes:
            w.w = ticket
            w.r = {}

    def op(self, eng, fn, reads=(), writes=()):
        deps = self._deps(eng, reads, writes)
        self._wait(eng, deps, skip_same=(eng == "pe"))
        self.cnt[eng] += 1
        sem = self.sem[eng]
        self.q[eng].append(lambda e, fn=fn, sem=sem: fn(e).then_inc(sem, 1))
        self._mark((eng, self.cnt[eng]), reads, writes)

    def dma(self, fn, reads=(), writes=(), eng="sp"):
        deps = self._deps(eng, reads, writes)
        j = self.rr
        self.rr = (self.rr + 1) % self.NDS
        if self.dcnt[j]:
            deps[("d", j)] = max(deps.get(("d", j), 0), self.dcnt[j])
        self._wait(eng, deps)
        self.dcnt[j] += 16
        sem = self.sem[("d", j)]
        self.q[eng].append(lambda e, fn=fn, sem=sem: fn(e).then_inc(sem, 16))
        self._mark((("d", j), self.dcnt[j]), reads, writes)

    def finish(self, eng="sp"):
        deps = {("d", j): self.dcnt[j] for j in range(self.NDS) if self.dcnt[j]}
        for k in self.names:
            if self.cnt[k]:
                deps[k] = self.cnt[k]
        self._wait(eng, deps)
        self.flush()

    def flush(self):
        q = self.q
        self.q = {k: [] for k in self.names}
        with self.nc.Block() as block:
            @block.tensor
            def _(e):
                for f in q["pe"]:
                    f(e)

            @block.vector
            def _(e):
                for f in q["dve"]:
                    f(e)

            @block.scalar
            def _(e):
                for f in q["act"]:
                    f(e)

            @block.gpsimd
            def _(e):
                for f in q["pool"]:
                    f(e)

            @block.sync
            def _(e):
                for f in q["sp"]:
                    f(e)

    def barrier(self):
        deps = {("d", j): self.dcnt[j] for j in range(self.NDS) if self.dcnt[j]}
        for k in self.names:
            if self.cnt[k]:
                deps[k] = self.cnt[k]
        for eng in self.names:
            self._wait(eng, dict(deps))
        self.flush()


def t5_bucket_np(rel):
    max_exact = 16
    relf = np.maximum(rel, 1).astype(np.float32)
    large = max_exact + (np.log(relf / np.float32(max_exact)) / np.float32(math.log(128 / max_exact))
                         * np.float32(32 - max_exact)).astype(np.int32)
    large = np.minimum(large, 31)
    return np.where(rel < max_exact, rel, large)


def build(T=4096, n_exp=NE, dbg=False, stop=None):
    nc = bass.Bass("TRN2", target_bir_lowering=False)
    NT = T // 128
    TS = min(512, T)
    NS = T // TS
    TPS = TS // 128

    def din(name, shape, dt=F32):
        return nc.dram_tensor(name, list(shape), dt, kind="ExternalInput").ap()

    x_d = din("x", [T, D])
    p_d = din("p", [T, PLE])
    w_in_d = din("w_in", [D, 1280])
    gvec_d = din("gvec", [128, 4, DC])
    grow_d = din("grow", [3, D])
    ssm_bt_d = din("ssm_bt", [128, 2, 2048])
    ssm_af_d = din("ssm_af", [1, 3 * 2048])
    ssm_ap_d = din("ssm_ap", [128, 3, 16])
    ssm_ct_d = din("ssm_ct", [128, 2, 2048])
    ssm_dg_d = din("ssm_dg", [128, 8])
    w_glu_d = din("w_glu", [512, 512])
    abias_d = din("attn_bias", [128, NH, 256])
    sinkb_d = din("sinks_b", [128, NH])
    w_gate_d = din("w_gate", [D, 2 * D])
    bgate_d = din("b_gate_l", [128, 16])
    w_bra_d = din("w_br_ssm", [512, D])
    w_brb_d = din("w_br_attn", [512, D])
    w_out_d = din("w_out", [D, D])
    w_rt_d = din("w_router", [D, 36])
    b_rt_d = din("b_router", [1, 36])
    w_eg_d = din("w_e_gate", [NE, D, DFF])
    w_eu_d = din("w_e_up", [NE, D, DFF])
    w_ed_d = din("w_e_down", [NE, DFF, D])
    w_pg_d = din("w_ple_gate", [D, D])
    b_pg_d = din("b_ple_gate", [1, D])
    w_pp_d = din("w_ple_proj", [PLE, D])
    g_fin_d = din("g_final", [1, D])
    out_d = nc.dram_tensor("out", [T, D], F32, kind="ExternalOutput").ap()
    x1_d = out_d
    dbg_d = {}

    def dout(name, shape):
        dbg_d[name] = nc.dram_tensor(name, list(shape), F32, kind="ExternalOutput").ap()
        return dbg_d[name]

    with ExitStack() as st:
        P = Prog(nc, st)
        R = P.R

        cur = [st]

        def sb(name, shape, dt=F32, stack=None):
            stack = stack or cur[-1]
            return stack.enter_context(nc.sbuf_tensor(name, list(shape), dt))

        PS = [st.enter_context(nc.psum_tensor("ps%d" % i, [128, 512], F32)) for i in range(8)]

        ident = sb("ident", [128, 128])
        P.op("pool", lambda e: e.memset(ident[:], 0.0), writes=[R("ident")])
        P.op("pool", lambda e: e.affine_select(out=ident[:], in_=ident[:], pattern=[[-1, 128]],
                                               compare_op=ALU.not_equal, fill=1.0, base=0,
                                               channel_multiplier=1),
             reads=[R("ident")], writes=[R("ident")])
        gvec = sb("gvec_sb", [128, 4, DC])
        P.dma(lambda e: e.dma_start(out=gvec[:], in_=gvec_d[:, :, :]), writes=[R("gvec")])
        epsc = sb("epsc", [128, 1])
        gb = sb("gb", [128, D])

        def load_gb(k_):
            P.dma(lambda e: e.dma_start(out=gb[:], in_=grow_d[k_:k_ + 1, :].partition_broadcast(128)), writes=[R("gb")])
        P.op("pool", lambda e: e.memset(epsc[:], EPS), writes=[R("epsc")])

        def load_w_bf16(dst, w_dram, K, N, gslot, pieces, stage, stage_key, tag):
            for c in range(K // 128):
                sidx = c % 2
                sres = R(stage_key, sidx)
                P.dma(lambda e, c=c, sidx=sidx: e.dma_start(out=stage[:, sidx, 0:N],
                                                            in_=w_dram[c * 128:(c + 1) * 128, :]),
                      writes=[sres])
                for (s0, n, d0) in pieces:
                    if c % 2 == 0:
                        P.op("act", lambda e, c=c, sidx=sidx, s0=s0, n=n, d0=d0:
                             e.copy(out=dst[:, c, d0:d0 + n], in_=stage[:, sidx, s0:s0 + n]),
                             reads=[sres], writes=[R(tag, c)])
                    else:
                        P.op("dve", lambda e, c=c, sidx=sidx, s0=s0, n=n, d0=d0:
                             e.tensor_copy(out=dst[:, c, d0:d0 + n], in_=stage[:, sidx, s0:s0 + n]),
                             reads=[sres], writes=[R(tag, c)])

        xn = sb("xn", [128, 2, D])
        ssq = sb("ssq", [128, 4])
        rstd = sb("rstd", [128, 4])
        junk = sb("junk", [128, D], BF16)

        def vtt(eng, out, a, b, op, reads, writes):
            P.op(eng, lambda e: e.tensor_tensor(out=out, in0=a, in1=b, op=op), reads=reads, writes=writes)

        def ts(eng, out, a, s1, op0, reads, writes, s2=None, op1=None):
            if op1 is None:
                P.op(eng, lambda e: e.tensor_scalar(out=out, in0=a, scalar1=s1, scalar2=None, op0=op0),
                     reads=reads, writes=writes)
            else:
                P.op(eng, lambda e: e.tensor_scalar(out=out, in0=a, scalar1=s1, scalar2=s2, op0=op0, op1=op1),
                     reads=reads, writes=writes)

        def stt(eng, out, a, sc, b, op0, op1, reads, writes):
            P.op(eng, lambda e: e.scalar_tensor_tensor(out=out, in0=a, scalar=sc, in1=b, op0=op0, op1=op1),
                 reads=reads, writes=writes)

        def act(out, in_, func, reads, writes, bias=None, scale=None, accum=None):
            kw = {}
            if bias is not None:
                kw["bias"] = bias
            if scale is not None:
                kw["scale"] = scale
            if accum is not None:
                kw["accum_out"] = accum
            P.op("act", lambda e: e.activation(out=out, in_=in_, func=func, **kw), reads=reads, writes=writes)

        cst = sb("cst", [128, 4])
        for i_, v_ in enumerate((math.pi / 2, 0.0, 1.0, -1.0)):
            P.op("pool", lambda e, i_=i_, v_=v_: e.memset(cst[:, i_:i_ + 1], v_), writes=[R("cst")])

        def sincos(ph, sn, cs, tf, ti, rin, rsn, rcs, rtf, rti):
            ts("dve", tf, ph, 1.0 / TWO_PI, ALU.mult, [rin], [rtf])
            P.op("dve", lambda e: e.tensor_copy(out=ti, in_=tf), reads=[rtf], writes=[rti])
            P.op("dve", lambda e: e.tensor_copy(out=tf, in_=ti), reads=[rti], writes=[rtf])
            stt("dve", ph, tf, -TWO_PI, ph, ALU.mult, ALU.add, [rtf, rin], [rin])
            ts("dve", tf, ph, math.pi, ALU.is_gt, [rin], [rtf])
            stt("dve", ph, tf, -TWO_PI, ph, ALU.mult, ALU.add, [rtf, rin], [rin])
            ts("dve", tf, ph, -math.pi, ALU.is_lt, [rin], [rtf])
            stt("dve", ph, tf, TWO_PI, ph, ALU.mult, ALU.add, [rtf, rin], [rin])
            ts("dve", ph, ph, -math.pi, ALU.max, [rin], [rin], s2=math.pi, op1=ALU.min)
            act(sn, ph, AF.Sin, [rin], [rsn])
            stt("dve", tf, ph, -1.0, ph, ALU.mult, ALU.max, [rin], [rtf])
            np_ = tf.shape[0]
            act(cs, tf, AF.Sin, [rtf, R("cst")], [rcs], bias=cst[0:np_, 0:1], scale=-1.0)

        st_mix = ExitStack()
        cur.append(st_mix)
        uT = sb("uT", [128, 4, T], BF16)
        qT = sb("qT", [128, 4, T], BF16)
        st_kv = ExitStack()
        cur.append(st_kv)
        kT = sb("kT", [128, 2, T], BF16)
        vS = sb("vS", [128, NT, 128], BF16)
        st_sw = ExitStack()
        cur.append(st_sw)
        xt = None
        hT = None

        def early_exit():
            zt = sb("zt", [128, D])
            P.op("pool", lambda e: e.memset(zt[:], 0.0), writes=[R("zt")])
            for tt_ in range(NT):
                P.dma(lambda e, tt_=tt_: e.dma_start(out=out_d[tt_ * 128:(tt_ + 1) * 128, :], in_=zt[:]),
                      reads=[R("zt")])
            P.finish()
            while len(cur) > 1:
                cur.pop().close()
            return nc

        if stop == 0:
            return early_exit()
        bbar = sb("bbar", [128, 2, 2048], BF16)
        ctb = sb("ctb", [128, 2, 2048], BF16)
        ssm_p = sb("ssm_p", [128, 3, 16])
        ssm_dg = sb("ssm_dg_sb", [128, 8])
        wglu_bf = sb("wglu_bf", [128, 4, 512], BF16)
        P.dma(lambda e: e.dma_start(out=ssm_dg[:], in_=ssm_dg_d[:, :]), writes=[R("ssm_dg")])
        st0 = ExitStack()
        cur.append(st0)
        AFt = sb("AFt", [128, 3, 1024])
        BTt = sb("BTt", [128, 2, 1024])
        tmps = [sb("s0t%d" % i, [128, 1024]) for i in range(8)]
        tmi = sb("s0ti", [128, 1024], I32)
        for hh in range(2):
          hs = slice(hh * 1024, (hh + 1) * 1024)
          for a_ in range(3):
            P.dma(lambda e, a_=a_, hh=hh: e.dma_start(
                out=AFt[:, a_, :],
                in_=ssm_af_d[0:1, a_ * 2048 + hh * 1024:a_ * 2048 + (hh + 1) * 1024].partition_broadcast(128)),
                writes=[R("AFt")])
          P.dma(lambda e, hs=hs: e.dma_start(out=BTt[:], in_=ssm_bt_d[:, :, hs]), writes=[R("BTt")])
          LR, LI, LD = AFt[:, 0, :], AFt[:, 1, :], AFt[:, 2, :]
          rA, rB = R("AFt"), R("BTt")
          rt = [R("s0t", i) for i in range(8)]
          T0, T1, T2, T3, T4, T5, T6, T7 = [t[:] for t in tmps]
          act(LD, LD, AF.Exp, [rA], [rA])
          vtt("dve", T0, LR, LD, ALU.mult, [rA], [rt[0]])
          act(T0, T0, AF.Exp, [rt[0]], [rt[0]])
          vtt("dve", T1, LI, LD, ALU.mult, [rA], [rt[1]])
          sincos(T1, T2, T3, T4, tmi[:], rt[1], rt[2], rt[3], rt[4], R("s0ti"))
          vtt("dve", T3, T0, T3, ALU.mult, [rt[0], rt[3]], [rt[3]])
          ts("dve", T3, T3, -1.0, ALU.add, [rt[3]], [rt[3]])
          vtt("dve", T2, T0, T2, ALU.mult, [rt[0], rt[2]], [rt[2]])
          vtt("dve", T0, LR, LR, ALU.mult, [rA], [rt[0]])
          vtt("dve", T1, LI, LI, ALU.mult, [rA], [rt[1]])
          vtt("dve", T0, T0, T1, ALU.add, [rt[0], rt[1]], [rt[0]])
          P.op("dve", lambda e: e.reciprocal(out=T0, in_=T0), reads=[rt[0]], writes=[rt[0]])
          vtt("dve", T1, T3, LR, ALU.mult, [rt[3], rA], [rt[1]])
          vtt("dve", T4, T2, LI, ALU.mult, [rt[2], rA], [rt[4]])
          vtt("dve", T1, T1, T4, ALU.add, [rt[1], rt[4]], [rt[1]])
          vtt("dve", T1, T1, T0, ALU.mult, [rt[1], rt[0]], [rt[1]])
          vtt("dve", T4, T2, LR, ALU.mult, [rt[2], rA], [rt[4]])
          vtt("dve", T5, T3, LI, ALU.mult, [rt[3], rA], [rt[5]])
          vtt("dve", T4, T4, T5, ALU.subtract, [rt[4], rt[5]], [rt[4]])
          vtt("dve", T4, T4, T0, ALU.mult, [rt[4], rt[0]], [rt[4]])
          Bre, Bim = BTt[:, 0, :], BTt[:, 1, :]
          vtt("dve", T5, T1, Bre, ALU.mult, [rt[1], rB], [rt[5]])
          vtt("dve", T6, T4, Bim, ALU.mult, [rt[4], rB], [rt[6]])
          vtt("dve", bbar[:, 0, hs], T5, T6, ALU.subtract, [rt[5], rt[6]], [R("bbar")])
          vtt("dve", T5, T1, Bim, ALU.mult, [rt[1], rB], [rt[5]])
          vtt("dve", T6, T4, Bre, ALU.mult, [rt[4], rB], [rt[6]])
          vtt("dve", bbar[:, 1, hs], T5, T6, ALU.add, [rt[5], rt[6]], [R("bbar")])
        for hh in range(2):
            hs = slice(hh * 1024, (hh + 1) * 1024)
            P.dma(lambda e, hs=hs: e.dma_start(out=BTt[:], in_=ssm_ct_d[:, :, hs]), reads=[], writes=[rB])
            P.op("pool", lambda e, hs=hs: e.tensor_copy(out=ctb[:, 0, hs], in_=BTt[:, 0, :]), reads=[rB], writes=[R("ctb")])
            ts("pool", ctb[:, 1, hs], BTt[:, 1, :], -1.0, ALU.mult, [rB], [R("ctb")])
        apt = sb("apt", [128, 3, 16])
        P.dma(lambda e: e.dma_start(out=apt[:], in_=ssm_ap_d[:, :, :]), writes=[R("apt")])
        act(apt[:, 2, :], apt[:, 2, :], AF.Exp, [R("apt")], [R("apt")])
        vtt("dve", ssm_p[:, 0, :], apt[:, 0, :], apt[:, 2, :], ALU.mult, [R("apt")], [R("ssm_p")])
        act(ssm_p[:, 0, :], ssm_p[:, 0, :], AF.Exp, [R("ssm_p")], [R("ssm_p")])
        vtt("dve", ssm_p[:, 1, :], apt[:, 1, :], apt[:, 2, :], ALU.mult, [R("apt")], [R("ssm_p")])
        wst0 = sb("wst0", [128, 2, 512])
        load_w_bf16(wglu_bf, w_glu_d, 512, 512, None, [(0, 512, 0)], wst0, "wst0", "wglu_bf")
        P.barrier()
        st0.close()
        cur.pop()

        def rms_rstd(src_ap, src_res, slot):
            P.op("act", lambda e: e.activation(out=junk[:], in_=src_ap, func=AF.Square,
                                               accum_out=ssq[:, slot:slot + 1]),
                 reads=[src_res], writes=[R("junk"), R("ssq", slot)])
            P.op("act", lambda e: e.activation(out=ssq[:, slot:slot + 1], in_=ssq[:, slot:slot + 1],
                                               func=AF.Sqrt, bias=epsc[:, 0:1], scale=1.0 / D),
                 reads=[R("ssq", slot), R("epsc")], writes=[R("ssq", slot)])
            P.op("dve", lambda e: e.reciprocal(out=rstd[:, slot:slot + 1], in_=ssq[:, slot:slot + 1]),
                 reads=[R("ssq", slot)], writes=[R("rstd", slot)])

        def emit_hT(s):
            for j in range(TPS):
                tok0 = s * TS + j * 128
                P.dma(lambda e, j=j, tok0=tok0: e.dma_start(out=xt[:, j, :], in_=x_d[tok0:tok0 + 128, :]),
                      writes=[R("xt", j)])
                slot = j % 4
                rms_rstd(xt[:, j, :], R("xt", j), slot)
                b = j % 2
                stt("dve", xn[:, b, :], xt[:, j, :], rstd[:, slot:slot + 1], gb[:], ALU.mult, ALU.mult,
                    [R("xt", j), R("rstd", slot), R("gb")], [R("xn", b)])
                for hf in range(2):
                    bank = hf
                    for cc in range(4):
                        c = hf * 4 + cc
                        P.op("pe", lambda e, b=b, c=c, cc=cc, bank=bank: e.transpose(
                            out=PS[bank][:, cc * 128:(cc + 1) * 128], in_=xn[:, b, c * 128:(c + 1) * 128],
                            identity=ident[:]), reads=[R("xn", b), R("ident")], writes=[R("ps", bank)])
                    eng = "dve" if hf == 0 else "act"
                    outap = hT[:, hf * 4:hf * 4 + 4, j * 128:(j + 1) * 128]
                    inap = PS[bank][:, :].rearrange("p (a b) -> p a b", a=4)
                    if eng == "dve":
                        P.op("dve", lambda e, outap=outap, inap=inap: e.tensor_copy(out=outap, in_=inap),
                             reads=[R("ps", bank)], writes=[R("hT", j)])
                    else:
                        P.op("act", lambda e, outap=outap, inap=inap: e.copy(out=outap, in_=inap),
                             reads=[R("ps", bank)], writes=[R("hT", j)])

        if stop == 1:
            return early_exit()
        st_a = ExitStack()
        cur.append(st_a)
        win_bf = sb("win_bf", [128, DC, DIN_L], BF16)
        wstage = sb("wstage", [128, 2, 2048], F32)
        xt = sb("xt", [128, TPS, D], F32)
        hT = sb("hT", [128, DC, TS], BF16)
        load_w_bf16(win_bf, w_in_d, D, 1280, 0,
                    [(0, 1024, 0), (1024, 64, 1024), (1024, 64, 1088), (1088, 64, 1152), (1088, 64, 1216),
                     (1152, 128, 1280)], wstage, "wstage", "win_bf")
        win_res = [R("win_bf", c) for c in range(DC)]

        load_gb(0)
        for s in range(NS):
            emit_hT(s)
            hres = [R("hT", j) for j in range(TPS)]
            cols = slice(s * TS, (s + 1) * TS)
            for o in range(10):
                bank = 2 + (o % 4)
                for c in range(DC):
                    P.op("pe", lambda e, o=o, c=c, bank=bank: e.matmul(
                        PS[bank][:, 0:TS], win_bf[:, c, o * 128:(o + 1) * 128], hT[:, c, :],
                        start=(c == 0), stop=(c == DC - 1)),
                        reads=hres + win_res, writes=[R("ps", bank)])
                if o < 4:
                    dst, key = uT[:, o, cols], ("uT", o, s)
                elif o < 8:
                    dst, key = qT[:, o - 4, cols], None
                else:
                    dst, key = kT[:, o - 8, cols], ("kT", o - 8, s)
                wr = [R(*key)] if key is not None else [R("qT", o - 4, s * TPS + jj) for jj in range(TPS)]
                if o % 2 == 0:
                    P.op("dve", lambda e, dst=dst, bank=bank: e.tensor_copy(out=dst, in_=PS[bank][:, 0:TS]),
                         reads=[R("ps", bank)], writes=wr)
                else:
                    P.op("act", lambda e, dst=dst, bank=bank: e.copy(out=dst, in_=PS[bank][:, 0:TS]),
                         reads=[R("ps", bank)], writes=wr)
            for j in range(TPS):
                bank = 6 + (j % 2)
                for c in range(DC):
                    P.op("pe", lambda e, j=j, c=c, bank=bank: e.matmul(
                        PS[bank][:, 0:128], hT[:, c, j * 128:(j + 1) * 128], win_bf[:, c, 1280:1408],
                        start=(c == 0), stop=(c == DC - 1)),
                        reads=hres + win_res, writes=[R("ps", bank)])
                tt = s * TPS + j
                P.op("dve", lambda e, tt=tt, bank=bank: e.tensor_copy(out=vS[:, tt, :], in_=PS[bank][:, 0:128]),
                     reads=[R("ps", bank)], writes=[R("vS", tt)])
        P.barrier()
        st_a.close()
        cur.pop()

        if dbg:
            for nm, src, nt_ in (("uT", uT, 4), ("qT", qT, 4), ("kT", kT, 2)):
                dd = dout("dbg_" + nm, [nt_ * 128, T])
                for o in range(nt_):
                    tmp = sb("dbgt_%s%d" % (nm, o), [128, T])
                    P.op("dve", lambda e, tmp=tmp, src=src, o=o: e.tensor_copy(out=tmp[:], in_=src[:, o, :]),
                         reads=([R(nm, o, s) for s in range(NS)] if nm != "qT" else [R("qT", o, b) for b in range(NT)]),
                         writes=[R("dbgt", nm, o)])
                    P.dma(lambda e, dd=dd, tmp=tmp, o=o: e.dma_start(out=dd[o * 128:(o + 1) * 128, :], in_=tmp[:]),
                          reads=[R("dbgt", nm, o)])
            dd = dout("dbg_v", [T, 128])
            for tt in range(NT):
                tmp = sb("dbgt_v%d" % tt, [128, 128])
                P.op("dve", lambda e, tmp=tmp, tt=tt: e.tensor_copy(out=tmp[:], in_=vS[:, tt, :]),
                     reads=[R("vS", tt)], writes=[R("dbgt", "v", tt)])
                P.dma(lambda e, dd=dd, tmp=tmp, tt=tt: e.dma_start(out=dd[tt * 128:(tt + 1) * 128, :], in_=tmp[:]),
                      reads=[R("dbgt", "v", tt)])


        if stop == 2:
            return early_exit()
        Tc = TS
        st_s = ExitStack()
        cur.append(st_s)
        io_i = sb("io_i", [128, Tc + 1], I32)
        io_f = sb("io_f", [128, Tc + 1])
        P.op("pool", lambda e: e.iota(io_i[:], pattern=[[1, Tc + 1]], base=0, channel_multiplier=0),
             writes=[R("io_i")])
        P.op("dve", lambda e: e.tensor_copy(out=io_f[:], in_=io_i[:]), reads=[R("io_i")], writes=[R("io_f")])
        cosT = sb("cosT", [128, 4, Tc + 1])
        sinT = sb("sinT", [128, 4, Tc + 1])
        rT = sb("rT", [128, 4, Tc])
        sc_f = sb("sc_f", [128, Tc + 1])
        sc_i = sb("sc_i", [128, Tc + 1], I32)
        sc_p = sb("sc_p", [128, Tc + 1])
        tA = sb("tA", [128, 2, 4, Tc])
        tE = sb("tE", [128, 2, 2, Tc])
        sRI = sb("sRI", [128, 2, 2, Tc], BF16)
        winit = sb("winit", [128, 16, 2])
        gl_t = sb("gl_t", [128, 2, Tc])
        st_t = ExitStack()
        cur.append(st_t)
        abias = sb("abias", [128, NH, 256])
        sinkb = sb("sinkb", [128, NH])
        P.dma(lambda e: e.dma_start(out=abias[:], in_=abias_d[:, :, :]), writes=[R("abias")])
        P.dma(lambda e: e.dma_start(out=sinkb[:], in_=sinkb_d[:, :]), writes=[R("sinkb")])
        sS = sb("sS", [128, 2, 256])
        eS = sb("eS", [128, 2, 256])
        pT = sb("pT", [128, 2, 2, 128], BF16)
        sm = sb("sm", [128, 2, 4])
        rden = sb("rden", [128, 2, NH])
        osb = sb("osb", [128, 2, 512])
        def ssm_gen():
            units = [(ct, s_, m) for ct in range(4) for s_ in range(NS) for m in range(4)]
            ybank = 2

            def emit_bu(k):
                ct, s_, m = units[k]
                gp = ct * 4 + m
                par = m % 2
                gsl = slice(gp * 128, (gp + 1) * 128)
                cols = slice(s_ * Tc, (s_ + 1) * Tc)
                ures = R("uT", ct, s_)
                P.op("pe", lambda e: e.matmul(PS[0][:, 0:Tc], bbar[:, 0, gsl], uT[:, ct, cols],
                                              start=True, stop=True),
                     reads=[R("bbar"), ures], writes=[R("ps", 0)])
                P.op("pe", lambda e: e.matmul(PS[1][:, 0:Tc], bbar[:, 1, gsl], uT[:, ct, cols],
                                              start=True, stop=True),
                     reads=[R("bbar"), ures], writes=[R("ps", 1)])

            emit_bu(0)
            for k, (ct, s_, m) in enumerate(units):
                if s_ == 0 and m == 0:
                    for m2 in range(4):
                        gp2 = ct * 4 + m2
                        ts("dve", sc_p[:], io_f[:], ssm_p[:, 1, gp2:gp2 + 1], ALU.mult,
                           [R("io_f"), R("ssm_p")], [R("sc_p")])
                        sincos(sc_p[:], sinT[:, m2, :], cosT[:, m2, :], sc_f[:], sc_i[:],
                               R("sc_p"), R("sinT", m2), R("cosT", m2), R("sc_f"), R("sc_i"))
                        ts("dve", rT[:, m2, :], io_f[:, 0:Tc], 0.0, ALU.mult, [R("io_f"), R("ssm_p")], [R("rT", m2)],
                           s2=ssm_p[:, 0, gp2:gp2 + 1], op1=ALU.add)
                cols = slice(s_ * Tc, (s_ + 1) * Tc)
                gp = ct * 4 + m
                par = m % 2
                gsl = slice(gp * 128, (gp + 1) * 128)
                b_re, b_im = PS[0], PS[1]
                rbr, rbi = R("ps", 0), R("ps", 1)
                A_, B_, C_, D_ = [tA[:, par, i, :] for i in range(4)]
                rA_, rB_, rC_, rD_ = [R("tA", par, i) for i in range(4)]
                cs_, sn_ = cosT[:, m, 0:Tc], sinT[:, m, 0:Tc]
                rcs, rsn = R("cosT", m), R("sinT", m)
                vtt("dve", A_, cs_, b_re[:, 0:Tc], ALU.mult, [rcs, rbr], [rA_])
                vtt("dve", B_, sn_, b_im[:, 0:Tc], ALU.mult, [rsn, rbi], [rB_])
                vtt("dve", A_, A_, B_, ALU.add, [rA_, rB_], [rA_])
                vtt("dve", C_, cs_, b_im[:, 0:Tc], ALU.mult, [rcs, rbi], [rC_])
                vtt("dve", D_, sn_, b_re[:, 0:Tc], ALU.mult, [rsn, rbr], [rD_])
                vtt("dve", C_, C_, D_, ALU.subtract, [rC_, rD_], [rC_])
                if k + 1 < len(units):
                    emit_bu(k + 1)
                if s_ == 0:
                    i_re, i_im = 0.0, 0.0
                else:
                    i_re, i_im = winit[:, gp, 0:1], winit[:, gp, 1:2]
                P.op("dve", lambda e, B_=B_, A_=A_, m=m, i_re=i_re: e.tensor_tensor_scan(
                    out=B_, data0=rT[:, m, :], data1=A_, initial=i_re, op0=ALU.mult, op1=ALU.add),
                    reads=[R("rT", m), rA_, R("winit", gp)], writes=[rB_])
                P.op("dve", lambda e, D_=D_, C_=C_, m=m, i_im=i_im: e.tensor_tensor_scan(
                    out=D_, data0=rT[:, m, :], data1=C_, initial=i_im, op0=ALU.mult, op1=ALU.add),
                    reads=[R("rT", m), rC_, R("winit", gp)], writes=[rD_])
                yield
                if s_ < NS - 1:
                    cT_, sT_ = cosT[:, m, Tc:Tc + 1], sinT[:, m, Tc:Tc + 1]
                    wl_re, wl_im = B_[:, Tc - 1:Tc], D_[:, Tc - 1:Tc]
                    ts("dve", winit[:, gp, 0:1], wl_re, cT_, ALU.mult, [rB_, rcs], [R("winit", gp)])
                    vtt("dve", sc_f[:, 0:1], wl_im, sT_, ALU.mult, [rD_, rsn], [R("sc_f")])
                    vtt("dve", winit[:, gp, 0:1], winit[:, gp, 0:1], sc_f[:, 0:1], ALU.subtract,
                        [R("sc_f"), R("winit", gp)], [R("winit", gp)])
                    ts("dve", winit[:, gp, 1:2], wl_re, sT_, ALU.mult, [rB_, rsn], [R("winit", gp)])
                    vtt("dve", sc_f[:, 0:1], wl_im, cT_, ALU.mult, [rD_, rcs], [R("sc_f")])
                    vtt("dve", winit[:, gp, 1:2], winit[:, gp, 1:2], sc_f[:, 0:1], ALU.add,
                        [R("sc_f"), R("winit", gp)], [R("winit", gp)])
                E_, F_ = tE[:, par, 0, :], tE[:, par, 1, :]
                rE_, rF_ = R("tE", par, 0), R("tE", par, 1)
                SR_, SI_ = sRI[:, par, 0, :], sRI[:, par, 1, :]
                rSR, rSI = R("sRI", par, 0), R("sRI", par, 1)
                vtt("dve", E_, cs_, B_, ALU.mult, [rcs, rB_], [rE_])
                vtt("dve", F_, sn_, D_, ALU.mult, [rsn, rD_], [rF_])
                vtt("dve", SR_, E_, F_, ALU.subtract, [rE_, rF_], [rSR])
                vtt("dve", E_, sn_, B_, ALU.mult, [rsn, rB_], [rE_])
                vtt("dve", F_, cs_, D_, ALU.mult, [rcs, rD_], [rF_])
                vtt("dve", SI_, E_, F_, ALU.add, [rE_, rF_], [rSI])
                P.op("pe", lambda e, gsl=gsl, SR_=SR_, m=m: e.matmul(PS[ybank][:, 0:Tc], ctb[:, 0, gsl], SR_, start=(m == 0), stop=False),
                     reads=[R("ctb"), rSR], writes=[R("ps", ybank)])
                P.op("pe", lambda e, gsl=gsl, SI_=SI_, m=m: e.matmul(PS[ybank][:, 0:Tc], ctb[:, 1, gsl], SI_, start=False, stop=(m == 3)),
                     reads=[R("ctb"), rSI], writes=[R("ps", ybank)])
                if m == 3:
                    un = k // 4
                    gt = gl_t[:, un % 2, :]
                    rg = R("gl_t", un % 2)
                    stt("dve", gt, uT[:, ct, cols], ssm_dg[:, ct:ct + 1], PS[ybank][:, 0:Tc], ALU.mult, ALU.add,
                        [R("uT", ct, s_), R("ssm_dg"), R("ps", ybank)], [rg])
                    act(uT[:, ct, cols], gt, AF.Gelu, [rg], [R("uT", ct, s_)])
                yield

        def att_gen():
            steps = [(qb, h) for qb in range(NT) for h in range(NH)]
            OB_ = 7

            def geom(qb):
                c0 = 0 if qb > 0 else 128
                kc0 = (qb - 1) * 128 if qb > 0 else 0
                kcols = slice(kc0, (qb + 1) * 128)
                s_cur = qb // TPS
                kres = [s_cur] if (qb == 0 or (qb - 1) // TPS == s_cur) else [s_cur - 1, s_cur]
                kbs = [0, 1] if qb > 0 else [1]
                return c0, kcols, kres, kbs

            def emit_S(i):
                qb, h = steps[i]
                c0, kcols, kres, kbs = geom(qb)
                kv, qt_, pb, par = h // 4, h // 2, 64 * (h % 2), h % 2
                so, SB_ = 0, 3 + par
                P.op("pe", lambda e: e.matmul(
                    PS[SB_][:, so + c0:so + 256], qT[pb:pb + 64, qt_, qb * 128:(qb + 1) * 128],
                    kT[pb:pb + 64, kv, kcols], start=True, stop=True),
                    reads=[R("qT", qt_, qb)] + [R("kT", kv, s_) for s_ in kres], writes=[R("ps", SB_)])

            def emit_st2(i):
                qb, h = steps[i]
                c0, kcols, kres, kbs = geom(qb)
                par = h % 2
                so, SB_ = 0, 3 + par
                rs, re_, rsm = R("sS", par), R("eS", par), R("sm", par)
                stt("dve", sS[:, par, c0:256], PS[SB_][:, so + c0:so + 256], HD ** -0.5, abias[:, h, c0:256],
                    ALU.mult, ALU.add, [R("ps", SB_), R("abias")], [rs])
                P.op("dve", lambda e: e.reduce_max(out=sm[:, par, 0:1], in_=sS[:, par, c0:256], axis=AX.X),
                     reads=[rs], writes=[rsm])
                vtt("dve", sm[:, par, 0:1], sm[:, par, 0:1], sinkb[:, h:h + 1], ALU.max, [rsm, R("sinkb")], [rsm])
                ts("dve", sm[:, par, 1:2], sm[:, par, 0:1], -1.0, ALU.mult, [rsm], [rsm])
                act(eS[:, par, c0:256], sS[:, par, c0:256], AF.Exp, [rs, rsm], [re_, rsm],
                    bias=sm[:, par, 1:2], accum=sm[:, par, 2:3])
                act(sm[:, par, 3:4], sinkb[:, h:h + 1], AF.Exp, [R("sinkb"), rsm], [rsm], bias=sm[:, par, 1:2])

            def emit_st3(i):
                qb, h = steps[i]
                c0, kcols, kres, kbs = geom(qb)
                par, qpar = h % 2, qb % 2
                to, TB_ = 0, 5 + par
                re_, rsm, rp = R("eS", par), R("sm", par), R("pT", par)
                vtt("dve", sm[:, par, 2:3], sm[:, par, 2:3], sm[:, par, 3:4], ALU.add, [rsm], [rsm])
                P.op("dve", lambda e: e.reciprocal(out=rden[:, qpar, h:h + 1], in_=sm[:, par, 2:3]),
                     reads=[rsm], writes=[R("rden", qpar)])
                for kb in kbs:
                    P.op("pe", lambda e, kb=kb: e.transpose(
                        out=PS[TB_][:, to + kb * 128:to + (kb + 1) * 128], in_=eS[:, par, kb * 128:(kb + 1) * 128],
                        identity=ident[:]), reads=[re_, R("ident")], writes=[R("ps", TB_)])
                k0 = kbs[0]
                P.op("act", lambda e: e.copy(
                    out=pT[:, par, k0:2, :], in_=PS[TB_][:, to + k0 * 128:to + 256].rearrange("p (a b) -> p a b", b=128)),
                    reads=[R("ps", TB_)], writes=[rp])

            def emit_PV(i):
                qb, h = steps[i]
                c0, kcols, kres, kbs = geom(qb)
                par, kv = h % 2, h // 4
                rp = R("pT", par)
                for kb in kbs:
                    vt = qb - 1 + kb
                    P.op("pe", lambda e, kb=kb, vt=vt: e.matmul(
                        PS[OB_][:, h * 64:(h + 1) * 64], pT[:, par, kb, :], vS[:, vt, kv * 64:(kv + 1) * 64],
                        start=(kb == kbs[0]), stop=(kb == 1)),
                        reads=[rp, R("vS", vt)], writes=[R("ps", OB_)])

            def emit_epi(qb):
                qpar = qb % 2
                ro = R("osb", qpar)
                P.op("dve", lambda e: e.tensor_tensor(
                    out=osb[:, qpar, :].rearrange("p (a b) -> p a b", b=64),
                    in0=PS[OB_][:, :].rearrange("p (a b) -> p a b", b=64),
                    in1=rden[:, qpar, :].unsqueeze(2).broadcast_to([128, NH, 64]), op=ALU.mult),
                    reads=[R("ps", OB_), R("rden", qpar)], writes=[ro])
                TB_ = 5
                tres = [R("ps", 5)]
                for ft in range(4):
                    P.op("pe", lambda e, ft=ft: e.transpose(
                        out=PS[TB_][:, ft * 128:(ft + 1) * 128], in_=osb[:, qpar, ft * 128:(ft + 1) * 128],
                        identity=ident[:]), reads=[ro, R("ident")], writes=tres)
                P.op("act", lambda e: e.copy(
                    out=qT[:, 0:4, qb * 128:(qb + 1) * 128], in_=PS[TB_][:, :].rearrange("p (a b) -> p a b", b=128)),
                    reads=tres, writes=[R("qT", ft, qb) for ft in range(4)])

            n = len(steps)
            emit_S(0)
            for i in range(n):
                if i + 1 < n:
                    emit_S(i + 1)
                if i >= 1:
                    emit_PV(i - 1)
                    if steps[i - 1][1] == NH - 1:
                        emit_epi(steps[i - 1][0])
                emit_st2(i)
                yield
                emit_st3(i)
                yield
            emit_PV(n - 1)
            emit_epi(steps[n - 1][0])
            yield

        g_ssm, g_att = ssm_gen(), att_gen()
        live = {"s": True, "a": True}

        def adv(g, k_):
            if live[k_]:
                try:
                    next(g)
                except StopIteration:
                    live[k_] = False

        while live["s"] or live["a"]:
            adv(g_att, "a")
            adv(g_ssm, "s")
            adv(g_att, "a")

        gbank = [0, 1, 2, 3]
        for s_ in range(NS):
            cols = slice(s_ * Tc, (s_ + 1) * Tc)
            yres = [R("uT", k, s_) for k in range(4)]
            for ft in range(4):
                for kc in range(4):
                    P.op("pe", lambda e, ft=ft, kc=kc, cols=cols: e.matmul(
                        PS[gbank[ft]][:, 0:Tc], wglu_bf[:, kc, ft * 128:(ft + 1) * 128], uT[:, kc, cols],
                        start=(kc == 0), stop=(kc == 3)),
                        reads=yres + [R("wglu_bf", kc)], writes=[R("ps", gbank[ft])])
            for ft in range(4):
                gt = gl_t[:, ft % 2, :]
                rg = R("gl_t", ft % 2)
                act(gt, PS[gbank[ft]][:, 0:Tc], AF.Sigmoid, [R("ps", gbank[ft]), R("ssm_dg")], [rg],
                    bias=ssm_dg[:, 4 + ft:5 + ft])
                vtt("dve", uT[:, ft, cols], uT[:, ft, cols], gt, ALU.mult, [rg, R("uT", ft, s_)], [R("uT", ft, s_)])
        if dbg:
            dd = dout("dbg_yssmT", [512, T])
            for o in range(4):
                tmp = sb("dbgys%d" % o, [128, T])
                P.op("dve", lambda e, tmp=tmp, o=o: e.tensor_copy(out=tmp[:], in_=uT[:, o, :]),
                     reads=[R("uT", o, s) for s in range(NS)], writes=[R("dbgys", o)])
                P.dma(lambda e, dd=dd, tmp=tmp, o=o: e.dma_start(out=dd[o * 128:(o + 1) * 128, :], in_=tmp[:]),
                      reads=[R("dbgys", o)])
        if dbg:
            dd = dout("dbg_yattT", [512, T])
            for o in range(4):
                tmp = sb("dbgya%d" % o, [128, T])
                P.op("dve", lambda e, tmp=tmp, o=o: e.tensor_copy(out=tmp[:], in_=qT[:, o, :]),
                     reads=[R("qT", o, b) for b in range(NT)], writes=[R("dbgya", o)])
                P.dma(lambda e, dd=dd, tmp=tmp, o=o: e.dma_start(out=dd[o * 128:(o + 1) * 128, :], in_=tmp[:]),
                      reads=[R("dbgya", o)])
        P.barrier()
        st_t.close()
        cur.pop()
        st_s.close()
        cur.pop()
        st_sw.close()
        cur.pop()
        st_kv.close()
        cur.pop()

        if stop == 4:
            return early_exit()
        st_5 = ExitStack()
        cur.append(st_5)
        xt = sb("xt5", [128, TPS, D], F32)
        hT = sb("hT5", [128, DC, TS], BF16)
        wgate_bf = sb("wgate_bf", [128, DC, 2 * D], BF16)
        wA_bf = sb("wA_bf", [128, 4, D], BF16)
        wB_bf = sb("wB_bf", [128, 4, D], BF16)
        wout_bf = sb("wout_bf", [128, DC, D], BF16)
        wst5 = sb("wst5", [128, 2, D], F32)
        bgate = sb("bgate", [128, 16])
        P.dma(lambda e: e.dma_start(out=bgate[:], in_=bgate_d[:, :]), writes=[R("bgate")])
        for hh in range(2):
            load_w_bf16(wgate_bf[:, :, hh * D:(hh + 1) * D], w_gate_d[:, hh * D:(hh + 1) * D], D, D, 0,
                        [(0, D, 0)], wst5, "wst5", "wgate_bf%d" % hh)
        load_w_bf16(wA_bf, w_bra_d, 512, D, None, [(0, D, 0)], wst5, "wst5", "wA_bf")
        load_w_bf16(wB_bf, w_brb_d, 512, D, None, [(0, D, 0)], wst5, "wst5", "wB_bf")
        load_w_bf16(wout_bf, w_out_d, D, D, None, [(0, D, 0)], wst5, "wst5", "wout_bf")
        wg_res = [R("wgate_bf0", c) for c in range(DC)] + [R("wgate_bf1", c) for c in range(DC)]
        gts = sb("gts", [128, 2, 2, TS])
        mT = sb("mT", [128, DC, TS], BF16)
        x1t = sb("x1t", [128, 2, D])
        for s in range(NS):
            emit_hT(s)
            hres = [R("hT", j) for j in range(TPS)]
            cols = slice(s * TS, (s + 1) * TS)
            for f in range(DC):
                fp = f % 2
                for br in range(2):
                    bank = 4 * fp + br
                    for c in range(DC):
                        P.op("pe", lambda e, bank=bank, c=c, f=f, br=br: e.matmul(
                            PS[bank][:, 0:TS], wgate_bf[:, c, br * D + f * 128:br * D + (f + 1) * 128], hT[:, c, :],
                            start=(c == 0), stop=(c == DC - 1)),
                            reads=hres + wg_res, writes=[R("ps", bank)])
                for br, (wbr, src, tag) in enumerate(((wA_bf, uT, "wA_bf"), (wB_bf, qT, "wB_bf"))):
                    bank = 4 * fp + 2 + br
                    if br == 0:
                        srcres = [R("uT", k, s) for k in range(4)]
                    else:
                        srcres = [R("qT", k, s * TPS + jj) for k in range(4) for jj in range(TPS)]
                    for k in range(4):
                        P.op("pe", lambda e, bank=bank, k=k, f=f, wbr=wbr, src=src, cols=cols: e.matmul(
                            PS[bank][:, 0:TS], wbr[:, k, f * 128:(f + 1) * 128], src[:, k, cols],
                            start=(k == 0), stop=(k == 3)),
                            reads=srcres + [R(tag, k) for k in range(4)], writes=[R("ps", bank)])
                for br in range(2):
                    bank = 4 * fp + br
                    rg = R("gts", fp, br)
                    act(gts[:, fp, br, :], PS[bank][:, 0:TS], AF.Sigmoid, [R("ps", bank), R("bgate")], [rg],
                        bias=bgate[:, br * 8 + f:br * 8 + f + 1])
                    vtt("dve", gts[:, fp, br, :], gts[:, fp, br, :], PS[4 * fp + 2 + br][:, 0:TS], ALU.mult,
                        [rg, R("ps", 4 * fp + 2 + br)], [rg])
                vtt("dve", mT[:, f, :], gts[:, fp, 0, :], gts[:, fp, 1, :], ALU.add,
                    [R("gts", fp, 0), R("gts", fp, 1)], [R("mT", f)])
            mres = [R("mT", f) for f in range(DC)]
            wo_res = [R("wout_bf", c) for c in range(DC)]
            for j in range(TPS):
                tix = s * TPS + j
                xb = tix % 2
                for hf in range(2):
                    bank = (2 * j + hf) % 8
                    for f in range(DC):
                        P.op("pe", lambda e, bank=bank, f=f, j=j, hf=hf: e.matmul(
                            PS[bank][:, :], mT[:, f, j * 128:(j + 1) * 128], wout_bf[:, f, hf * 512:(hf + 1) * 512],
                            start=(f == 0), stop=(f == DC - 1)),
                            reads=mres + wo_res, writes=[R("ps", bank)])
                    vtt("dve", x1t[:, xb, hf * 512:(hf + 1) * 512], PS[bank][:, :], xt[:, j, hf * 512:(hf + 1) * 512],
                        ALU.add, [R("ps", bank), R("xt", j)], [R("x1t", xb)])
                P.dma(lambda e, tix=tix, xb=xb: e.dma_start(out=x1_d[tix * 128:(tix + 1) * 128, :], in_=x1t[:, xb, :]),
                      reads=[R("x1t", xb)], writes=[R("x1d", tix)])
                if dbg:
                    if tix == 0:
                        dbg_x1 = dout("dbg_x1", [T, D])
                    P.dma(lambda e, tix=tix, xb=xb: e.dma_start(out=dbg_x1[tix * 128:(tix + 1) * 128, :], in_=x1t[:, xb, :]),
                          reads=[R("x1t", xb)])
        P.barrier()
        st_5.close()
        cur.pop()
        st_mix.close()
        cur.pop()

        if stop == 5:
            return early_exit()
        NHALF = 2 if T >= 1024 else 1
        TH = T // NHALF
        NTH = TH // 128
        TCH = min(512, TH)
        NCH = TH // TCH
        TPC = TCH // 128
        BIG = 1.0e4
        st_b = ExitStack()
        cur.append(st_b)
        acc = sb("acc", [128, NTH, D])
        h2T = sb("h2T", [128, DC, TH], BF16)
        wden = sb("wden", [128, NTH, NE])
        for half in range(NHALF):
            st_b0 = ExitStack()
            cur.append(st_b0)
            wr32 = sb("wr32_%d" % half, [128, DC, 36])
            brt = sb("brt_%d" % half, [128, 36])
            wrs = sb("wrs_%d" % half, [128, DC, 36])
            wr_hi = sb("wr_hi_%d" % half, [128, DC, 36], BF16)
            wr_lo = sb("wr_lo_%d" % half, [128, DC, 36], BF16)
            P.dma(lambda e: e.dma_start(out=brt[:], in_=b_rt_d[0:1, :].partition_broadcast(128)), writes=[R("brt")])
            P.dma(lambda e: e.dma_start(out=wr32[:], in_=w_rt_d.rearrange("(c p) n -> p c n", p=128)), writes=[R("wr32")])
            P.op("pool", lambda e: e.tensor_copy(out=wr_hi[:], in_=wr32[:]), reads=[R("wr32")], writes=[R("wr_hi")])
            P.op("pool", lambda e: e.tensor_copy(out=wrs[:], in_=wr_hi[:]), reads=[R("wr_hi")], writes=[R("wrs")])
            vtt("pool", wrs[:], wr32[:], wrs[:], ALU.subtract, [R("wr32"), R("wrs")], [R("wrs")])
            P.op("pool", lambda e: e.tensor_copy(out=wr_lo[:], in_=wrs[:]), reads=[R("wrs")], writes=[R("wr_lo")])
            load_gb(1)
            lgall = sb("lgall_%d" % half, [128, NTH, 36])
            hlo2 = sb("hlo2_%d" % half, [128, 2, DC, 128], BF16)
            pend = []

            def emit_lg(j, rbank):
                vtt("dve", lgall[:, j, :], PS[rbank][:, 0:36], brt[:], ALU.add, [R("ps", rbank), R("brt")], [R("lgall")])

            for j in range(NTH):
                tix = half * NTH + j
                P.dma(lambda e, j=j, tix=tix: e.dma_start(out=acc[:, j, :], in_=x1_d[tix * 128:(tix + 1) * 128, :]),
                      reads=[R("x1d", tix)], writes=[R("acc", j)])
                slot = j % 4
                rms_rstd(acc[:, j, :], R("acc", j), slot)
                b = j % 2
                stt("dve", xn[:, b, :], acc[:, j, :], rstd[:, slot:slot + 1], gb[:], ALU.mult, ALU.mult,
                    [R("acc", j), R("rstd", slot), R("gb")], [R("xn", b)])
                jc = slice(j * 128, (j + 1) * 128)
                for hf in range(2):
                    bank = hf
                    for cc in range(4):
                        c = hf * 4 + cc
                        P.op("pe", lambda e, b=b, c=c, cc=cc, bank=bank: e.transpose(
                            out=PS[bank][:, cc * 128:(cc + 1) * 128], in_=xn[:, b, c * 128:(c + 1) * 128],
                            identity=ident[:]), reads=[R("xn", b), R("ident")], writes=[R("ps", bank)])
                    inap = PS[bank][:, :].rearrange("p (a b) -> p a b", a=4)
                    hsl = slice(hf * 4, hf * 4 + 4)
                    P.op("dve", lambda e, hsl=hsl, jc=jc, inap=inap: e.tensor_copy(out=h2T[:, hsl, jc], in_=inap),
                         reads=[R("ps", bank)], writes=[R("h2T", j)])
                    P.op("dve", lambda e, hsl=hsl, jc=jc, inap=inap, b=b: e.tensor_tensor(
                        out=hlo2[:, b, hsl, :], in0=inap, in1=h2T[:, hsl, jc], op=ALU.subtract),
                        reads=[R("ps", bank), R("h2T", j)], writes=[R("hlo2", b)])
                rbank = 2 + (j % 2)
                k_ = 0
                for (ha, hres_, wa, wres_) in ((h2T[:, :, jc], R("h2T", j), wr_hi, R("wr_hi")),
                                               (h2T[:, :, jc], R("h2T", j), wr_lo, R("wr_lo")),
                                               (hlo2[:, b, :, :], R("hlo2", b), wr_hi, R("wr_hi"))):
                    for c in range(DC):
                        P.op("pe", lambda e, c=c, rbank=rbank, ha=ha, wa=wa, k_=k_: e.matmul(
                            PS[rbank][:, 0:36], ha[:, c, :], wa[:, c, :], start=(k_ == 0), stop=(k_ == 3 * DC - 1)),
                            reads=[hres_, wres_], writes=[R("ps", rbank)])
                        k_ += 1
                if pend:
                    emit_lg(*pend.pop())
                pend.append((j, rbank))
            emit_lg(*pend.pop())
            N_ = NTH
            r1 = sb("r1_%d" % half, [128, 8, N_])
            g4 = sb("g4_%d" % half, [128, 3, N_, 4])
            e32 = sb("e32_%d" % half, [128, 3, N_, NE])
            rL, r1r, rg4, re32 = R("lgall"), R("r1"), R("g4"), R("e32")
            G_ = lgall[:, :, 0:4]
            gm, gsum, gpr, m1, m2, ex_, w1, w2 = (r1[:, i, :] for i in range(8))

            def bc(v, n):
                return v.unsqueeze(2).broadcast_to([128, N_, n])

            P.op("dve", lambda e: e.reduce_max(out=gm, in_=G_, axis=AX.X), reads=[rL], writes=[r1r])
            vtt("dve", g4[:, 0, :, :], G_, bc(gm, 4), ALU.subtract, [rL, r1r], [rg4])
            ts("dve", g4[:, 1, :, :], g4[:, 0, :, :], 0.0, ALU.is_ge, [rg4], [rg4])
            act(g4[:, 0, :, :], g4[:, 0, :, :], AF.Exp, [rg4], [rg4])
            P.op("dve", lambda e: e.reduce_sum(out=gsum, in_=g4[:, 0, :, :], axis=AX.X), reads=[rg4], writes=[r1r])
            P.op("dve", lambda e: e.reciprocal(out=gpr, in_=gsum), reads=[r1r], writes=[r1r])
            ts("dve", g4[:, 2, :, :], g4[:, 1, :, :], BIG, ALU.mult, [rg4], [rg4], s2=-BIG, op1=ALU.add)
            em_, oh1, oh2 = e32[:, 0, :, :], e32[:, 1, :, :], e32[:, 2, :, :]
            P.op("dve", lambda e: e.tensor_copy(out=em_, in_=lgall[:, :, 4:36]), reads=[rL], writes=[re32])
            P.op("dve", lambda e: e.tensor_tensor(
                out=em_.rearrange("p n (a b) -> p (n a) b", b=8), in0=em_.rearrange("p n (a b) -> p (n a) b", b=8),
                in1=g4[:, 2, :, :].rearrange("p n a -> p (n a)").unsqueeze(2).broadcast_to([128, N_ * 4, 8]),
                op=ALU.add), reads=[rg4, re32], writes=[re32])
            P.op("dve", lambda e: e.reduce_max(out=m1, in_=em_, axis=AX.X), reads=[re32], writes=[r1r])
            vtt("dve", oh1, em_, bc(m1, NE), ALU.subtract, [re32, r1r], [re32])
            ts("dve", oh1, oh1, 0.0, ALU.is_ge, [re32], [re32])
            stt("dve", em_, oh1, -BIG, em_, ALU.mult, ALU.add, [re32], [re32])
            P.op("dve", lambda e: e.reduce_max(out=m2, in_=em_, axis=AX.X), reads=[re32], writes=[r1r])
            vtt("dve", oh2, em_, bc(m2, NE), ALU.subtract, [re32, r1r], [re32])
            ts("dve", oh2, oh2, 0.0, ALU.is_ge, [re32], [re32])
            vtt("dve", ex_, m2, m1, ALU.subtract, [r1r], [r1r])
            act(ex_, ex_, AF.Exp, [r1r], [r1r])
            ts("dve", w1, ex_, 1.0, ALU.add, [r1r], [r1r])
            P.op("dve", lambda e: e.reciprocal(out=w1, in_=w1), reads=[r1r], writes=[r1r])
            vtt("dve", w2, ex_, w1, ALU.mult, [r1r], [r1r])
            vtt("dve", w1, w1, gpr, ALU.mult, [r1r], [r1r])
            vtt("dve", w2, w2, gpr, ALU.mult, [r1r], [r1r])
            wres_all = [R("wden", j) for j in range(NTH)]
            vtt("dve", oh1, oh1, bc(w1, NE), ALU.mult, [re32, r1r], [re32])
            vtt("dve", oh2, oh2, bc(w2, NE), ALU.mult, [re32, r1r], [re32])
            vtt("dve", wden[:, :, :], oh1, oh2, ALU.add, [re32], wres_all)
            P.barrier()
            st_b0.close()
            cur.pop()
            if stop == 6:
                return early_exit()
            st_e = ExitStack()
            cur.append(st_e)
            wgu_bf = sb("wgu_bf%d" % half, [128, 2, 2, DC, DFF], BF16)
            wd_bf = sb("wd_bf%d" % half, [128, 2, 4, D], BF16)
            wste = sb("wste%d" % half, [128, 2, 4096])
            aT = sb("aT%d" % half, [128, 2, 4, TCH], BF16)
            sgt = sb("sgt%d" % half, [128, 2, TCH])
            h2res = [R("h2T", j) for j in range(NTH)]
            stg = [0]

            def load_expert(e_):
                bsel = e_ % 2
                for mi, wd_ in enumerate((w_eg_d, w_eu_d, w_ed_d)):
                    si = stg[0] % 2
                    stg[0] += 1
                    rs_ = R("wste", si)
                    if mi < 2:
                        P.dma(lambda e, wd_=wd_, si=si, e_=e_: e.dma_start(
                            out=wste[:, si, :].rearrange("p (c n) -> p c n", n=DFF),
                            in_=wd_[e_].rearrange("(c p) n -> p c n", p=128)), writes=[rs_])
                        P.op("act", lambda e, bsel=bsel, mi=mi, si=si: e.copy(
                            out=wgu_bf[:, bsel, mi, :, :], in_=wste[:, si, :].rearrange("p (c n) -> p c n", n=DFF)),
                            reads=[rs_], writes=[R("wgu", bsel, mi)])
                    else:
                        P.dma(lambda e, wd_=wd_, si=si, e_=e_: e.dma_start(
                            out=wste[:, si, :].rearrange("p (c n) -> p c n", n=D),
                            in_=wd_[e_].rearrange("(c p) n -> p c n", p=128)), writes=[rs_])
                        P.op("dve", lambda e, bsel=bsel, si=si: e.tensor_copy(
                            out=wd_bf[:, bsel, :, :], in_=wste[:, si, :].rearrange("p (c n) -> p c n", n=D)),
                            reads=[rs_], writes=[R("wd", bsel)])

            load_expert(0)
            for e_ in range(n_exp):
                if e_ + 1 < n_exp:
                    load_expert(e_ + 1)
                bsel = e_ % 2
                for ch in range(NCH):
                    ap_ = ch % 2
                    ccols = slice(ch * TCH, (ch + 1) * TCH)
                    hres_c = h2res[ch * TPC:(ch + 1) * TPC]
                    for f in range(4):
                        fp = f % 2
                        for mi in range(2):
                            bank = 2 * fp + mi
                            for c in range(DC):
                                P.op("pe", lambda e, bank=bank, bsel=bsel, mi=mi, c=c, f=f, ccols=ccols: e.matmul(
                                    PS[bank][:, 0:TCH], wgu_bf[:, bsel, mi, c, f * 128:(f + 1) * 128], h2T[:, c, ccols],
                                    start=(c == 0), stop=(c == DC - 1)),
                                    reads=hres_c + [R("wgu", bsel, mi)], writes=[R("ps", bank)])
                        act(sgt[:, fp, :], PS[2 * fp][:, 0:TCH], AF.Silu, [R("ps", 2 * fp)], [R("sgt", fp)])
                        vtt("dve", aT[:, ap_, f, :], sgt[:, fp, :], PS[2 * fp + 1][:, 0:TCH], ALU.mult,
                            [R("sgt", fp), R("ps", 2 * fp + 1)], [R("aT", ap_, f)])
                    ares = [R("aT", ap_, f) for f in range(4)]
                    for jj in range(TPC):
                        j = ch * TPC + jj
                        for hf in range(2):
                            bank = 4 + (2 * jj + hf) % 4
                            for f in range(4):
                                P.op("pe", lambda e, bank=bank, ap_=ap_, f=f, jj=jj, bsel=bsel, hf=hf: e.matmul(
                                    PS[bank][:, :], aT[:, ap_, f, jj * 128:(jj + 1) * 128],
                                    wd_bf[:, bsel, f, hf * 512:(hf + 1) * 512], start=(f == 0), stop=(f == 3)),
                                    reads=ares + [R("wd", bsel)], writes=[R("ps", bank)])
                            stt("dve", acc[:, j, hf * 512:(hf + 1) * 512], PS[bank][:, :], wden[:, j, e_:e_ + 1],
                                acc[:, j, hf * 512:(hf + 1) * 512], ALU.mult, ALU.add,
                                [R("ps", bank), R("wden", j), R("acc", j)], [R("acc", j)])
            P.barrier()
            st_e.close()
            cur.pop()
            if stop == 7:
                return early_exit()
            st_c = ExitStack()
            cur.append(st_c)
            wpg_bf = sb("wpg_bf%d" % half, [128, DC, D], BF16)
            wpp_bf = sb("wpp_bf%d" % half, [128, 2, D], BF16)
            wstc = sb("wstc%d" % half, [128, 2, D])
            load_gb(2)
            bpg_b = sb("bpg_b%d" % half, [128, D])
            gfin_b = sb("gfin_b%d" % half, [128, D])
            P.dma(lambda e: e.dma_start(out=bpg_b[:], in_=b_pg_d[0:1, :].partition_broadcast(128)), writes=[R("bpg_b")])
            P.dma(lambda e: e.dma_start(out=gfin_b[:], in_=g_fin_d[0:1, :].partition_broadcast(128)), writes=[R("gfin_b")])
            load_w_bf16(wpg_bf, w_pg_d, D, D, 2, [(0, D, 0)], wstc, "wstc", "wpg_bf")
            load_w_bf16(wpp_bf, w_pp_d, PLE, D, None, [(0, D, 0)], wstc, "wstc", "wpp_bf")
            wpg_res = [R("wpg_bf", c) for c in range(DC)]
            wpp_res = [R("wpp_bf", c) for c in range(2)]
            h3T = sb("h3T%d" % half, [128, 2, DC, 128], BF16)
            ptl = sb("ptl%d" % half, [128, 2, PLE])
            pT_ = sb("pTt%d" % half, [128, 2, 2, 128], BF16)
            gtm = sb("gtm%d" % half, [128, 2, D])
            x3t = sb("x3t%d" % half, [128, 2, D])
            def c_tile(j):
                tix = half * NTH + j
                b = j % 2
                slot = j % 4
                slot2 = (j + 2) % 4
                base = 4 * b
                rms_rstd(acc[:, j, :], R("acc", j), slot)
                P.dma(lambda e: e.dma_start(out=ptl[:, b, :], in_=p_d[tix * 128:(tix + 1) * 128, :]),
                      writes=[R("ptl", b)])
                yield
                stt("dve", xn[:, b, :], acc[:, j, :], rstd[:, slot:slot + 1], gb[:], ALU.mult, ALU.mult,
                    [R("acc", j), R("rstd", slot), R("gb")], [R("xn", b)])
                for hf in range(2):
                    bank = base + hf
                    for cc in range(4):
                        c = hf * 4 + cc
                        P.op("pe", lambda e, c=c, cc=cc, bank=bank: e.transpose(
                            out=PS[bank][:, cc * 128:(cc + 1) * 128], in_=xn[:, b, c * 128:(c + 1) * 128],
                            identity=ident[:]), reads=[R("xn", b), R("ident")], writes=[R("ps", bank)])
                for c2 in range(2):
                    P.op("pe", lambda e, c2=c2: e.transpose(
                        out=PS[base + 2][:, c2 * 128:(c2 + 1) * 128], in_=ptl[:, b, c2 * 128:(c2 + 1) * 128],
                        identity=ident[:]), reads=[R("ptl", b), R("ident")], writes=[R("ps", base + 2)])
                yield
                for hf in range(2):
                    bank = base + hf
                    inap = PS[bank][:, :].rearrange("p (a b) -> p a b", a=4)
                    if hf == 0:
                        P.op("dve", lambda e, hf=hf, inap=inap: e.tensor_copy(out=h3T[:, b, hf * 4:hf * 4 + 4, :], in_=inap),
                             reads=[R("ps", bank)], writes=[R("h3T", b)])
                    else:
                        P.op("act", lambda e, hf=hf, inap=inap: e.copy(out=h3T[:, b, hf * 4:hf * 4 + 4, :], in_=inap),
                             reads=[R("ps", bank)], writes=[R("h3T", b)])
                P.op("act", lambda e: e.copy(out=pT_[:, b, :, :],
                                             in_=PS[base + 2][:, 0:256].rearrange("p (a b) -> p a b", b=128)),
                     reads=[R("ps", base + 2)], writes=[R("pT_", b)])
                yield
                for hf in range(2):
                    gbk, pb_ = base + 2 + hf, base + hf
                    hs = slice(hf * 512, (hf + 1) * 512)
                    for c in range(DC):
                        P.op("pe", lambda e, gbk=gbk, c=c, hs=hs: e.matmul(
                            PS[gbk][:, :], h3T[:, b, c, :], wpg_bf[:, c, hs], start=(c == 0), stop=(c == DC - 1)),
                            reads=[R("h3T", b)] + wpg_res, writes=[R("ps", gbk)])
                    for c2 in range(2):
                        P.op("pe", lambda e, pb_=pb_, c2=c2, hs=hs: e.matmul(
                            PS[pb_][:, :], pT_[:, b, c2, :], wpp_bf[:, c2, hs], start=(c2 == 0), stop=(c2 == 1)),
                            reads=[R("pT_", b)] + wpp_res, writes=[R("ps", pb_)])
                yield
                for hf in range(2):
                    gbk = base + 2 + hf
                    hs = slice(hf * 512, (hf + 1) * 512)
                    rg = R("gtm", b, hf)
                    vtt("dve", gtm[:, b, hs], PS[gbk][:, :], bpg_b[:, hs], ALU.add, [R("ps", gbk), R("bpg_b")], [rg])
                    act(gtm[:, b, hs], gtm[:, b, hs], AF.Sigmoid, [rg], [rg])
                yield
                for hf in range(2):
                    pb_ = base + hf
                    hs = slice(hf * 512, (hf + 1) * 512)
                    rg = R("gtm", b, hf)
                    vtt("dve", gtm[:, b, hs], gtm[:, b, hs], PS[pb_][:, :], ALU.mult, [rg, R("ps", pb_)], [rg])
                    vtt("dve", x3t[:, b, hs], gtm[:, b, hs], acc[:, j, hs], ALU.add, [rg, R("acc", j)], [R("x3t", b, hf)])
                x3res = [R("x3t", b, 0), R("x3t", b, 1)]
                if dbg:
                    P.dma(lambda e: e.dma_start(out=dbg_c[0][tix * 128:(tix + 1) * 128, :], in_=acc[:, j, :]),
                          reads=[R("acc", j)])
                    P.dma(lambda e: e.dma_start(out=dbg_c[1][tix * 128:(tix + 1) * 128, :], in_=x3t[:, b, :]),
                          reads=x3res)
                P.op("act", lambda e: e.activation(out=junk[:], in_=x3t[:, b, :], func=AF.Square,
                                                   accum_out=ssq[:, slot2:slot2 + 1]),
                     reads=x3res, writes=[R("junk"), R("ssq", slot2)])
                P.op("act", lambda e: e.activation(out=ssq[:, slot2:slot2 + 1], in_=ssq[:, slot2:slot2 + 1],
                                                   func=AF.Sqrt, bias=epsc[:, 0:1], scale=1.0 / D),
                     reads=[R("ssq", slot2), R("epsc")], writes=[R("ssq", slot2)])
                yield
                P.op("dve", lambda e: e.reciprocal(out=rstd[:, slot2:slot2 + 1], in_=ssq[:, slot2:slot2 + 1]),
                     reads=[R("ssq", slot2)], writes=[R("rstd", slot2)])
                stt("dve", x3t[:, b, :], x3t[:, b, :], rstd[:, slot2:slot2 + 1], gfin_b[:], ALU.mult, ALU.mult,
                    x3res + [R("rstd", slot2), R("gfin_b")], x3res)
                P.dma(lambda e: e.dma_start(out=out_d[tix * 128:(tix + 1) * 128, :], in_=x3t[:, b, :]),
                      reads=x3res, writes=[R("x1d", tix)])
                yield

            if dbg and half == 0:
                dbg_c = [dout("dbg_x2", [T, D]), dout("dbg_x3", [T, D])]
            NSTEP, STAG = 7, 4
            gens = [c_tile(j) for j in range(NTH)]
            for t_ in range(STAG * (NTH - 1) + NSTEP):
                for j in range(NTH):
                    if 0 <= t_ - STAG * j < NSTEP:
                        next(gens[j])
            P.barrier()
            st_c.close()
            cur.pop()
        st_b.close()
        cur.pop()
        P.finish()
    return nc


def host_prep(inp, core):
    g = np.zeros((128, 4, DC), np.float32)
    g[:, 0, :] = inp["g_mix"][0].reshape(DC, 128).T
    g[:, 1, :] = inp["g_ffn"][0].reshape(DC, 128).T
    g[:, 2, :] = inp["g_ple"][0].reshape(DC, 128).T
    bt = np.zeros((128, 2, 16, 128), np.float32)
    ct_ = np.zeros((128, 2, 16, 128), np.float32)
    af = np.zeros((3, 16, 128), np.float32)
    ap = np.zeros((128, 3, 16), np.float32)
    are, aim, ldt = inp["ssm_a_re"][0], inp["ssm_a_im"][0], inp["ssm_log_dt"][0]
    for ri, (B_, C_) in enumerate(((inp["ssm_b_re"][0], inp["ssm_c_re"][0]), (inp["ssm_b_im"][0], inp["ssm_c_im"][0]))):
        for g_ in range(32):
            gp_, g2 = g_ // 2, g_ % 2
            g8 = g_ % 8
            bt[g8 * 16:(g8 + 1) * 16, ri, gp_, g2 * 64:(g2 + 1) * 64] = B_[g_].T
            ct_[g2 * 64:(g2 + 1) * 64, ri, gp_, g8 * 16:(g8 + 1) * 16] = C_[g_].T
    for g_ in range(32):
        gp_, g2 = g_ // 2, g_ % 2
        af[0, gp_, g2 * 64:(g2 + 1) * 64] = are[g_]
        af[1, gp_, g2 * 64:(g2 + 1) * 64] = aim[g_]
        af[2, gp_, g2 * 64:(g2 + 1) * 64] = ldt[g_]
        ap[g2 * 64:(g2 + 1) * 64, 0, gp_] = are[g_]
        ap[g2 * 64:(g2 + 1) * 64, 1, gp_] = aim[g_]
        ap[g2 * 64:(g2 + 1) * 64, 2, gp_] = ldt[g_]
    dg = np.zeros((128, 8), np.float32)
    dg[:, 0:4] = inp["ssm_d"][0].reshape(4, 128).T
    dg[:, 4:8] = inp["b_glu"][0].reshape(4, 128).T
    q_loc = np.arange(128)[:, None]
    c_loc = np.arange(256)[None, :]
    rel = q_loc + 128 - c_loc
    valid = (rel >= 0) & (rel < 128)
    bkt = t5_bucket_np(np.maximum(rel, 0))
    rb = inp["rel_bias"]
    ab = np.full((128, NH, 256), NEG, np.float32)
    for h in range(NH):
        ab[:, h, :] = np.where(valid, rb[bkt, h], np.float32(NEG))
    m = {
        "w_router": np.ascontiguousarray(np.concatenate([inp["w_router_group"][0], inp["w_router_expert"][0]], axis=1)),
        "b_router": np.ascontiguousarray(np.concatenate([inp["b_router_group"][0], inp["b_router_expert"][0]])[None, :]),
        "w_e_gate": np.ascontiguousarray(inp["w_e_gate"][0]),
        "w_e_up": np.ascontiguousarray(inp["w_e_up"][0]),
        "w_e_down": np.ascontiguousarray(inp["w_e_down"][0]),
        "w_ple_gate": np.ascontiguousarray(inp["w_ple_gate"][0]),
        "b_ple_gate": np.ascontiguousarray(inp["b_ple_gate"][0][None, :]),
        "w_ple_proj": np.ascontiguousarray(inp["w_ple_proj"][0]),
        "g_final": np.ascontiguousarray(inp["g_final"][None, :]),
        "attn_bias": ab,
        "sinks_b": np.ascontiguousarray(np.broadcast_to(inp["sinks"][0][None, :], (128, NH))).astype(np.float32),
        "w_gate": np.ascontiguousarray(inp["w_gate"][0]),
        "b_gate_l": np.ascontiguousarray(inp["b_gate"][0].reshape(16, 128).T),
        "w_br_ssm": np.ascontiguousarray(inp["w_br_ssm"][0]),
        "w_br_attn": np.ascontiguousarray(inp["w_br_attn"][0]),
        "w_out": np.ascontiguousarray(inp["w_out"][0]),
        "ssm_bt": bt.reshape(128, 2, 2048), "ssm_ct": ct_.reshape(128, 2, 2048),
        "ssm_af": af.reshape(1, 3 * 2048), "ssm_ap": ap, "ssm_dg": dg,
        "w_glu": np.ascontiguousarray(inp["w_glu"][0]),
        "x": np.ascontiguousarray(inp["x"][core]),
        "p": np.ascontiguousarray(inp["p"][0, core]),
        "w_in": np.ascontiguousarray(inp["w_in"][0]),
        "gvec": g,
        "grow": np.ascontiguousarray(np.stack([inp["g_mix"][0], inp["g_ffn"][0], inp["g_ple"][0]], axis=0)).astype(np.float32),
    }
    return m


def kernel(**inputs):
    inp = {k: np.asarray(v) for k, v in inputs.items()}
    T = inp["x"].shape[1]
    nc = build(T)
    in_maps = [host_prep(inp, c) for c in range(8)]
    res = run_bass_kernel_spmd(nc, in_maps, core_ids=list(range(8)))
    return np.stack([r["out"] for r in res.results], axis=0).astype(np.float32)
```

```python
import math
import os
from contextlib import ExitStack
import numpy as np
import concourse.bass as bass
import concourse.mybir as mybir
from concourse.bass_utils import run_bass_kernel_spmd

F32 = mybir.dt.float32
BF16 = mybir.dt.bfloat16
I32 = mybir.dt.int32
AF = mybir.ActivationFunctionType
ALU = mybir.AluOpType
AX = mybir.AxisListType

D = 1024
DC = 8
D_SSM = 512
NG = 32
NST = 64
NH = 8
NKV = 2
HD = 64
DIN_L = 1408
NE = 32
DFF = 512
PLE = 256
EPS = 1e-6
NEG = -30000.0
TWO_PI = 2.0 * math.pi


class Res:
    __slots__ = ("w", "r")

    def __init__(self):
        self.w = None
        self.r = {}


class Prog:
    NDS = 24

    def __init__(self, nc, stack):
        self.nc = nc
        self.names = ["pe", "dve", "act", "pool", "sp"]
        self.q = {k: [] for k in self.names}
        self.cnt = {k: 0 for k in self.names}
        self.sem = {k: stack.enter_context(nc.semaphore("s_" + k)) for k in self.names}
        self.seen = {k: {} for k in self.names}
        for j in range(self.NDS):
            self.sem[("d", j)] = stack.enter_context(nc.semaphore("s_d%d" % j))
        self.dcnt = [0] * self.NDS
        self.rr = 0
        self.res = {}

    def R(self, *key):
        r = self.res.get(key)
        if r is None:
            r = self.res[key] = Res()
        return r

    def _deps(self, eng, reads, writes):
        deps = {}

        def add(t):
            if t is None:
                return
            k, v = t
            if deps.get(k, 0) < v:
                deps[k] = v

        for r in reads:
            add(r.w)
        for w in writes:
            add(w.w)
            for k, v in w.r.items():
                add((k, v))
        return deps

    def _wait(self, eng, deps, skip_same=False):
        seen = self.seen[eng]
        for k, v in deps.items():
            if skip_same and k == eng:
                continue
            if seen.get(k, 0) < v:
                seen[k] = v
                sem = self.sem[k]
                self.q[eng].append(lambda e, sem=sem, v=v: e.wait_ge(sem, v))

    def _mark(self, ticket, reads, writes):
        k, v = ticket
        for r in reads:
            if r.r.get(k, 0) < v:
                r.r[k] = v
        for w in writes:
            w.w = ticket
            w.r = {}

    def op(self, eng, fn, reads=(), writes=()):
        deps = self._deps(eng, reads, writes)
        self._wait(eng, deps, skip_same=(eng == "pe"))
        self.cnt[eng] += 1
        sem = self.sem[eng]
        self.q[eng].append(lambda e, fn=fn, sem=sem: fn(e).then_inc(sem, 1))
        self._mark((eng, self.cnt[eng]), reads, writes)

    def dma(self, fn, reads=(), writes=(), eng="sp"):
        deps = self._deps(eng, reads, writes)
        j = self.rr
        self.rr = (self.rr + 1) % self.NDS
        if self.dcnt[j]:
            deps[("d", j)] = max(deps.get(("d", j), 0), self.dcnt[j])
        self._wait(eng, deps)
        self.dcnt[j] += 16
        sem = self.sem[("d", j)]
        self.q[eng].append(lambda e, fn=fn, sem=sem: fn(e).then_inc(sem, 16))
        self._mark((("d", j), self.dcnt[j]), reads, writes)

    def finish(self, eng="sp"):
        deps = {("d", j): self.dcnt[j] for j in range(self.NDS) if self.dcnt[j]}
        for k in self.names:
            if self.cnt[k]:
                deps[k] = self.cnt[k]
        self._wait(eng, deps)
        self.flush()

    def flush(self):
        q = self.q
        self.q = {k: [] for k in self.names}
        with self.nc.Block() as block:
            @block.tensor
            def _(e):
                for f in q["pe"]:
                    f(e)

            @block.vector
            def _(e):
                for f in q["dve"]:
                    f(e)

            @block.scalar
            def _(e):
                for f in q["act"]:
                    f(e)

            @block.gpsimd
            def _(e):
                for f in q["pool"]:
                    f(e)

            @block.sync
            def _(e):
                for f in q["sp"]:
                    f(e)

    def barrier(self):
        deps = {("d", j): self.dcnt[j] for j in range(self.NDS) if self.dcnt[j]}
        for k in self.names:
            if self.cnt[k]:
                deps[k] = self.cnt[k]
        for eng in self.names:
            self._wait(eng, dict(deps))
        self.flush()


def t5_bucket_np(rel):
    max_exact = 16
    relf = np.maximum(rel, 1).astype(np.float32)
    large = max_exact + (np.log(relf / np.float32(max_exact)) / np.float32(math.log(128 / max_exact))
                         * np.float32(32 - max_exact)).astype(np.int32)
    large = np.minimum(large, 31)
    return np.where(rel < max_exact, rel, large)


def build(T=4096, n_exp=NE, dbg=False, stop=None):
    nc = bass.Bass("TRN2", target_bir_lowering=False)
    NT = T // 128
    TS = min(512, T)
    NS = T // TS
    TPS = TS // 128

    def din(name, shape, dt=F32):
        return nc.dram_tensor(name, list(shape), dt, kind="ExternalInput").ap()

    x_d = din("x", [T, D])
    p_d = din("p", [T, PLE])
    w_in_d = din("w_in", [D, 1280])
    gvec_d = din("gvec", [128, 4, DC])
    grow_d = din("grow", [3, D])
    ssm_bt_d = din("ssm_bt", [128, 2, 2048])
    ssm_af_d = din("ssm_af", [1, 3 * 2048])
    ssm_ap_d = din("ssm_ap", [128, 3, 16])
    ssm_ct_d = din("ssm_ct", [128, 2, 2048])
    ssm_dg_d = din("ssm_dg", [128, 8])
    w_glu_d = din("w_glu", [512, 512])
    abias_d = din("attn_bias", [128, NH, 256])
    sinkb_d = din("sinks_b", [128, NH])
    w_gate_d = din("w_gate", [D, 2 * D])
    bgate_d = din("b_gate_l", [128, 16])
    w_bra_d = din("w_br_ssm", [512, D])
    w_brb_d = din("w_br_attn", [512, D])
    w_out_d = din("w_out", [D, D])
    w_rt_d = din("w_router", [D, 36])
    b_rt_d = din("b_router", [1, 36])
    w_eg_d = din("w_e_gate", [NE, D, DFF])
    w_eu_d = din("w_e_up", [NE, D, DFF])
    w_ed_d = din("w_e_down", [NE, DFF, D])
    w_pg_d = din("w_ple_gate", [D, D])
    b_pg_d = din("b_ple_gate", [1, D])
    w_pp_d = din("w_ple_proj", [PLE, D])
    g_fin_d = din("g_final", [1, D])
    out_d = nc.dram_tensor("out", [T, D], F32, kind="ExternalOutput").ap()
    x1_d = out_d
    dbg_d = {}

    def dout(name, shape):
        dbg_d[name] = nc.dram_tensor(name, list(shape), F32, kind="ExternalOutput").ap()
        return dbg_d[name]

    with ExitStack() as st:
        P = Prog(nc, st)
        R = P.R

        cur = [st]

        def sb(name, shape, dt=F32, stack=None):
            stack = stack or cur[-1]
            return stack.enter_context(nc.sbuf_tensor(name, list(shape), dt))

        PS = [st.enter_context(nc.psum_tensor("ps%d" % i, [128, 512], F32)) for i in range(8)]

        ident = sb("ident", [128, 128])
        P.op("pool", lambda e: e.memset(ident[:], 0.0), writes=[R("ident")])
        P.op("pool", lambda e: e.affine_select(out=ident[:], in_=ident[:], pattern=[[-1, 128]],
                                               compare_op=ALU.not_equal, fill=1.0, base=0,
                                               channel_multiplier=1),
             reads=[R("ident")], writes=[R("ident")])
        gvec = sb("gvec_sb", [128, 4, DC])
        P.dma(lambda e: e.dma_start(out=gvec[:], in_=gvec_d[:, :, :]), writes=[R("gvec")])
        epsc = sb("epsc", [128, 1])
        gb = sb("gb", [128, D])

        def load_gb(k_):
            P.dma(lambda e: e.dma_start(out=gb[:], in_=grow_d[k_:k_ + 1, :].partition_broadcast(128)), writes=[R("gb")])
        P.op("pool", lambda e: e.memset(epsc[:], EPS), writes=[R("epsc")])

        def load_w_bf16(dst, w_dram, K, N, gslot, pieces, stage, stage_key, tag):
            for c in range(K // 128):
                sidx = c % 2
                sres = R(stage_key, sidx)
                P.dma(lambda e, c=c, sidx=sidx: e.dma_start(out=stage[:, sidx, 0:N],
                                                            in_=w_dram[c * 128:(c + 1) * 128, :]),
                      writes=[sres])
                for (s0, n, d0) in pieces:
                    if c % 2 == 0:
                        P.op("act", lambda e, c=c, sidx=sidx, s0=s0, n=n, d0=d0:
                             e.copy(out=dst[:, c, d0:d0 + n], in_=stage[:, sidx, s0:s0 + n]),
                             reads=[sres], writes=[R(tag, c)])
                    else:
                        P.op("dve", lambda e, c=c, sidx=sidx, s0=s0, n=n, d0=d0:
                             e.tensor_copy(out=dst[:, c, d0:d0 + n], in_=stage[:, sidx, s0:s0 + n]),
                             reads=[sres], writes=[R(tag, c)])

        xn = sb("xn", [128, 2, D])
        ssq = sb("ssq", [128, 4])
        rstd = sb("rstd", [128, 4])
        junk = sb("junk", [128, D], BF16)

        def vtt(eng, out, a, b, op, reads, writes):
            P.op(eng, lambda e: e.tensor_tensor(out=out, in0=a, in1=b, op=op), reads=reads, writes=writes)

        def ts(eng, out, a, s1, op0, reads, writes, s2=None, op1=None):
            if op1 is None:
                P.op(eng, lambda e: e.tensor_scalar(out=out, in0=a, scalar1=s1, scalar2=None, op0=op0),
                     reads=reads, writes=writes)
            else:
                P.op(eng, lambda e: e.tensor_scalar(out=out, in0=a, scalar1=s1, scalar2=s2, op0=op0, op1=op1),
                     reads=reads, writes=writes)

        def stt(eng, out, a, sc, b, op0, op1, reads, writes):
            P.op(eng, lambda e: e.scalar_tensor_tensor(out=out, in0=a, scalar=sc, in1=b, op0=op0, op1=op1),
                 reads=reads, writes=writes)

        def act(out, in_, func, reads, writes, bias=None, scale=None, accum=None):
            kw = {}
            if bias is not None:
                kw["bias"] = bias
            if scale is not None:
                kw["scale"] = scale
            if accum is not None:
                kw["accum_out"] = accum
            P.op("act", lambda e: e.activation(out=out, in_=in_, func=func, **kw), reads=reads, writes=writes)

        cst = sb("cst", [128, 4])
        for i_, v_ in enumerate((math.pi / 2, 0.0, 1.0, -1.0)):
            P.op("pool", lambda e, i_=i_, v_=v_: e.memset(cst[:, i_:i_ + 1], v_), writes=[R("cst")])

        def sincos(ph, sn, cs, tf, ti, rin, rsn, rcs, rtf, rti):
            ts("dve", tf, ph, 1.0 / TWO_PI, ALU.mult, [rin], [rtf])
            P.op("dve", lambda e: e.tensor_copy(out=ti, in_=tf), reads=[rtf], writes=[rti])
            P.op("dve", lambda e: e.tensor_copy(out=tf, in_=ti), reads=[rti], writes=[rtf])
            stt("dve", ph, tf, -TWO_PI, ph, ALU.mult, ALU.add, [rtf, rin], [rin])
            ts("dve", tf, ph, math.pi, ALU.is_gt, [rin], [rtf])
            stt("dve", ph, tf, -TWO_PI, ph, ALU.mult, ALU.add, [rtf, rin], [rin])
            ts("dve", tf, ph, -math.pi, ALU.is_lt, [rin], [rtf])
            stt("dve", ph, tf, TWO_PI, ph, ALU.mult, ALU.add, [rtf, rin], [rin])
            ts("dve", ph, ph, -math.pi, ALU.max, [rin], [rin], s2=math.pi, op1=ALU.min)
            act(sn, ph, AF.Sin, [rin], [rsn])
            stt("dve", tf, ph, -1.0, ph, ALU.mult, ALU.max, [rin], [rtf])
            np_ = tf.shape[0]
            act(cs, tf, AF.Sin, [rtf, R("cst")], [rcs], bias=cst[0:np_, 0:1], scale=-1.0)

        st_mix = ExitStack()
        cur.append(st_mix)
        uT = sb("uT", [128, 4, T], BF16)
        qT = sb("qT", [128, 4, T], BF16)
        st_kv = ExitStack()
        cur.append(st_kv)
        kT = sb("kT", [128, 2, T], BF16)
        vS = sb("vS", [128, NT, 128], BF16)
        st_sw = ExitStack()
        cur.append(st_sw)
        xt = None
        hT = None

        def early_exit():
            zt = sb("zt", [128, D])
            P.op("pool", lambda e: e.memset(zt[:], 0.0), writes=[R("zt")])
            for tt_ in range(NT):
                P.dma(lambda e, tt_=tt_: e.dma_start(out=out_d[tt_ * 128:(tt_ + 1) * 128, :], in_=zt[:]),
                      reads=[R("zt")])
            P.finish()
            while len(cur) > 1:
                cur.pop().close()
            return nc

        if stop == 0:
            return early_exit()
        bbar = sb("bbar", [128, 2, 2048], BF16)
        ctb = sb("ctb", [128, 2, 2048], BF16)
        ssm_p = sb("ssm_p", [128, 3, 16])
        ssm_dg = sb("ssm_dg_sb", [128, 8])
        wglu_bf = sb("wglu_bf", [128, 4, 512], BF16)
        P.dma(lambda e: e.dma_start(out=ssm_dg[:], in_=ssm_dg_d[:, :]), writes=[R("ssm_dg")])
        st0 = ExitStack()
        cur.append(st0)
        AFt = sb("AFt", [128, 3, 1024])
        BTt = sb("BTt", [128, 2, 1024])
        tmps = [sb("s0t%d" % i, [128, 1024]) for i in range(8)]
        tmi = sb("s0ti", [128, 1024], I32)
        for hh in range(2):
          hs = slice(hh * 1024, (hh + 1) * 1024)
          for a_ in range(3):
            P.dma(lambda e, a_=a_, hh=hh: e.dma_start(
                out=AFt[:, a_, :],
                in_=ssm_af_d[0:1, a_ * 2048 + hh * 1024:a_ * 2048 + (hh + 1) * 1024].partition_broadcast(128)),
                writes=[R("AFt")])
          P.dma(lambda e, hs=hs: e.dma_start(out=BTt[:], in_=ssm_bt_d[:, :, hs]), writes=[R("BTt")])
          LR, LI, LD = AFt[:, 0, :], AFt[:, 1, :], AFt[:, 2, :]
          rA, rB = R("AFt"), R("BTt")
          rt = [R("s0t", i) for i in range(8)]
          T0, T1, T2, T3, T4, T5, T6, T7 = [t[:] for t in tmps]
          act(LD, LD, AF.Exp, [rA], [rA])
          vtt("dve", T0, LR, LD, ALU.mult, [rA], [rt[0]])
          act(T0, T0, AF.Exp, [rt[0]], [rt[0]])
          vtt("dve", T1, LI, LD, ALU.mult, [rA], [rt[1]])
          sincos(T1, T2, T3, T4, tmi[:], rt[1], rt[2], rt[3], rt[4], R("s0ti"))
          vtt("dve", T3, T0, T3, ALU.mult, [rt[0], rt[3]], [rt[3]])
          ts("dve", T3, T3, -1.0, ALU.add, [rt[3]], [rt[3]])
          vtt("dve", T2, T0, T2, ALU.mult, [rt[0], rt[2]], [rt[2]])
          vtt("dve", T0, LR, LR, ALU.mult, [rA], [rt[0]])
          vtt("dve", T1, LI, LI, ALU.mult, [rA], [rt[1]])
          vtt("dve", T0, T0, T1, ALU.add, [rt[0], rt[1]], [rt[0]])
          P.op("dve", lambda e: e.reciprocal(out=T0, in_=T0), reads=[rt[0]], writes=[rt[0]])
          vtt("dve", T1, T3, LR, ALU.mult, [rt[3], rA], [rt[1]])
          vtt("dve", T4, T2, LI, ALU.mult, [rt[2], rA], [rt[4]])
          vtt("dve", T1, T1, T4, ALU.add, [rt[1], rt[4]], [rt[1]])
          vtt("dve", T1, T1, T0, ALU.mult, [rt[1], rt[0]], [rt[1]])
          vtt("dve", T4, T2, LR, ALU.mult, [rt[2], rA], [rt[4]])
          vtt("dve", T5, T3, LI, ALU.mult, [rt[3], rA], [rt[5]])
          vtt("dve", T4, T4, T5, ALU.subtract, [rt[4], rt[5]], [rt[4]])
          vtt("dve", T4, T4, T0, ALU.mult, [rt[4], rt[0]], [rt[4]])
          Bre, Bim = BTt[:, 0, :], BTt[:, 1, :]
          vtt("dve", T5, T1, Bre, ALU.mult, [rt[1], rB], [rt[5]])
          vtt("dve", T6, T4, Bim, ALU.mult, [rt[4], rB], [rt[6]])
          vtt("dve", bbar[:, 0, hs], T5, T6, ALU.subtract, [rt[5], rt[6]], [R("bbar")])
          vtt("dve", T5, T1, Bim, ALU.mult, [rt[1], rB], [rt[5]])
          vtt("dve", T6, T4, Bre, ALU.mult, [rt[4], rB], [rt[6]])
          vtt("dve", bbar[:, 1, hs], T5, T6, ALU.add, [rt[5], rt[6]], [R("bbar")])
        for hh in range(2):
            hs = slice(hh * 1024, (hh + 1) * 1024)
            P.dma(lambda e, hs=hs: e.dma_start(out=BTt[:], in_=ssm_ct_d[:, :, hs]), reads=[], writes=[rB])
            P.op("pool", lambda e, hs=hs: e.tensor_copy(out=ctb[:, 0, hs], in_=BTt[:, 0, :]), reads=[rB], writes=[R("ctb")])
            ts("pool", ctb[:, 1, hs], BTt[:, 1, :], -1.0, ALU.mult, [rB], [R("ctb")])
        apt = sb("apt", [128, 3, 16])
        P.dma(lambda e: e.dma_start(out=apt[:], in_=ssm_ap_d[:, :, :]), writes=[R("apt")])
        act(apt[:, 2, :], apt[:, 2, :], AF.Exp, [R("apt")], [R("apt")])
        vtt("dve", ssm_p[:, 0, :], apt[:, 0, :], apt[:, 2, :], ALU.mult, [R("apt")], [R("ssm_p")])
        act(ssm_p[:, 0, :], ssm_p[:, 0, :], AF.Exp, [R("ssm_p")], [R("ssm_p")])
        vtt("dve", ssm_p[:, 1, :], apt[:, 1, :], apt[:, 2, :], ALU.mult, [R("apt")], [R("ssm_p")])
        wst0 = sb("wst0", [128, 2, 512])
        load_w_bf16(wglu_bf, w_glu_d, 512, 512, None, [(0, 512, 0)], wst0, "wst0", "wglu_bf")
        P.barrier()
        st0.close()
        cur.pop()

        def rms_rstd(src_ap, src_res, slot):
            P.op("act", lambda e: e.activation(out=junk[:], in_=src_ap, func=AF.Square,
                                               accum_out=ssq[:, slot:slot + 1]),
                 reads=[src_res], writes=[R("junk"), R("ssq", slot)])
            P.op("act", lambda e: e.activation(out=ssq[:, slot:slot + 1], in_=ssq[:, slot:slot + 1],
                                               func=AF.Sqrt, bias=epsc[:, 0:1], scale=1.0 / D),
                 reads=[R("ssq", slot), R("epsc")], writes=[R("ssq", slot)])
            P.op("dve", lambda e: e.reciprocal(out=rstd[:, slot:slot + 1], in_=ssq[:, slot:slot + 1]),
                 reads=[R("ssq", slot)], writes=[R("rstd", slot)])

        def emit_hT(s):
            for j in range(TPS):
                tok0 = s * TS + j * 128
                P.dma(lambda e, j=j, tok0=tok0: e.dma_start(out=xt[:, j, :], in_=x_d[tok0:tok0 + 128, :]),
                      writes=[R("xt", j)])
                slot = j % 4
                rms_rstd(xt[:, j, :], R("xt", j), slot)
                b = j % 2
                stt("dve", xn[:, b, :], xt[:, j, :], rstd[:, slot:slot + 1], gb[:], ALU.mult, ALU.mult,
                    [R("xt", j), R("rstd", slot), R("gb")], [R("xn", b)])
                for hf in range(2):
                    bank = hf
                    for cc in range(4):
                        c = hf * 4 + cc
                        P.op("pe", lambda e, b=b, c=c, cc=cc, bank=bank: e.transpose(
                            out=PS[bank][:, cc * 128:(cc + 1) * 128], in_=xn[:, b, c * 128:(c + 1) * 128],
                            identity=ident[:]), reads=[R("xn", b), R("ident")], writes=[R("ps", bank)])
                    eng = "dve" if hf == 0 else "act"
                    outap = hT[:, hf * 4:hf * 4 + 4, j * 128:(j + 1) * 128]
                    inap = PS[bank][:, :].rearrange("p (a b) -> p a b", a=4)
                    if eng == "dve":
                        P.op("dve", lambda e, outap=outap, inap=inap: e.tensor_copy(out=outap, in_=inap),
                             reads=[R("ps", bank)], writes=[R("hT", j)])
                    else:
                        P.op("act", lambda e, outap=outap, inap=inap: e.copy(out=outap, in_=inap),
                             reads=[R("ps", bank)], writes=[R("hT", j)])

        if stop == 1:
            return early_exit()
        st_a = ExitStack()
        cur.append(st_a)
        win_bf = sb("win_bf", [128, DC, DIN_L], BF16)
        wstage = sb("wstage", [128, 2, 2048], F32)
        xt = sb("xt", [128, TPS, D], F32)
        hT = sb("hT", [128, DC, TS], BF16)
        load_w_bf16(win_bf, w_in_d, D, 1280, 0,
                    [(0, 1024, 0), (1024, 64, 1024), (1024, 64, 1088), (1088, 64, 1152), (1088, 64, 1216),
                     (1152, 128, 1280)], wstage, "wstage", "win_bf")
        win_res = [R("win_bf", c) for c in range(DC)]

        load_gb(0)
        for s in range(NS):
            emit_hT(s)
            hres = [R("hT", j) for j in range(TPS)]
            cols = slice(s * TS, (s + 1) * TS)
            for o in range(10):
                bank = 2 + (o % 4)
                for c in range(DC):
                    P.op("pe", lambda e, o=o, c=c, bank=bank: e.matmul(
                        PS[bank][:, 0:TS], win_bf[:, c, o * 128:(o + 1) * 128], hT[:, c, :],
                        start=(c == 0), stop=(c == DC - 1)),
                        reads=hres + win_res, writes=[R("ps", bank)])
                if o < 4:
                    dst, key = uT[:, o, cols], ("uT", o, s)
                elif o < 8:
                    dst, key = qT[:, o - 4, cols], None
                else:
                    dst, key = kT[:, o - 8, cols], ("kT", o - 8, s)
                wr = [R(*key)] if key is not None else [R("qT", o - 4, s * TPS + jj) for jj in range(TPS)]
                if o % 2 == 0:
                    P.op("dve", lambda e, dst=dst, bank=bank: e.tensor_copy(out=dst, in_=PS[bank][:, 0:TS]),
                         reads=[R("ps", bank)], writes=wr)
                else:
                    P.op("act", lambda e, dst=dst, bank=bank: e.copy(out=dst, in_=PS[bank][:, 0:TS]),
                         reads=[R("ps", bank)], writes=wr)
            for j in range(TPS):
                bank = 6 + (j % 2)
                for c in range(DC):
                    P.op("pe", lambda e, j=j, c=c, bank=bank: e.matmul(
                        PS[bank][:, 0:128], hT[:, c, j * 128:(j + 1) * 128], win_bf[:, c, 1280:1408],
                        start=(c == 0), stop=(c == DC - 1)),
                        reads=hres + win_res, writes=[R("ps", bank)])
                tt = s * TPS + j
                P.op("dve", lambda e, tt=tt, bank=bank: e.tensor_copy(out=vS[:, tt, :], in_=PS[bank][:, 0:128]),
                     reads=[R("ps", bank)], writes=[R("vS", tt)])
        P.barrier()
        st_a.close()
        cur.pop()

        if dbg:
            for nm, src, nt_ in (("uT", uT, 4), ("qT", qT, 4), ("kT", kT, 2)):
                dd = dout("dbg_" + nm, [nt_ * 128, T])
                for o in range(nt_):
                    tmp = sb("dbgt_%s%d" % (nm, o), [128, T])
                    P.op("dve", lambda e, tmp=tmp, src=src, o=o: e.tensor_copy(out=tmp[:], in_=src[:, o, :]),
                         reads=([R(nm, o, s) for s in range(NS)] if nm != "qT" else [R("qT", o, b) for b in range(NT)]),
                         writes=[R("dbgt", nm, o)])
                    P.dma(lambda e, dd=dd, tmp=tmp, o=o: e.dma_start(out=dd[o * 128:(o + 1) * 128, :], in_=tmp[:]),
                          reads=[R("dbgt", nm, o)])
            dd = dout("dbg_v", [T, 128])
            for tt in range(NT):
                tmp = sb("dbgt_v%d" % tt, [128, 128])
                P.op("dve", lambda e, tmp=tmp, tt=tt: e.tensor_copy(out=tmp[:], in_=vS[:, tt, :]),
                     reads=[R("vS", tt)], writes=[R("dbgt", "v", tt)])
                P.dma(lambda e, dd=dd, tmp=tmp, tt=tt: e.dma_start(out=dd[tt * 128:(tt + 1) * 128, :], in_=tmp[:]),
                      reads=[R("dbgt", "v", tt)])


        if stop == 2:
            return early_exit()
        Tc = TS
        st_s = ExitStack()
        cur.append(st_s)
        io_i = sb("io_i", [128, Tc + 1], I32)
        io_f = sb("io_f", [128, Tc + 1])
        P.op("pool", lambda e: e.iota(io_i[:], pattern=[[1, Tc + 1]], base=0, channel_multiplier=0),
             writes=[R("io_i")])
        P.op("dve", lambda e: e.tensor_copy(out=io_f[:], in_=io_i[:]), reads=[R("io_i")], writes=[R("io_f")])
        cosT = sb("cosT", [128, 4, Tc + 1])
        sinT = sb("sinT", [128, 4, Tc + 1])
        rT = sb("rT", [128, 4, Tc])
        sc_f = sb("sc_f", [128, Tc + 1])
        sc_i = sb("sc_i", [128, Tc + 1], I32)
        sc_p = sb("sc_p", [128, Tc + 1])
        tA = sb("tA", [128, 2, 4, Tc])
        tE = sb("tE", [128, 2, 2, Tc])
        sRI = sb("sRI", [128, 2, 2, Tc], BF16)
        winit = sb("winit", [128, 16, 2])
        gl_t = sb("gl_t", [128, 2, Tc])
        st_t = ExitStack()
        cur.append(st_t)
        abias = sb("abias", [128, NH, 256])
        sinkb = sb("sinkb", [128, NH])
        P.dma(lambda e: e.dma_start(out=abias[:], in_=abias_d[:, :, :]), writes=[R("abias")])
        P.dma(lambda e: e.dma_start(out=sinkb[:], in_=sinkb_d[:, :]), writes=[R("sinkb")])
        sS = sb("sS", [128, 2, 256])
        eS = sb("eS", [128, 2, 256])
        pT = sb("pT", [128, 2, 2, 128], BF16)
        sm = sb("sm", [128, 2, 4])
        rden = sb("rden", [128, 2, NH])
        osb = sb("osb", [128, 2, 512])
        def ssm_gen():
            units = [(ct, s_, m) for ct in range(4) for s_ in range(NS) for m in range(4)]
            ybank = 2

            def emit_bu(k):
                ct, s_, m = units[k]
                gp = ct * 4 + m
                par = m % 2
                gsl = slice(gp * 128, (gp + 1) * 128)
                cols = slice(s_ * Tc, (s_ + 1) * Tc)
                ures = R("uT", ct, s_)
                P.op("pe", lambda e: e.matmul(PS[0][:, 0:Tc], bbar[:, 0, gsl], uT[:, ct, cols],
                                              start=True, stop=True),
                     reads=[R("bbar"), ures], writes=[R("ps", 0)])
                P.op("pe", lambda e: e.matmul(PS[1][:, 0:Tc], bbar[:, 1, gsl], uT[:, ct, cols],
                                              start=True, stop=True),
                     reads=[R("bbar"), ures], writes=[R("ps", 1)])

            emit_bu(0)
            for k, (ct, s_, m) in enumerate(units):
                if s_ == 0 and m == 0:
                    for m2 in range(4):
                        gp2 = ct * 4 + m2
                        ts("dve", sc_p[:], io_f[:], ssm_p[:, 1, gp2:gp2 + 1], ALU.mult,
                           [R("io_f"), R("ssm_p")], [R("sc_p")])
                        sincos(sc_p[:], sinT[:, m2, :], cosT[:, m2, :], sc_f[:], sc_i[:],
                               R("sc_p"), R("sinT", m2), R("cosT", m2), R("sc_f"), R("sc_i"))
                        ts("dve", rT[:, m2, :], io_f[:, 0:Tc], 0.0, ALU.mult, [R("io_f"), R("ssm_p")], [R("rT", m2)],
                           s2=ssm_p[:, 0, gp2:gp2 + 1], op1=ALU.add)
                cols = slice(s_ * Tc, (s_ + 1) * Tc)
                gp = ct * 4 + m
                par = m % 2
                gsl = slice(gp * 128, (gp + 1) * 128)
                b_re, b_im = PS[0], PS[1]
                rbr, rbi = R("ps", 0), R("ps", 1)
                A_, B_, C_, D_ = [tA[:, par, i, :] for i in range(4)]
                rA_, rB_, rC_, rD_ = [R("tA", par, i) for i in range(4)]
                cs_, sn_ = cosT[:, m, 0:Tc], sinT[:, m, 0:Tc]
                rcs, rsn = R("cosT", m), R("sinT", m)
                vtt("dve", A_, cs_, b_re[:, 0:Tc], ALU.mult, [rcs, rbr], [rA_])
                vtt("dve", B_, sn_, b_im[:, 0:Tc], ALU.mult, [rsn, rbi], [rB_])
                vtt("dve", A_, A_, B_, ALU.add, [rA_, rB_], [rA_])
                vtt("dve", C_, cs_, b_im[:, 0:Tc], ALU.mult, [rcs, rbi], [rC_])
                vtt("dve", D_, sn_, b_re[:, 0:Tc], ALU.mult, [rsn, rbr], [rD_])
                vtt("dve", C_, C_, D_, ALU.subtract, [rC_, rD_], [rC_])
                if k + 1 < len(units):
                    emit_bu(k + 1)
                if s_ == 0:
                    i_re, i_im = 0.0, 0.0
                else:
                    i_re, i_im = winit[:, gp, 0:1], winit[:, gp, 1:2]
                P.op("dve", lambda e, B_=B_, A_=A_, m=m, i_re=i_re: e.tensor_tensor_scan(
                    out=B_, data0=rT[:, m, :], data1=A_, initial=i_re, op0=ALU.mult, op1=ALU.add),
                    reads=[R("rT", m), rA_, R("winit", gp)], writes=[rB_])
                P.op("dve", lambda e, D_=D_, C_=C_, m=m, i_im=i_im: e.tensor_tensor_scan(
                    out=D_, data0=rT[:, m, :], data1=C_, initial=i_im, op0=ALU.mult, op1=ALU.add),
                    reads=[R("rT", m), rC_, R("winit", gp)], writes=[rD_])
                yield
                if s_ < NS - 1:
                    cT_, sT_ = cosT[:, m, Tc:Tc + 1], sinT[:, m, Tc:Tc + 1]
                    wl_re, wl_im = B_[:, Tc - 1:Tc], D_[:, Tc - 1:Tc]
                    ts("dve", winit[:, gp, 0:1], wl_re, cT_, ALU.mult, [rB_, rcs], [R("winit", gp)])
                    vtt("dve", sc_f[:, 0:1], wl_im, sT_, ALU.mult, [rD_, rsn], [R("sc_f")])
                    vtt("dve", winit[:, gp, 0:1], winit[:, gp, 0:1], sc_f[:, 0:1], ALU.subtract,
                        [R("sc_f"), R("winit", gp)], [R("winit", gp)])
                    ts("dve", winit[:, gp, 1:2], wl_re, sT_, ALU.mult, [rB_, rsn], [R("winit", gp)])
                    vtt("dve", sc_f[:, 0:1], wl_im, cT_, ALU.mult, [rD_, rcs], [R("sc_f")])
                    vtt("dve", winit[:, gp, 1:2], winit[:, gp, 1:2], sc_f[:, 0:1], ALU.add,
                        [R("sc_f"), R("winit", gp)], [R("winit", gp)])
                E_, F_ = tE[:, par, 0, :], tE[:, par, 1, :]
                rE_, rF_ = R("tE", par, 0), R("tE", par, 1)
                SR_, SI_ = sRI[:, par, 0, :], sRI[:, par, 1, :]
                rSR, rSI = R("sRI", par, 0), R("sRI", par, 1)
                E2_, F2_, rE2_, rF2_ = A_, C_, rA_, rC_
                vtt("dve", E_, cs_, B_, ALU.mult, [rcs, rB_], [rE_])
                vtt("dve", F_, sn_, D_, ALU.mult, [rsn, rD_], [rF_])
                vtt("pool", SR_, E_, F_, ALU.subtract, [rE_, rF_], [rSR])
                vtt("dve", E2_, sn_, B_, ALU.mult, [rsn, rB_], [rE2_])
                vtt("dve", F2_, cs_, D_, ALU.mult, [rcs, rD_], [rF2_])
                vtt("pool", SI_, E2_, F2_, ALU.add, [rE2_, rF2_], [rSI])
                P.op("pe", lambda e, gsl=gsl, SR_=SR_, m=m: e.matmul(PS[ybank][:, 0:Tc], ctb[:, 0, gsl], SR_, start=(m == 0), stop=False),
                     reads=[R("ctb"), rSR], writes=[R("ps", ybank)])
                P.op("pe", lambda e, gsl=gsl, SI_=SI_, m=m: e.matmul(PS[ybank][:, 0:Tc], ctb[:, 1, gsl], SI_, start=False, stop=(m == 3)),
                     reads=[R("ctb"), rSI], writes=[R("ps", ybank)])
                if m == 3:
                    un = k // 4
                    gt = gl_t[:, un % 2, :]
                    rg = R("gl_t", un % 2)
                    stt("dve", gt, uT[:, ct, cols], ssm_dg[:, ct:ct + 1], PS[ybank][:, 0:Tc], ALU.mult, ALU.add,
                        [R("uT", ct, s_), R("ssm_dg"), R("ps", ybank)], [rg])
                    act(uT[:, ct, cols], gt, AF.Gelu, [rg], [R("uT", ct, s_)])
                yield

        def att_gen():
            steps = [(qb, h) for qb in range(NT) for h in range(NH)]
            OB_ = 7

            def geom(qb):
                c0 = 0 if qb > 0 else 128
                kc0 = (qb - 1) * 128 if qb > 0 else 0
                kcols = slice(kc0, (qb + 1) * 128)
                s_cur = qb // TPS
                kres = [s_cur] if (qb == 0 or (qb - 1) // TPS == s_cur) else [s_cur - 1, s_cur]
                kbs = [0, 1] if qb > 0 else [1]
                return c0, kcols, kres, kbs

            def emit_S(i):
                qb, h = steps[i]
                c0, kcols, kres, kbs = geom(qb)
                kv, qt_, pb, par = h // 4, h // 2, 64 * (h % 2), h % 2
                so, SB_ = 0, 3 + par
                P.op("pe", lambda e: e.matmul(
                    PS[SB_][:, so + c0:so + 256], qT[pb:pb + 64, qt_, qb * 128:(qb + 1) * 128],
                    kT[pb:pb + 64, kv, kcols], start=True, stop=True),
                    reads=[R("qT", qt_, qb)] + [R("kT", kv, s_) for s_ in kres], writes=[R("ps", SB_)])

            def emit_st2(i):
                qb, h = steps[i]
                c0, kcols, kres, kbs = geom(qb)
                par = h % 2
                so, SB_ = 0, 3 + par
                rs, re_, rsm = R("sS", par), R("eS", par), R("sm", par)
                stt("dve", sS[:, par, c0:256], PS[SB_][:, so + c0:so + 256], HD ** -0.5, abias[:, h, c0:256],
                    ALU.mult, ALU.add, [R("ps", SB_), R("abias")], [rs])
                P.op("dve", lambda e: e.reduce_max(out=sm[:, par, 0:1], in_=sS[:, par, c0:256], axis=AX.X),
                     reads=[rs], writes=[rsm])
                vtt("dve", sm[:, par, 0:1], sm[:, par, 0:1], sinkb[:, h:h + 1], ALU.max, [rsm, R("sinkb")], [rsm])
                ts("dve", sm[:, par, 1:2], sm[:, par, 0:1], -1.0, ALU.mult, [rsm], [rsm])
                act(eS[:, par, c0:256], sS[:, par, c0:256], AF.Exp, [rs, rsm], [re_, rsm],
                    bias=sm[:, par, 1:2], accum=sm[:, par, 2:3])
                act(sm[:, par, 3:4], sinkb[:, h:h + 1], AF.Exp, [R("sinkb"), rsm], [rsm], bias=sm[:, par, 1:2])

            def emit_st3(i):
                qb, h = steps[i]
                c0, kcols, kres, kbs = geom(qb)
                par, qpar = h % 2, qb % 2
                to, TB_ = 0, 5 + par
                re_, rsm, rp = R("eS", par), R("sm", par), R("pT", par)
                vtt("dve", sm[:, par, 2:3], sm[:, par, 2:3], sm[:, par, 3:4], ALU.add, [rsm], [rsm])
                P.op("dve", lambda e: e.reciprocal(out=rden[:, qpar, h:h + 1], in_=sm[:, par, 2:3]),
                     reads=[rsm], writes=[R("rden", qpar)])
                for kb in kbs:
                    P.op("pe", lambda e, kb=kb: e.transpose(
                        out=PS[TB_][:, to + kb * 128:to + (kb + 1) * 128], in_=eS[:, par, kb * 128:(kb + 1) * 128],
                        identity=ident[:]), reads=[re_, R("ident")], writes=[R("ps", TB_)])
                k0 = kbs[0]
                P.op("act", lambda e: e.copy(
                    out=pT[:, par, k0:2, :], in_=PS[TB_][:, to + k0 * 128:to + 256].rearrange("p (a b) -> p a b", b=128)),
                    reads=[R("ps", TB_)], writes=[rp])

            def emit_PV(i):
                qb, h = steps[i]
                c0, kcols, kres, kbs = geom(qb)
                par, kv = h % 2, h // 4
                rp = R("pT", par)
                for kb in kbs:
                    vt = qb - 1 + kb
                    P.op("pe", lambda e, kb=kb, vt=vt: e.matmul(
                        PS[OB_][:, h * 64:(h + 1) * 64], pT[:, par, kb, :], vS[:, vt, kv * 64:(kv + 1) * 64],
                        start=(kb == kbs[0]), stop=(kb == 1)),
                        reads=[rp, R("vS", vt)], writes=[R("ps", OB_)])

            def emit_epi(qb):
                qpar = qb % 2
                ro = R("osb", qpar)
                P.op("dve", lambda e: e.tensor_tensor(
                    out=osb[:, qpar, :].rearrange("p (a b) -> p a b", b=64),
                    in0=PS[OB_][:, :].rearrange("p (a b) -> p a b", b=64),
                    in1=rden[:, qpar, :].unsqueeze(2).broadcast_to([128, NH, 64]), op=ALU.mult),
                    reads=[R("ps", OB_), R("rden", qpar)], writes=[ro])
                TB_ = 5
                tres = [R("ps", 5)]
                for ft in range(4):
                    P.op("pe", lambda e, ft=ft: e.transpose(
                        out=PS[TB_][:, ft * 128:(ft + 1) * 128], in_=osb[:, qpar, ft * 128:(ft + 1) * 128],
                        identity=ident[:]), reads=[ro, R("ident")], writes=tres)
                P.op("act", lambda e: e.copy(
                    out=qT[:, 0:4, qb * 128:(qb + 1) * 128], in_=PS[TB_][:, :].rearrange("p (a b) -> p a b", b=128)),
                    reads=tres, writes=[R("qT", ft, qb) for ft in range(4)])

            n = len(steps)
            emit_S(0)
            for i in range(n):
                if i + 1 < n:
                    emit_S(i + 1)
                if i >= 1:
                    emit_PV(i - 1)
                    if steps[i - 1][1] == NH - 1:
                        emit_epi(steps[i - 1][0])
                emit_st2(i)
                yield
                emit_st3(i)
                yield
            emit_PV(n - 1)
            emit_epi(steps[n - 1][0])
            yield

        g_ssm, g_att = ssm_gen(), att_gen()
        live = {"s": True, "a": True}

        def adv(g, k_):
            if live[k_]:
                try:
                    next(g)
                except StopIteration:
                    live[k_] = False

        while live["s"] or live["a"]:
            adv(g_att, "a")
            adv(g_ssm, "s")
            adv(g_att, "a")

        gbank = [0, 1, 2, 3]
        for s_ in range(NS):
            cols = slice(s_ * Tc, (s_ + 1) * Tc)
            yres = [R("uT", k, s_) for k in range(4)]
            for ft in range(4):
                for kc in range(4):
                    P.op("pe", lambda e, ft=ft, kc=kc, cols=cols: e.matmul(
                        PS[gbank[ft]][:, 0:Tc], wglu_bf[:, kc, ft * 128:(ft + 1) * 128], uT[:, kc, cols],
                        start=(kc == 0), stop=(kc == 3)),
                        reads=yres + [R("wglu_bf", kc)], writes=[R("ps", gbank[ft])])
            for ft in range(4):
                gt = gl_t[:, ft % 2, :]
                rg = R("gl_t", ft % 2)
                act(gt, PS[gbank[ft]][:, 0:Tc], AF.Sigmoid, [R("ps", gbank[ft]), R("ssm_dg")], [rg],
                    bias=ssm_dg[:, 4 + ft:5 + ft])
                vtt("dve", uT[:, ft, cols], uT[:, ft, cols], gt, ALU.mult, [rg, R("uT", ft, s_)], [R("uT", ft, s_)])
        if dbg:
            dd = dout("dbg_yssmT", [512, T])
            for o in range(4):
                tmp = sb("dbgys%d" % o, [128, T])
                P.op("dve", lambda e, tmp=tmp, o=o: e.tensor_copy(out=tmp[:], in_=uT[:, o, :]),
                     reads=[R("uT", o, s) for s in range(NS)], writes=[R("dbgys", o)])
                P.dma(lambda e, dd=dd, tmp=tmp, o=o: e.dma_start(out=dd[o * 128:(o + 1) * 128, :], in_=tmp[:]),
                      reads=[R("dbgys", o)])
        if dbg:
            dd = dout("dbg_yattT", [512, T])
            for o in range(4):
                tmp = sb("dbgya%d" % o, [128, T])
                P.op("dve", lambda e, tmp=tmp, o=o: e.tensor_copy(out=tmp[:], in_=qT[:, o, :]),
                     reads=[R("qT", o, b) for b in range(NT)], writes=[R("dbgya", o)])
                P.dma(lambda e, dd=dd, tmp=tmp, o=o: e.dma_start(out=dd[o * 128:(o + 1) * 128, :], in_=tmp[:]),
                      reads=[R("dbgya", o)])
        P.barrier()
        st_t.close()
        cur.pop()
        st_s.close()
        cur.pop()
        st_sw.close()
        cur.pop()
        st_kv.close()
        cur.pop()

        if stop == 4:
            return early_exit()
        st_5 = ExitStack()
        cur.append(st_5)
        xt = sb("xt5", [128, TPS, D], F32)
        hT = sb("hT5", [128, DC, TS], BF16)
        wgate_bf = sb("wgate_bf", [128, DC, 2 * D], BF16)
        wA_bf = sb("wA_bf", [128, 4, D], BF16)
        wB_bf = sb("wB_bf", [128, 4, D], BF16)
        wout_bf = sb("wout_bf", [128, DC, D], BF16)
        wst5 = sb("wst5", [128, 2, D], F32)
        bgate = sb("bgate", [128, 16])
        P.dma(lambda e: e.dma_start(out=bgate[:], in_=bgate_d[:, :]), writes=[R("bgate")])
        for hh in range(2):
            load_w_bf16(wgate_bf[:, :, hh * D:(hh + 1) * D], w_gate_d[:, hh * D:(hh + 1) * D], D, D, 0,
                        [(0, D, 0)], wst5, "wst5", "wgate_bf%d" % hh)
        load_w_bf16(wA_bf, w_bra_d, 512, D, None, [(0, D, 0)], wst5, "wst5", "wA_bf")
        load_w_bf16(wB_bf, w_brb_d, 512, D, None, [(0, D, 0)], wst5, "wst5", "wB_bf")
        load_w_bf16(wout_bf, w_out_d, D, D, None, [(0, D, 0)], wst5, "wst5", "wout_bf")
        wg_res = [R("wgate_bf0", c) for c in range(DC)] + [R("wgate_bf1", c) for c in range(DC)]
        gts = sb("gts", [128, 2, 2, TS])
        mT = sb("mT", [128, DC, TS], BF16)
        x1t = sb("x1t", [128, 2, D])
        for s in range(NS):
            emit_hT(s)
            hres = [R("hT", j) for j in range(TPS)]
            cols = slice(s * TS, (s + 1) * TS)
            for f in range(DC):
                fp = f % 2
                for br in range(2):
                    bank = 4 * fp + br
                    for c in range(DC):
                        P.op("pe", lambda e, bank=bank, c=c, f=f, br=br: e.matmul(
                            PS[bank][:, 0:TS], wgate_bf[:, c, br * D + f * 128:br * D + (f + 1) * 128], hT[:, c, :],
                            start=(c == 0), stop=(c == DC - 1)),
                            reads=hres + wg_res, writes=[R("ps", bank)])
                for br, (wbr, src, tag) in enumerate(((wA_bf, uT, "wA_bf"), (wB_bf, qT, "wB_bf"))):
                    bank = 4 * fp + 2 + br
                    if br == 0:
                        srcres = [R("uT", k, s) for k in range(4)]
                    else:
                        srcres = [R("qT", k, s * TPS + jj) for k in range(4) for jj in range(TPS)]
                    for k in range(4):
                        P.op("pe", lambda e, bank=bank, k=k, f=f, wbr=wbr, src=src, cols=cols: e.matmul(
                            PS[bank][:, 0:TS], wbr[:, k, f * 128:(f + 1) * 128], src[:, k, cols],
                            start=(k == 0), stop=(k == 3)),
                            reads=srcres + [R(tag, k) for k in range(4)], writes=[R("ps", bank)])
                for br in range(2):
                    bank = 4 * fp + br
                    rg = R("gts", fp, br)
                    act(gts[:, fp, br, :], PS[bank][:, 0:TS], AF.Sigmoid, [R("ps", bank), R("bgate")], [rg],
                        bias=bgate[:, br * 8 + f:br * 8 + f + 1])
                    vtt("dve", gts[:, fp, br, :], gts[:, fp, br, :], PS[4 * fp + 2 + br][:, 0:TS], ALU.mult,
                        [rg, R("ps", 4 * fp + 2 + br)], [rg])
                vtt("dve", mT[:, f, :], gts[:, fp, 0, :], gts[:, fp, 1, :], ALU.add,
                    [R("gts", fp, 0), R("gts", fp, 1)], [R("mT", f)])
            mres = [R("mT", f) for f in range(DC)]
            wo_res = [R("wout_bf", c) for c in range(DC)]
            for j in range(TPS):
                tix = s * TPS + j
                xb = tix % 2
                for hf in range(2):
                    bank = (2 * j + hf) % 8
                    for f in range(DC):
                        P.op("pe", lambda e, bank=bank, f=f, j=j, hf=hf: e.matmul(
                            PS[bank][:, :], mT[:, f, j * 128:(j + 1) * 128], wout_bf[:, f, hf * 512:(hf + 1) * 512],
                            start=(f == 0), stop=(f == DC - 1)),
                            reads=mres + wo_res, writes=[R("ps", bank)])
                    vtt("dve", x1t[:, xb, hf * 512:(hf + 1) * 512], PS[bank][:, :], xt[:, j, hf * 512:(hf + 1) * 512],
                        ALU.add, [R("ps", bank), R("xt", j)], [R("x1t", xb)])
                P.dma(lambda e, tix=tix, xb=xb: e.dma_start(out=x1_d[tix * 128:(tix + 1) * 128, :], in_=x1t[:, xb, :]),
                      reads=[R("x1t", xb)], writes=[R("x1d", tix)])
                if dbg:
                    if tix == 0:
                        dbg_x1 = dout("dbg_x1", [T, D])
                    P.dma(lambda e, tix=tix, xb=xb: e.dma_start(out=dbg_x1[tix * 128:(tix + 1) * 128, :], in_=x1t[:, xb, :]),
                          reads=[R("x1t", xb)])
        P.barrier()
        st_5.close()
        cur.pop()
        st_mix.close()
        cur.pop()

        if stop == 5:
            return early_exit()
        NHALF = 2 if T >= 1024 else 1
        TH = T // NHALF
        NTH = TH // 128
        TCH = min(512, TH)
        NCH = TH // TCH
        TPC = TCH // 128
        BIG = 1.0e4
        st_b = ExitStack()
        cur.append(st_b)
        acc = sb("acc", [128, NTH, D])
        h2T = sb("h2T", [128, DC, TH], BF16)
        wden = sb("wden", [128, NTH, NE])
        for half in range(NHALF):
            st_b0 = ExitStack()
            cur.append(st_b0)
            wr32 = sb("wr32_%d" % half, [128, DC, 36])
            brt = sb("brt_%d" % half, [128, 36])
            wrs = sb("wrs_%d" % half, [128, DC, 36])
            wr_hi = sb("wr_hi_%d" % half, [128, DC, 36], BF16)
            wr_lo = sb("wr_lo_%d" % half, [128, DC, 36], BF16)
            P.dma(lambda e: e.dma_start(out=brt[:], in_=b_rt_d[0:1, :].partition_broadcast(128)), writes=[R("brt")])
            P.dma(lambda e: e.dma_start(out=wr32[:], in_=w_rt_d.rearrange("(c p) n -> p c n", p=128)), writes=[R("wr32")])
            P.op("pool", lambda e: e.tensor_copy(out=wr_hi[:], in_=wr32[:]), reads=[R("wr32")], writes=[R("wr_hi")])
            P.op("pool", lambda e: e.tensor_copy(out=wrs[:], in_=wr_hi[:]), reads=[R("wr_hi")], writes=[R("wrs")])
            vtt("pool", wrs[:], wr32[:], wrs[:], ALU.subtract, [R("wr32"), R("wrs")], [R("wrs")])
            P.op("pool", lambda e: e.tensor_copy(out=wr_lo[:], in_=wrs[:]), reads=[R("wrs")], writes=[R("wr_lo")])
            load_gb(1)
            lgall = sb("lgall_%d" % half, [128, NTH, 36])
            hlo2 = sb("hlo2_%d" % half, [128, 2, DC, 128], BF16)
            pend = []

            def emit_lg(j, rbank):
                vtt("dve", lgall[:, j, :], PS[rbank][:, 0:36], brt[:], ALU.add, [R("ps", rbank), R("brt")], [R("lgall")])

            for j in range(NTH):
                tix = half * NTH + j
                P.dma(lambda e, j=j, tix=tix: e.dma_start(out=acc[:, j, :], in_=x1_d[tix * 128:(tix + 1) * 128, :]),
                      reads=[R("x1d", tix)], writes=[R("acc", j)])
                slot = j % 4
                rms_rstd(acc[:, j, :], R("acc", j), slot)
                b = j % 2
                stt("dve", xn[:, b, :], acc[:, j, :], rstd[:, slot:slot + 1], gb[:], ALU.mult, ALU.mult,
                    [R("acc", j), R("rstd", slot), R("gb")], [R("xn", b)])
                jc = slice(j * 128, (j + 1) * 128)
                for hf in range(2):
                    bank = hf
                    for cc in range(4):
                        c = hf * 4 + cc
                        P.op("pe", lambda e, b=b, c=c, cc=cc, bank=bank: e.transpose(
                            out=PS[bank][:, cc * 128:(cc + 1) * 128], in_=xn[:, b, c * 128:(c + 1) * 128],
                            identity=ident[:]), reads=[R("xn", b), R("ident")], writes=[R("ps", bank)])
                    inap = PS[bank][:, :].rearrange("p (a b) -> p a b", a=4)
                    hsl = slice(hf * 4, hf * 4 + 4)
                    P.op("dve", lambda e, hsl=hsl, jc=jc, inap=inap: e.tensor_copy(out=h2T[:, hsl, jc], in_=inap),
                         reads=[R("ps", bank)], writes=[R("h2T", j)])
                    P.op("dve", lambda e, hsl=hsl, jc=jc, inap=inap, b=b: e.tensor_tensor(
                        out=hlo2[:, b, hsl, :], in0=inap, in1=h2T[:, hsl, jc], op=ALU.subtract),
                        reads=[R("ps", bank), R("h2T", j)], writes=[R("hlo2", b)])
                rbank = 2 + (j % 2)
                k_ = 0
                for (ha, hres_, wa, wres_) in ((h2T[:, :, jc], R("h2T", j), wr_hi, R("wr_hi")),
                                               (h2T[:, :, jc], R("h2T", j), wr_lo, R("wr_lo")),
                                               (hlo2[:, b, :, :], R("hlo2", b), wr_hi, R("wr_hi"))):
                    for c in range(DC):
                        P.op("pe", lambda e, c=c, rbank=rbank, ha=ha, wa=wa, k_=k_: e.matmul(
                            PS[rbank][:, 0:36], ha[:, c, :], wa[:, c, :], start=(k_ == 0), stop=(k_ == 3 * DC - 1)),
                            reads=[hres_, wres_], writes=[R("ps", rbank)])
                        k_ += 1
                if pend:
                    emit_lg(*pend.pop())
                pend.append((j, rbank))
            emit_lg(*pend.pop())
            N_ = NTH
            r1 = sb("r1_%d" % half, [128, 8, N_])
            g4 = sb("g4_%d" % half, [128, 3, N_, 4])
            e32 = sb("e32_%d" % half, [128, 3, N_, NE])
            rL, r1r, rg4, re32 = R("lgall"), R("r1"), R("g4"), R("e32")
            G_ = lgall[:, :, 0:4]
            gm, gsum, gpr, m1, m2, ex_, w1, w2 = (r1[:, i, :] for i in range(8))

            def bc(v, n):
                return v.unsqueeze(2).broadcast_to([128, N_, n])

            P.op("dve", lambda e: e.reduce_max(out=gm, in_=G_, axis=AX.X), reads=[rL], writes=[r1r])
            vtt("dve", g4[:, 0, :, :], G_, bc(gm, 4), ALU.subtract, [rL, r1r], [rg4])
            ts("dve", g4[:, 1, :, :], g4[:, 0, :, :], 0.0, ALU.is_ge, [rg4], [rg4])
            act(g4[:, 0, :, :], g4[:, 0, :, :], AF.Exp, [rg4], [rg4])
            P.op("dve", lambda e: e.reduce_sum(out=gsum, in_=g4[:, 0, :, :], axis=AX.X), reads=[rg4], writes=[r1r])
            P.op("dve", lambda e: e.reciprocal(out=gpr, in_=gsum), reads=[r1r], writes=[r1r])
            ts("dve", g4[:, 2, :, :], g4[:, 1, :, :], BIG, ALU.mult, [rg4], [rg4], s2=-BIG, op1=ALU.add)
            em_, oh1, oh2 = e32[:, 0, :, :], e32[:, 1, :, :], e32[:, 2, :, :]
            P.op("dve", lambda e: e.tensor_copy(out=em_, in_=lgall[:, :, 4:36]), reads=[rL], writes=[re32])
            P.op("dve", lambda e: e.tensor_tensor(
                out=em_.rearrange("p n (a b) -> p (n a) b", b=8), in0=em_.rearrange("p n (a b) -> p (n a) b", b=8),
                in1=g4[:, 2, :, :].rearrange("p n a -> p (n a)").unsqueeze(2).broadcast_to([128, N_ * 4, 8]),
                op=ALU.add), reads=[rg4, re32], writes=[re32])
            P.op("dve", lambda e: e.reduce_max(out=m1, in_=em_, axis=AX.X), reads=[re32], writes=[r1r])
            vtt("dve", oh1, em_, bc(m1, NE), ALU.subtract, [re32, r1r], [re32])
            ts("dve", oh1, oh1, 0.0, ALU.is_ge, [re32], [re32])
            stt("dve", em_, oh1, -BIG, em_, ALU.mult, ALU.add, [re32], [re32])
            P.op("dve", lambda e: e.reduce_max(out=m2, in_=em_, axis=AX.X), reads=[re32], writes=[r1r])
            vtt("dve", oh2, em_, bc(m2, NE), ALU.subtract, [re32, r1r], [re32])
            ts("dve", oh2, oh2, 0.0, ALU.is_ge, [re32], [re32])
            vtt("dve", ex_, m2, m1, ALU.subtract, [r1r], [r1r])
            act(ex_, ex_, AF.Exp, [r1r], [r1r])
            ts("dve", w1, ex_, 1.0, ALU.add, [r1r], [r1r])
            P.op("dve", lambda e: e.reciprocal(out=w1, in_=w1), reads=[r1r], writes=[r1r])
            vtt("dve", w2, ex_, w1, ALU.mult, [r1r], [r1r])
            vtt("dve", w1, w1, gpr, ALU.mult, [r1r], [r1r])
            vtt("dve", w2, w2, gpr, ALU.mult, [r1r], [r1r])
            wres_all = [R("wden", j) for j in range(NTH)]
            vtt("dve", oh1, oh1, bc(w1, NE), ALU.mult, [re32, r1r], [re32])
            vtt("dve", oh2, oh2, bc(w2, NE), ALU.mult, [re32, r1r], [re32])
            vtt("dve", wden[:, :, :], oh1, oh2, ALU.add, [re32], wres_all)
            P.barrier()
            st_b0.close()
            cur.pop()
            if stop == 6:
                return early_exit()
            st_e = ExitStack()
            cur.append(st_e)
            wgu_bf = sb("wgu_bf%d" % half, [128, 2, 2, DC, DFF], BF16)
            wd_bf = sb("wd_bf%d" % half, [128, 2, 4, D], BF16)
            wste = sb("wste%d" % half, [128, 2, 4096])
            aT = sb("aT%d" % half, [128, 2, 4, TCH], BF16)
            sgt = sb("sgt%d" % half, [128, 2, TCH])
            h2res = [R("h2T", j) for j in range(NTH)]
            stg = [0]

            def load_expert(e_):
                bsel = e_ % 2
                for mi, wd_ in enumerate((w_eg_d, w_eu_d, w_ed_d)):
                    si = stg[0] % 2
                    stg[0] += 1
                    rs_ = R("wste", si)
                    if mi < 2:
                        P.dma(lambda e, wd_=wd_, si=si, e_=e_: e.dma_start(
                            out=wste[:, si, :].rearrange("p (c n) -> p c n", n=DFF),
                            in_=wd_[e_].rearrange("(c p) n -> p c n", p=128)), writes=[rs_])
                        P.op("act", lambda e, bsel=bsel, mi=mi, si=si: e.copy(
                            out=wgu_bf[:, bsel, mi, :, :], in_=wste[:, si, :].rearrange("p (c n) -> p c n", n=DFF)),
                            reads=[rs_], writes=[R("wgu", bsel, mi)])
                    else:
                        P.dma(lambda e, wd_=wd_, si=si, e_=e_: e.dma_start(
                            out=wste[:, si, :].rearrange("p (c n) -> p c n", n=D),
                            in_=wd_[e_].rearrange("(c p) n -> p c n", p=128)), writes=[rs_])
                        P.op("dve", lambda e, bsel=bsel, si=si: e.tensor_copy(
                            out=wd_bf[:, bsel, :, :], in_=wste[:, si, :].rearrange("p (c n) -> p c n", n=D)),
                            reads=[rs_], writes=[R("wd", bsel)])

            load_expert(0)
            for e_ in range(n_exp):
                if e_ + 1 < n_exp:
                    load_expert(e_ + 1)
                bsel = e_ % 2
                for ch in range(NCH):
                    ap_ = ch % 2
                    ccols = slice(ch * TCH, (ch + 1) * TCH)
                    hres_c = h2res[ch * TPC:(ch + 1) * TPC]
                    for f in range(4):
                        fp = f % 2
                        for mi in range(2):
                            bank = 2 * fp + mi
                            for c in range(DC):
                                P.op("pe", lambda e, bank=bank, bsel=bsel, mi=mi, c=c, f=f, ccols=ccols: e.matmul(
                                    PS[bank][:, 0:TCH], wgu_bf[:, bsel, mi, c, f * 128:(f + 1) * 128], h2T[:, c, ccols],
                                    start=(c == 0), stop=(c == DC - 1)),
                                    reads=hres_c + [R("wgu", bsel, mi)], writes=[R("ps", bank)])
                        act(sgt[:, fp, :], PS[2 * fp][:, 0:TCH], AF.Silu, [R("ps", 2 * fp)], [R("sgt", fp)])
                        vtt("dve", aT[:, ap_, f, :], sgt[:, fp, :], PS[2 * fp + 1][:, 0:TCH], ALU.mult,
                            [R("sgt", fp), R("ps", 2 * fp + 1)], [R("aT", ap_, f)])
                    ares = [R("aT", ap_, f) for f in range(4)]
                    for jj in range(TPC):
                        j = ch * TPC + jj
                        for hf in range(2):
                            bank = 4 + (2 * jj + hf) % 4
                            for f in range(4):
                                P.op("pe", lambda e, bank=bank, ap_=ap_, f=f, jj=jj, bsel=bsel, hf=hf: e.matmul(
                                    PS[bank][:, :], aT[:, ap_, f, jj * 128:(jj + 1) * 128],
                                    wd_bf[:, bsel, f, hf * 512:(hf + 1) * 512], start=(f == 0), stop=(f == 3)),
                                    reads=ares + [R("wd", bsel)], writes=[R("ps", bank)])
                            stt("dve", acc[:, j, hf * 512:(hf + 1) * 512], PS[bank][:, :], wden[:, j, e_:e_ + 1],
                                acc[:, j, hf * 512:(hf + 1) * 512], ALU.mult, ALU.add,
                                [R("ps", bank), R("wden", j), R("acc", j)], [R("acc", j)])
            P.barrier()
            st_e.close()
            cur.pop()
            if stop == 7:
                return early_exit()
            st_c = ExitStack()
            cur.append(st_c)
            wpg_bf = sb("wpg_bf%d" % half, [128, DC, D], BF16)
            wpp_bf = sb("wpp_bf%d" % half, [128, 2, D], BF16)
            wstc = sb("wstc%d" % half, [128, 2, D])
            load_gb(2)
            bpg_b = sb("bpg_b%d" % half, [128, D])
            gfin_b = sb("gfin_b%d" % half, [128, D])
            P.dma(lambda e: e.dma_start(out=bpg_b[:], in_=b_pg_d[0:1, :].partition_broadcast(128)), writes=[R("bpg_b")])
            P.dma(lambda e: e.dma_start(out=gfin_b[:], in_=g_fin_d[0:1, :].partition_broadcast(128)), writes=[R("gfin_b")])
            load_w_bf16(wpg_bf, w_pg_d, D, D, 2, [(0, D, 0)], wstc, "wstc", "wpg_bf")
            load_w_bf16(wpp_bf, w_pp_d, PLE, D, None, [(0, D, 0)], wstc, "wstc", "wpp_bf")
            wpg_res = [R("wpg_bf", c) for c in range(DC)]
            wpp_res = [R("wpp_bf", c) for c in range(2)]
            h3T = sb("h3T%d" % half, [128, 2, DC, 128], BF16)
            ptl = sb("ptl%d" % half, [128, 2, PLE])
            pT_ = sb("pTt%d" % half, [128, 2, 2, 128], BF16)
            gtm = sb("gtm%d" % half, [128, 2, D])
            x3t = sb("x3t%d" % half, [128, 2, D])
            def c_tile(j):
                tix = half * NTH + j
                b = j % 2
                slot = j % 4
                slot2 = (j + 2) % 4
                base = 4 * b
                rms_rstd(acc[:, j, :], R("acc", j), slot)
                P.dma(lambda e: e.dma_start(out=ptl[:, b, :], in_=p_d[tix * 128:(tix + 1) * 128, :]),
                      writes=[R("ptl", b)])
                yield
                stt("dve", xn[:, b, :], acc[:, j, :], rstd[:, slot:slot + 1], gb[:], ALU.mult, ALU.mult,
                    [R("acc", j), R("rstd", slot), R("gb")], [R("xn", b)])
                for hf in range(2):
                    bank = base + hf
                    for cc in range(4):
                        c = hf * 4 + cc
                        P.op("pe", lambda e, c=c, cc=cc, bank=bank: e.transpose(
                            out=PS[bank][:, cc * 128:(cc + 1) * 128], in_=xn[:, b, c * 128:(c + 1) * 128],
                            identity=ident[:]), reads=[R("xn", b), R("ident")], writes=[R("ps", bank)])
                for c2 in range(2):
                    P.op("pe", lambda e, c2=c2: e.transpose(
                        out=PS[base + 2][:, c2 * 128:(c2 + 1) * 128], in_=ptl[:, b, c2 * 128:(c2 + 1) * 128],
                        identity=ident[:]), reads=[R("ptl", b), R("ident")], writes=[R("ps", base + 2)])
                yield
                for hf in range(2):
                    bank = base + hf
                    inap = PS[bank][:, :].rearrange("p (a b) -> p a b", a=4)
                    if hf == 0:
                        P.op("dve", lambda e, hf=hf, inap=inap: e.tensor_copy(out=h3T[:, b, hf * 4:hf * 4 + 4, :], in_=inap),
                             reads=[R("ps", bank)], writes=[R("h3T", b)])
                    else:
                        P.op("act", lambda e, hf=hf, inap=inap: e.copy(out=h3T[:, b, hf * 4:hf * 4 + 4, :], in_=inap),
                             reads=[R("ps", bank)], writes=[R("h3T", b)])
                P.op("act", lambda e: e.copy(out=pT_[:, b, :, :],
                                             in_=PS[base + 2][:, 0:256].rearrange("p (a b) -> p a b", b=128)),
                     reads=[R("ps", base + 2)], writes=[R("pT_", b)])
                yield
                for hf in range(2):
                    gbk, pb_ = base + 2 + hf, base + hf
                    hs = slice(hf * 512, (hf + 1) * 512)
                    for c in range(DC):
                        P.op("pe", lambda e, gbk=gbk, c=c, hs=hs: e.matmul(
                            PS[gbk][:, :], h3T[:, b, c, :], wpg_bf[:, c, hs], start=(c == 0), stop=(c == DC - 1)),
                            reads=[R("h3T", b)] + wpg_res, writes=[R("ps", gbk)])
                    for c2 in range(2):
                        P.op("pe", lambda e, pb_=pb_, c2=c2, hs=hs: e.matmul(
                            PS[pb_][:, :], pT_[:, b, c2, :], wpp_bf[:, c2, hs], start=(c2 == 0), stop=(c2 == 1)),
                            reads=[R("pT_", b)] + wpp_res, writes=[R("ps", pb_)])
                yield
                for hf in range(2):
                    gbk = base + 2 + hf
                    hs = slice(hf * 512, (hf + 1) * 512)
                    rg = R("gtm", b, hf)
                    vtt("dve", gtm[:, b, hs], PS[gbk][:, :], bpg_b[:, hs], ALU.add, [R("ps", gbk), R("bpg_b")], [rg])
                    act(gtm[:, b, hs], gtm[:, b, hs], AF.Sigmoid, [rg], [rg])
                yield
                for hf in range(2):
                    pb_ = base + hf
                    hs = slice(hf * 512, (hf + 1) * 512)
                    rg = R("gtm", b, hf)
                    vtt("dve", gtm[:, b, hs], gtm[:, b, hs], PS[pb_][:, :], ALU.mult, [rg, R("ps", pb_)], [rg])
                    vtt("dve", x3t[:, b, hs], gtm[:, b, hs], acc[:, j, hs], ALU.add, [rg, R("acc", j)], [R("x3t", b, hf)])
                x3res = [R("x3t", b, 0), R("x3t", b, 1)]
                if dbg:
                    P.dma(lambda e: e.dma_start(out=dbg_c[0][tix * 128:(tix + 1) * 128, :], in_=acc[:, j, :]),
                          reads=[R("acc", j)])
                    P.dma(lambda e: e.dma_start(out=dbg_c[1][tix * 128:(tix + 1) * 128, :], in_=x3t[:, b, :]),
                          reads=x3res)
                P.op("act", lambda e: e.activation(out=junk[:], in_=x3t[:, b, :], func=AF.Square,
                                                   accum_out=ssq[:, slot2:slot2 + 1]),
                     reads=x3res, writes=[R("junk"), R("ssq", slot2)])
                P.op("act", lambda e: e.activation(out=ssq[:, slot2:slot2 + 1], in_=ssq[:, slot2:slot2 + 1],
                                                   func=AF.Sqrt, bias=epsc[:, 0:1], scale=1.0 / D),
                     reads=[R("ssq", slot2), R("epsc")], writes=[R("ssq", slot2)])
                yield
                P.op("dve", lambda e: e.reciprocal(out=rstd[:, slot2:slot2 + 1], in_=ssq[:, slot2:slot2 + 1]),
                     reads=[R("ssq", slot2)], writes=[R("rstd", slot2)])
                stt("dve", x3t[:, b, :], x3t[:, b, :], rstd[:, slot2:slot2 + 1], gfin_b[:], ALU.mult, ALU.mult,
                    x3res + [R("rstd", slot2), R("gfin_b")], x3res)
                P.dma(lambda e: e.dma_start(out=out_d[tix * 128:(tix + 1) * 128, :], in_=x3t[:, b, :]),
                      reads=x3res, writes=[R("x1d", tix)])
                yield

            if dbg and half == 0:
                dbg_c = [dout("dbg_x2", [T, D]), dout("dbg_x3", [T, D])]
            NSTEP, STAG = 7, 4
            gens = [c_tile(j) for j in range(NTH)]
            for t_ in range(STAG * (NTH - 1) + NSTEP):
                for j in range(NTH):
                    if 0 <= t_ - STAG * j < NSTEP:
                        next(gens[j])
            P.barrier()
            st_c.close()
            cur.pop()
        st_b.close()
        cur.pop()
        P.finish()
    return nc


def host_prep(inp, core):
    g = np.zeros((128, 4, DC), np.float32)
    g[:, 0, :] = inp["g_mix"][0].reshape(DC, 128).T
    g[:, 1, :] = inp["g_ffn"][0].reshape(DC, 128).T
    g[:, 2, :] = inp["g_ple"][0].reshape(DC, 128).T
    bt = np.zeros((128, 2, 16, 128), np.float32)
    ct_ = np.zeros((128, 2, 16, 128), np.float32)
    af = np.zeros((3, 16, 128), np.float32)
    ap = np.zeros((128, 3, 16), np.float32)
    are, aim, ldt = inp["ssm_a_re"][0], inp["ssm_a_im"][0], inp["ssm_log_dt"][0]
    for ri, (B_, C_) in enumerate(((inp["ssm_b_re"][0], inp["ssm_c_re"][0]), (inp["ssm_b_im"][0], inp["ssm_c_im"][0]))):
        for g_ in range(32):
            gp_, g2 = g_ // 2, g_ % 2
            g8 = g_ % 8
            bt[g8 * 16:(g8 + 1) * 16, ri, gp_, g2 * 64:(g2 + 1) * 64] = B_[g_].T
            ct_[g2 * 64:(g2 + 1) * 64, ri, gp_, g8 * 16:(g8 + 1) * 16] = C_[g_].T
    for g_ in range(32):
        gp_, g2 = g_ // 2, g_ % 2
        af[0, gp_, g2 * 64:(g2 + 1) * 64] = are[g_]
        af[1, gp_, g2 * 64:(g2 + 1) * 64] = aim[g_]
        af[2, gp_, g2 * 64:(g2 + 1) * 64] = ldt[g_]
        ap[g2 * 64:(g2 + 1) * 64, 0, gp_] = are[g_]
        ap[g2 * 64:(g2 + 1) * 64, 1, gp_] = aim[g_]
        ap[g2 * 64:(g2 + 1) * 64, 2, gp_] = ldt[g_]
    dg = np.zeros((128, 8), np.float32)
    dg[:, 0:4] = inp["ssm_d"][0].reshape(4, 128).T
    dg[:, 4:8] = inp["b_glu"][0].reshape(4, 128).T
    q_loc = np.arange(128)[:, None]
    c_loc = np.arange(256)[None, :]
    rel = q_loc + 128 - c_loc
    valid = (rel >= 0) & (rel < 128)
    bkt = t5_bucket_np(np.maximum(rel, 0))
    rb = inp["rel_bias"]
    ab = np.full((128, NH, 256), NEG, np.float32)
    for h in range(NH):
        ab[:, h, :] = np.where(valid, rb[bkt, h], np.float32(NEG))
    m = {
        "w_router": np.ascontiguousarray(np.concatenate([inp["w_router_group"][0], inp["w_router_expert"][0]], axis=1)),
        "b_router": np.ascontiguousarray(np.concatenate([inp["b_router_group"][0], inp["b_router_expert"][0]])[None, :]),
        "w_e_gate": np.ascontiguousarray(inp["w_e_gate"][0]),
        "w_e_up": np.ascontiguousarray(inp["w_e_up"][0]),
        "w_e_down": np.ascontiguousarray(inp["w_e_down"][0]),
        "w_ple_gate": np.ascontiguousarray(inp["w_ple_gate"][0]),
        "b_ple_gate": np.ascontiguousarray(inp["b_ple_gate"][0][None, :]),
        "w_ple_proj": np.ascontiguousarray(inp["w_ple_proj"][0]),
        "g_final": np.ascontiguousarray(inp["g_final"][None, :]),
        "attn_bias": ab,
        "sinks_b": np.ascontiguousarray(np.broadcast_to(inp["sinks"][0][None, :], (128, NH))).astype(np.float32),
        "w_gate": np.ascontiguousarray(inp["w_gate"][0]),
        "b_gate_l": np.ascontiguousarray(inp["b_gate"][0].reshape(16, 128).T),
        "w_br_ssm": np.ascontiguousarray(inp["w_br_ssm"][0]),
        "w_br_attn": np.ascontiguousarray(inp["w_br_attn"][0]),
        "w_out": np.ascontiguousarray(inp["w_out"][0]),
        "ssm_bt": bt.reshape(128, 2, 2048), "ssm_ct": ct_.reshape(128, 2, 2048),
        "ssm_af": af.reshape(1, 3 * 2048), "ssm_ap": ap, "ssm_dg": dg,
        "w_glu": np.ascontiguousarray(inp["w_glu"][0]),
        "x": np.ascontiguousarray(inp["x"][core]),
        "p": np.ascontiguousarray(inp["p"][0, core]),
        "w_in": np.ascontiguousarray(inp["w_in"][0]),
        "gvec": g,
        "grow": np.ascontiguousarray(np.stack([inp["g_mix"][0], inp["g_ffn"][0], inp["g_ple"][0]], axis=0)).astype(np.float32),
    }
    return m


def kernel(**inputs):
    inp = {k: np.asarray(v) for k, v in inputs.items()}
    T = inp["x"].shape[1]
    nc = build(T)
    in_maps = [host_prep(inp, c) for c in range(8)]
    res = run_bass_kernel_spmd(nc, in_maps, core_ids=list(range(8)))
    return np.stack([r["out"] for r in res.results], axis=0).astype(np.float32)
```

```python
import math
import os
from contextlib import ExitStack
import numpy as np
import concourse.bass as bass
import concourse.mybir as mybir
from concourse.bass_utils import run_bass_kernel_spmd

F32 = mybir.dt.float32
BF16 = mybir.dt.bfloat16
I32 = mybir.dt.int32
AF = mybir.ActivationFunctionType
ALU = mybir.AluOpType
AX = mybir.AxisListType

D = 1024
DC = 8
D_SSM = 512
NG = 32
NST = 64
NH = 8
NKV = 2
HD = 64
DIN_L = 1408
NE = 32
DFF = 512
PLE = 256
EPS = 1e-6
NEG = -30000.0
TWO_PI = 2.0 * math.pi


class Res:
    __slots__ = ("w", "r")

    def __init__(self):
        self.w = None
        self.r = {}


class Prog:
    NDS = 24

    def __init__(self, nc, stack):
        self.nc = nc
        self.names = ["pe", "dve", "act", "pool", "sp"]
        self.q = {k: [] for k in self.names}
        self.cnt = {k: 0 for k in self.names}
        self.sem = {k: stack.enter_context(nc.semaphore("s_" + k)) for k in self.names}
        self.seen = {k: {} for k in self.names}
        for j in range(self.NDS):
            self.sem[("d", j)] = stack.enter_context(nc.semaphore("s_d%d" % j))
        self.dcnt = [0] * self.NDS
        self.rr = 0
        self.res = {}

    def R(self, *key):
        r = self.res.get(key)
        if r is None:
            r = self.res[key] = Res()
        return r

    def _deps(self, eng, reads, writes):
        deps = {}

        def add(t):
            if t is None:
                return
            k, v = t
            if deps.get(k, 0) < v:
                deps[k] = v

        for r in reads:
            add(r.w)
        for w in writes:
            add(w.w)
            for k, v in w.r.items():
                add((k, v))
        return deps

    def _wait(self, eng, deps, skip_same=False):
        seen = self.seen[eng]
        for k, v in deps.items():
            if skip_same and k == eng:
                continue
            if seen.get(k, 0) < v:
                seen[k] = v
                sem = self.sem[k]
                self.q[eng].append(lambda e, sem=sem, v=v: e.wait_ge(sem, v))

    def _mark(self, ticket, reads, writes):
        k, v = ticket
        for r in reads:
            if r.r.get(k, 0) < v:
                r.r[k] = v
        for w in writes:
            w.w = ticket
            w.r = {}

    def op(self, eng, fn, reads=(), writes=()):
        deps = self._deps(eng, reads, writes)
        self._wait(eng, deps, skip_same=(eng == "pe"))
        self.cnt[eng] += 1
        sem = self.sem[eng]
        self.q[eng].append(lambda e, fn=fn, sem=sem: fn(e).then_inc(sem, 1))
        self._mark((eng, self.cnt[eng]), reads, writes)

    def dma(self, fn, reads=(), writes=(), eng="sp"):
        deps = self._deps(eng, reads, writes)
        j = self.rr
        self.rr = (self.rr + 1) % self.NDS
        if self.dcnt[j]:
            deps[("d", j)] = max(deps.get(("d", j), 0), self.dcnt[j])
        self._wait(eng, deps)
        self.dcnt[j] += 16
        sem = self.sem[("d", j)]
        self.q[eng].append(lambda e, fn=fn, sem=sem: fn(e).then_inc(sem, 16))
        self._mark((("d", j), self.dcnt[j]), reads, writes)

    def finish(self, eng="sp"):
        deps = {("d", j): self.dcnt[j] for j in range(self.NDS) if self.dcnt[j]}
        for k in self.names:
            if self.cnt[k]:
                deps[k] = self.cnt[k]
        self._wait(eng, deps)
        self.flush()

    def flush(self):
        q = self.q
        self.q = {k: [] for k in self.names}
        with self.nc.Block() as block:
            @block.tensor
            def _(e):
                for f in q["pe"]:
                    f(e)

            @block.vector
            def _(e):
                for f in q["dve"]:
                    f(e)

            @block.scalar
            def _(e):
                for f in q["act"]:
                    f(e)

            @block.gpsimd
            def _(e):
                for f in q["pool"]:
                    f(e)

            @block.sync
            def _(e):
                for f in q["sp"]:
                    f(e)

    def barrier(self):
        deps = {("d", j): self.dcnt[j] for j in range(self.NDS) if self.dcnt[j]}
        for k in self.names:
            if self.cnt[k]:
                deps[k] = self.cnt[k]
        for eng in self.names:
            self._wait(eng, dict(deps))
        self.flush()


def t5_bucket_np(rel):
    max_exact = 16
    relf = np.maximum(rel, 1).astype(np.float32)
    large = max_exact + (np.log(relf / np.float32(max_exact)) / np.float32(math.log(128 / max_exact))
                         * np.float32(32 - max_exact)).astype(np.int32)
    large = np.minimum(large, 31)
    return np.where(rel < max_exact, rel, large)


def build(T=4096, n_exp=NE, dbg=False, stop=None):
    nc = bass.Bass("TRN2", target_bir_lowering=False)
    NT = T // 128
    TS = min(512, T)
    NS = T // TS
    TPS = TS // 128

    def din(name, shape, dt=F32):
        return nc.dram_tensor(name, list(shape), dt, kind="ExternalInput").ap()

    x_d = din("x", [T, D])
    p_d = din("p", [T, PLE])
    w_in_d = din("w_in", [D, 1280])
    gvec_d = din("gvec", [128, 4, DC])
    grow_d = din("grow", [3, D])
    ssm_bt_d = din("ssm_bt", [128, 2, 2048])
    ssm_af_d = din("ssm_af", [1, 3 * 2048])
    ssm_ap_d = din("ssm_ap", [128, 3, 16])
    ssm_ct_d = din("ssm_ct", [128, 2, 2048])
    ssm_dg_d = din("ssm_dg", [128, 8])
    w_glu_d = din("w_glu", [512, 512])
    abias_d = din("attn_bias", [128, NH, 256])
    sinkb_d = din("sinks_b", [128, NH])
    w_gate_d = din("w_gate", [D, 2 * D])
    bgate_d = din("b_gate_l", [128, 16])
    w_bra_d = din("w_br_ssm", [512, D])
    w_brb_d = din("w_br_attn", [512, D])
    w_out_d = din("w_out", [D, D])
    w_rt_d = din("w_router", [D, 36])
    b_rt_d = din("b_router", [1, 36])
    w_eg_d = din("w_e_gate", [NE, D, DFF])
    w_eu_d = din("w_e_up", [NE, D, DFF])
    w_ed_d = din("w_e_down", [NE, DFF, D])
    w_pg_d = din("w_ple_gate", [D, D])
    b_pg_d = din("b_ple_gate", [1, D])
    w_pp_d = din("w_ple_proj", [PLE, D])
    g_fin_d = din("g_final", [1, D])
    out_d = nc.dram_tensor("out", [T, D], F32, kind="ExternalOutput").ap()
    x1_d = out_d
    dbg_d = {}

    def dout(name, shape):
        dbg_d[name] = nc.dram_tensor(name, list(shape), F32, kind="ExternalOutput").ap()
        return dbg_d[name]

    with ExitStack() as st:
        P = Prog(nc, st)
        R = P.R

        cur = [st]

        def sb(name, shape, dt=F32, stack=None):
            stack = stack or cur[-1]
            return stack.enter_context(nc.sbuf_tensor(name, list(shape), dt))

        PS = [st.enter_context(nc.psum_tensor("ps%d" % i, [128, 512], F32)) for i in range(8)]

        ident = sb("ident", [128, 128])
        P.op("pool", lambda e: e.memset(ident[:], 0.0), writes=[R("ident")])
        P.op("pool", lambda e: e.affine_select(out=ident[:], in_=ident[:], pattern=[[-1, 128]],
                                               compare_op=ALU.not_equal, fill=1.0, base=0,
                                               channel_multiplier=1),
             reads=[R("ident")], writes=[R("ident")])
        gvec = sb("gvec_sb", [128, 4, DC])
        P.dma(lambda e: e.dma_start(out=gvec[:], in_=gvec_d[:, :, :]), writes=[R("gvec")])
        epsc = sb("epsc", [128, 1])
        gb = sb("gb", [128, D])

        def load_gb(k_):
            P.dma(lambda e: e.dma_start(out=gb[:], in_=grow_d[k_:k_ + 1, :].partition_broadcast(128)), writes=[R("gb")])
        P.op("pool", lambda e: e.memset(epsc[:], EPS), writes=[R("epsc")])

        def load_w_bf16(dst, w_dram, K, N, gslot, pieces, stage, stage_key, tag):
            for c in range(K // 128):
                sidx = c % 2
                sres = R(stage_key, sidx)
                P.dma(lambda e, c=c, sidx=sidx: e.dma_start(out=stage[:, sidx, 0:N],
                                                            in_=w_dram[c * 128:(c + 1) * 128, :]),
                      writes=[sres])
                for (s0, n, d0) in pieces:
                    if c % 2 == 0:
                        P.op("act", lambda e, c=c, sidx=sidx, s0=s0, n=n, d0=d0:
                             e.copy(out=dst[:, c, d0:d0 + n], in_=stage[:, sidx, s0:s0 + n]),
                             reads=[sres], writes=[R(tag, c)])
                    else:
                        P.op("dve", lambda e, c=c, sidx=sidx, s0=s0, n=n, d0=d0:
                             e.tensor_copy(out=dst[:, c, d0:d0 + n], in_=stage[:, sidx, s0:s0 + n]),
                             reads=[sres], writes=[R(tag, c)])

        xn = sb("xn", [128, 2, D])
        ssq = sb("ssq", [128, 4])
        rstd = sb("rstd", [128, 4])
        junk = sb("junk", [128, D], BF16)

        def vtt(eng, out, a, b, op, reads, writes):
            P.op(eng, lambda e: e.tensor_tensor(out=out, in0=a, in1=b, op=op), reads=reads, writes=writes)

        def ts(eng, out, a, s1, op0, reads, writes, s2=None, op1=None):
            if op1 is None:
                P.op(eng, lambda e: e.tensor_scalar(out=out, in0=a, scalar1=s1, scalar2=None, op0=op0),
                     reads=reads, writes=writes)
            else:
                P.op(eng, lambda e: e.tensor_scalar(out=out, in0=a, scalar1=s1, scalar2=s2, op0=op0, op1=op1),
                     reads=reads, writes=writes)

        def stt(eng, out, a, sc, b, op0, op1, reads, writes):
            P.op(eng, lambda e: e.scalar_tensor_tensor(out=out, in0=a, scalar=sc, in1=b, op0=op0, op1=op1),
                 reads=reads, writes=writes)

        def act(out, in_, func, reads, writes, bias=None, scale=None, accum=None):
            kw = {}
            if bias is not None:
                kw["bias"] = bias
            if scale is not None:
                kw["scale"] = scale
            if accum is not None:
                kw["accum_out"] = accum
            P.op("act", lambda e: e.activation(out=out, in_=in_, func=func, **kw), reads=reads, writes=writes)

        cst = sb("cst", [128, 4])
        for i_, v_ in enumerate((math.pi / 2, 0.0, 1.0, -1.0)):
            P.op("pool", lambda e, i_=i_, v_=v_: e.memset(cst[:, i_:i_ + 1], v_), writes=[R("cst")])

        def sincos(ph, sn, cs, tf, ti, rin, rsn, rcs, rtf, rti):
            ts("dve", tf, ph, 1.0 / TWO_PI, ALU.mult, [rin], [rtf])
            P.op("dve", lambda e: e.tensor_copy(out=ti, in_=tf), reads=[rtf], writes=[rti])
            P.op("dve", lambda e: e.tensor_copy(out=tf, in_=ti), reads=[rti], writes=[rtf])
            stt("dve", ph, tf, -TWO_PI, ph, ALU.mult, ALU.add, [rtf, rin], [rin])
            ts("dve", tf, ph, math.pi, ALU.is_gt, [rin], [rtf])
            stt("dve", ph, tf, -TWO_PI, ph, ALU.mult, ALU.add, [rtf, rin], [rin])
            ts("dve", tf, ph, -math.pi, ALU.is_lt, [rin], [rtf])
            stt("dve", ph, tf, TWO_PI, ph, ALU.mult, ALU.add, [rtf, rin], [rin])
            ts("dve", ph, ph, -math.pi, ALU.max, [rin], [rin], s2=math.pi, op1=ALU.min)
            act(sn, ph, AF.Sin, [rin], [rsn])
            stt("dve", tf, ph, -1.0, ph, ALU.mult, ALU.max, [rin], [rtf])
            np_ = tf.shape[0]
            act(cs, tf, AF.Sin, [rtf, R("cst")], [rcs], bias=cst[0:np_, 0:1], scale=-1.0)

        st_mix = ExitStack()
        cur.append(st_mix)
        uT = sb("uT", [128, 4, T], BF16)
        qT = sb("qT", [128, 4, T], BF16)
        st_kv = ExitStack()
        cur.append(st_kv)
        kT = sb("kT", [128, 2, T], BF16)
        vS = sb("vS", [128, NT, 128], BF16)
        st_sw = ExitStack()
        cur.append(st_sw)
        xt = None
        hT = None

        def early_exit():
            zt = sb("zt", [128, D])
            P.op("pool", lambda e: e.memset(zt[:], 0.0), writes=[R("zt")])
            for tt_ in range(NT):
                P.dma(lambda e, tt_=tt_: e.dma_start(out=out_d[tt_ * 128:(tt_ + 1) * 128, :], in_=zt[:]),
                      reads=[R("zt")])
            P.finish()
            while len(cur) > 1:
                cur.pop().close()
            return nc

        if stop == 0:
            return early_exit()
        bbar = sb("bbar", [128, 2, 2048], BF16)
        ctb = sb("ctb", [128, 2, 2048], BF16)
        ssm_p = sb("ssm_p", [128, 3, 16])
        ssm_dg = sb("ssm_dg_sb", [128, 8])
        wglu_bf = sb("wglu_bf", [128, 4, 512], BF16)
        P.dma(lambda e: e.dma_start(out=ssm_dg[:], in_=ssm_dg_d[:, :]), writes=[R("ssm_dg")])
        st0 = ExitStack()
        cur.append(st0)
        AFt = sb("AFt", [128, 3, 1024])
        BTt = sb("BTt", [128, 2, 1024])
        tmps = [sb("s0t%d" % i, [128, 1024]) for i in range(8)]
        tmi = sb("s0ti", [128, 1024], I32)
        for hh in range(2):
          hs = slice(hh * 1024, (hh + 1) * 1024)
          for a_ in range(3):
            P.dma(lambda e, a_=a_, hh=hh: e.dma_start(
                out=AFt[:, a_, :],
                in_=ssm_af_d[0:1, a_ * 2048 + hh * 1024:a_ * 2048 + (hh + 1) * 1024].partition_broadcast(128)),
                writes=[R("AFt")])
          P.dma(lambda e, hs=hs: e.dma_start(out=BTt[:], in_=ssm_bt_d[:, :, hs]), writes=[R("BTt")])
          LR, LI, LD = AFt[:, 0, :], AFt[:, 1, :], AFt[:, 2, :]
          rA, rB = R("AFt"), R("BTt")
          rt = [R("s0t", i) for i in range(8)]
          T0, T1, T2, T3, T4, T5, T6, T7 = [t[:] for t in tmps]
          act(LD, LD, AF.Exp, [rA], [rA])
          vtt("dve", T0, LR, LD, ALU.mult, [rA], [rt[0]])
          act(T0, T0, AF.Exp, [rt[0]], [rt[0]])
          vtt("dve", T1, LI, LD, ALU.mult, [rA], [rt[1]])
          sincos(T1, T2, T3, T4, tmi[:], rt[1], rt[2], rt[3], rt[4], R("s0ti"))
          vtt("dve", T3, T0, T3, ALU.mult, [rt[0], rt[3]], [rt[3]])
          ts("dve", T3, T3, -1.0, ALU.add, [rt[3]], [rt[3]])
          vtt("dve", T2, T0, T2, ALU.mult, [rt[0], rt[2]], [rt[2]])
          vtt("dve", T0, LR, LR, ALU.mult, [rA], [rt[0]])
          vtt("dve", T1, LI, LI, ALU.mult, [rA], [rt[1]])
          vtt("dve", T0, T0, T1, ALU.add, [rt[0], rt[1]], [rt[0]])
          P.op("dve", lambda e: e.reciprocal(out=T0, in_=T0), reads=[rt[0]], writes=[rt[0]])
          vtt("dve", T1, T3, LR, ALU.mult, [rt[3], rA], [rt[1]])
          vtt("dve", T4, T2, LI, ALU.mult, [rt[2], rA], [rt[4]])
          vtt("dve", T1, T1, T4, ALU.add, [rt[1], rt[4]], [rt[1]])
          vtt("dve", T1, T1, T0, ALU.mult, [rt[1], rt[0]], [rt[1]])
          vtt("dve", T4, T2, LR, ALU.mult, [rt[2], rA], [rt[4]])
          vtt("dve", T5, T3, LI, ALU.mult, [rt[3], rA], [rt[5]])
          vtt("dve", T4, T4, T5, ALU.subtract, [rt[4], rt[5]], [rt[4]])
          vtt("dve", T4, T4, T0, ALU.mult, [rt[4], rt[0]], [rt[4]])
          Bre, Bim = BTt[:, 0, :], BTt[:, 1, :]
          vtt("dve", T5, T1, Bre, ALU.mult, [rt[1], rB], [rt[5]])
          vtt("dve", T6, T4, Bim, ALU.mult, [rt[4], rB], [rt[6]])
          vtt("dve", bbar[:, 0, hs], T5, T6, ALU.subtract, [rt[5], rt[6]], [R("bbar")])
          vtt("dve", T5, T1, Bim, ALU.mult, [rt[1], rB], [rt[5]])
          vtt("dve", T6, T4, Bre, ALU.mult, [rt[4], rB], [rt[6]])
          vtt("dve", bbar[:, 1, hs], T5, T6, ALU.add, [rt[5], rt[6]], [R("bbar")])
        for hh in range(2):
            hs = slice(hh * 1024, (hh + 1) * 1024)
            P.dma(lambda e, hs=hs: e.dma_start(out=BTt[:], in_=ssm_ct_d[:, :, hs]), reads=[], writes=[rB])
            P.op("pool", lambda e, hs=hs: e.tensor_copy(out=ctb[:, 0, hs], in_=BTt[:, 0, :]), reads=[rB], writes=[R("ctb")])
            ts("pool", ctb[:, 1, hs], BTt[:, 1, :], -1.0, ALU.mult, [rB], [R("ctb")])
        apt = sb("apt", [128, 3, 16])
        P.dma(lambda e: e.dma_start(out=apt[:], in_=ssm_ap_d[:, :, :]), writes=[R("apt")])
        act(apt[:, 2, :], apt[:, 2, :], AF.Exp, [R("apt")], [R("apt")])
        vtt("dve", ssm_p[:, 0, :], apt[:, 0, :], apt[:, 2, :], ALU.mult, [R("apt")], [R("ssm_p")])
        act(ssm_p[:, 0, :], ssm_p[:, 0, :], AF.Exp, [R("ssm_p")], [R("ssm_p")])
        vtt("dve", ssm_p[:, 1, :], apt[:, 1, :], apt[:, 2, :], ALU.mult, [R("apt")], [R("ssm_p")])
        wst0 = sb("wst0", [128, 2, 512])
        load_w_bf16(wglu_bf, w_glu_d, 512, 512, None, [(0, 512, 0)], wst0, "wst0", "wglu_bf")
        P.barrier()
        st0.close()
        cur.pop()

        def rms_rstd(src_ap, src_res, slot):
            P.op("act", lambda e: e.activation(out=junk[:], in_=src_ap, func=AF.Square,
                                               accum_out=ssq[:, slot:slot + 1]),
                 reads=[src_res], writes=[R("junk"), R("ssq", slot)])
            P.op("act", lambda e: e.activation(out=ssq[:, slot:slot + 1], in_=ssq[:, slot:slot + 1],
                                               func=AF.Sqrt, bias=epsc[:, 0:1], scale=1.0 / D),
                 reads=[R("ssq", slot), R("epsc")], writes=[R("ssq", slot)])
            P.op("dve", lambda e: e.reciprocal(out=rstd[:, slot:slot + 1], in_=ssq[:, slot:slot + 1]),
                 reads=[R("ssq", slot)], writes=[R("rstd", slot)])

        def emit_hT(s):
            for j in range(TPS):
                tok0 = s * TS + j * 128
                P.dma(lambda e, j=j, tok0=tok0: e.dma_start(out=xt[:, j, :], in_=x_d[tok0:tok0 + 128, :]),
                      writes=[R("xt", j)])
                slot = j % 4
                rms_rstd(xt[:, j, :], R("xt", j), slot)
                b = j % 2
                stt("dve", xn[:, b, :], xt[:, j, :], rstd[:, slot:slot + 1], gb[:], ALU.mult, ALU.mult,
                    [R("xt", j), R("rstd", slot), R("gb")], [R("xn", b)])
                for hf in range(2):
                    bank = hf
                    for cc in range(4):
                        c = hf * 4 + cc
                        P.op("pe", lambda e, b=b, c=c, cc=cc, bank=bank: e.transpose(
                            out=PS[bank][:, cc * 128:(cc + 1) * 128], in_=xn[:, b, c * 128:(c + 1) * 128],
                            identity=ident[:]), reads=[R("xn", b), R("ident")], writes=[R("ps", bank)])
                    eng = "dve" if hf == 0 else "act"
                    outap = hT[:, hf * 4:hf * 4 + 4, j * 128:(j + 1) * 128]
                    inap = PS[bank][:, :].rearrange("p (a b) -> p a b", a=4)
                    if eng == "dve":
                        P.op("dve", lambda e, outap=outap, inap=inap: e.tensor_copy(out=outap, in_=inap),
                             reads=[R("ps", bank)], writes=[R("hT", j)])
                    else:
                        P.op("act", lambda e, outap=outap, inap=inap: e.copy(out=outap, in_=inap),
                             reads=[R("ps", bank)], writes=[R("hT", j)])

        if stop == 1:
            return early_exit()
        st_a = ExitStack()
        cur.append(st_a)
        win_bf = sb("win_bf", [128, DC, DIN_L], BF16)
        wstage = sb("wstage", [128, 2, 2048], F32)
        xt = sb("xt", [128, TPS, D], F32)
        hT = sb("hT", [128, DC, TS], BF16)
        load_w_bf16(win_bf, w_in_d, D, 1280, 0,
                    [(0, 1024, 0), (1024, 64, 1024), (1024, 64, 1088), (1088, 64, 1152), (1088, 64, 1216),
                     (1152, 128, 1280)], wstage, "wstage", "win_bf")
        win_res = [R("win_bf", c) for c in range(DC)]

        load_gb(0)
        for s in range(NS):
            emit_hT(s)
            hres = [R("hT", j) for j in range(TPS)]
            cols = slice(s * TS, (s + 1) * TS)
            for o in range(10):
                bank = 2 + (o % 4)
                for c in range(DC):
                    P.op("pe", lambda e, o=o, c=c, bank=bank: e.matmul(
                        PS[bank][:, 0:TS], win_bf[:, c, o * 128:(o + 1) * 128], hT[:, c, :],
                        start=(c == 0), stop=(c == DC - 1)),
                        reads=hres + win_res, writes=[R("ps", bank)])
                if o < 4:
                    dst, key = uT[:, o, cols], ("uT", o, s)
                elif o < 8:
                    dst, key = qT[:, o - 4, cols], None
                else:
                    dst, key = kT[:, o - 8, cols], ("kT", o - 8, s)
                wr = [R(*key)] if key is not None else [R("qT", o - 4, s * TPS + jj) for jj in range(TPS)]
                if o % 2 == 0:
                    P.op("dve", lambda e, dst=dst, bank=bank: e.tensor_copy(out=dst, in_=PS[bank][:, 0:TS]),
                         reads=[R("ps", bank)], writes=wr)
                else:
                    P.op("act", lambda e, dst=dst, bank=bank: e.copy(out=dst, in_=PS[bank][:, 0:TS]),
                         reads=[R("ps", bank)], writes=wr)
            for j in range(TPS):
                bank = 6 + (j % 2)
                for c in range(DC):
                    P.op("pe", lambda e, j=j, c=c, bank=bank: e.matmul(
                        PS[bank][:, 0:128], hT[:, c, j * 128:(j + 1) * 128], win_bf[:, c, 1280:1408],
                        start=(c == 0), stop=(c == DC - 1)),
                        reads=hres + win_res, writes=[R("ps", bank)])
                tt = s * TPS + j
                P.op("dve", lambda e, tt=tt, bank=bank: e.tensor_copy(out=vS[:, tt, :], in_=PS[bank][:, 0:128]),
                     reads=[R("ps", bank)], writes=[R("vS", tt)])
        P.barrier()
        st_a.close()
        cur.pop()

        if dbg:
            for nm, src, nt_ in (("uT", uT, 4), ("qT", qT, 4), ("kT", kT, 2)):
                dd = dout("dbg_" + nm, [nt_ * 128, T])
                for o in range(nt_):
                    tmp = sb("dbgt_%s%d" % (nm, o), [128, T])
                    P.op("dve", lambda e, tmp=tmp, src=src, o=o: e.tensor_copy(out=tmp[:], in_=src[:, o, :]),
                         reads=([R(nm, o, s) for s in range(NS)] if nm != "qT" else [R("qT", o, b) for b in range(NT)]),
                         writes=[R("dbgt", nm, o)])
                    P.dma(lambda e, dd=dd, tmp=tmp, o=o: e.dma_start(out=dd[o * 128:(o + 1) * 128, :], in_=tmp[:]),
                          reads=[R("dbgt", nm, o)])
            dd = dout("dbg_v", [T, 128])
            for tt in range(NT):
                tmp = sb("dbgt_v%d" % tt, [128, 128])
                P.op("dve", lambda e, tmp=tmp, tt=tt: e.tensor_copy(out=tmp[:], in_=vS[:, tt, :]),
                     reads=[R("vS", tt)], writes=[R("dbgt", "v", tt)])
                P.dma(lambda e, dd=dd, tmp=tmp, tt=tt: e.dma_start(out=dd[tt * 128:(tt + 1) * 128, :], in_=tmp[:]),
                      reads=[R("dbgt", "v", tt)])


        if stop == 2:
            return early_exit()
        Tc = TS
        st_s = ExitStack()
        cur.append(st_s)
        io_i = sb("io_i", [128, Tc + 1], I32)
        io_f = sb("io_f", [128, Tc + 1])
        P.op("pool", lambda e: e.iota(io_i[:], pattern=[[1, Tc + 1]], base=0, channel_multiplier=0),
             writes=[R("io_i")])
        P.op("dve", lambda e: e.tensor_copy(out=io_f[:], in_=io_i[:]), reads=[R("io_i")], writes=[R("io_f")])
        cosT = sb("cosT", [128, 4, Tc + 1])
        sinT = sb("sinT", [128, 4, Tc + 1])
        rT = sb("rT", [128, 4, Tc])
        sc_f = sb("sc_f", [128, Tc + 1])
        sc_i = sb("sc_i", [128, Tc + 1], I32)
        sc_p = sb("sc_p", [128, Tc + 1])
        tA = sb("tA", [128, 2, 4, Tc])
        tE = sb("tE", [128, 2, 2, Tc])
        sRI = sb("sRI", [128, 2, 2, Tc], BF16)
        winit = sb("winit", [128, 16, 2])
        gl_t = sb("gl_t", [128, 2, Tc])
        st_t = ExitStack()
        cur.append(st_t)
        abias = sb("abias", [128, NH, 256])
        sinkb = sb("sinkb", [128, NH])
        P.dma(lambda e: e.dma_start(out=abias[:], in_=abias_d[:, :, :]), writes=[R("abias")])
        P.dma(lambda e: e.dma_start(out=sinkb[:], in_=sinkb_d[:, :]), writes=[R("sinkb")])
        sS = sb("sS", [128, 2, 256])
        eS = sb("eS", [128, 2, 256])
        pT = sb("pT", [128, 2, 2, 128], BF16)
        sm = sb("sm", [128, 2, 4])
        rden = sb("rden", [128, 2, NH])
        osb = sb("osb", [128, 2, 512])
        def ssm_gen():
            units = [(ct, s_, m) for ct in range(4) for s_ in range(NS) for m in range(4)]
            ybank = 2

            def emit_bu(k):
                ct, s_, m = units[k]
                gp = ct * 4 + m
                par = m % 2
                gsl = slice(gp * 128, (gp + 1) * 128)
                cols = slice(s_ * Tc, (s_ + 1) * Tc)
                ures = R("uT", ct, s_)
                P.op("pe", lambda e: e.matmul(PS[0][:, 0:Tc], bbar[:, 0, gsl], uT[:, ct, cols],
                                              start=True, stop=True),
                     reads=[R("bbar"), ures], writes=[R("ps", 0)])
                P.op("pe", lambda e: e.matmul(PS[1][:, 0:Tc], bbar[:, 1, gsl], uT[:, ct, cols],
                                              start=True, stop=True),
                     reads=[R("bbar"), ures], writes=[R("ps", 1)])

            emit_bu(0)
            for k, (ct, s_, m) in enumerate(units):
                if s_ == 0 and m == 0:
                    for m2 in range(4):
                        gp2 = ct * 4 + m2
                        ts("dve", sc_p[:], io_f[:], ssm_p[:, 1, gp2:gp2 + 1], ALU.mult,
                           [R("io_f"), R("ssm_p")], [R("sc_p")])
                        sincos(sc_p[:], sinT[:, m2, :], cosT[:, m2, :], sc_f[:], sc_i[:],
                               R("sc_p"), R("sinT", m2), R("cosT", m2), R("sc_f"), R("sc_i"))
                        ts("dve", rT[:, m2, :], io_f[:, 0:Tc], 0.0, ALU.mult, [R("io_f"), R("ssm_p")], [R("rT", m2)],
                           s2=ssm_p[:, 0, gp2:gp2 + 1], op1=ALU.add)
                cols = slice(s_ * Tc, (s_ + 1) * Tc)
                gp = ct * 4 + m
                par = m % 2
                gsl = slice(gp * 128, (gp + 1) * 128)
                b_re, b_im = PS[0], PS[1]
                rbr, rbi = R("ps", 0), R("ps", 1)
                A_, B_, C_, D_ = [tA[:, par, i, :] for i in range(4)]
                rA_, rB_, rC_, rD_ = [R("tA", par, i) for i in range(4)]
                cs_, sn_ = cosT[:, m, 0:Tc], sinT[:, m, 0:Tc]
                rcs, rsn = R("cosT", m), R("sinT", m)
                vtt("dve", A_, cs_, b_re[:, 0:Tc], ALU.mult, [rcs, rbr], [rA_])
                vtt("dve", B_, sn_, b_im[:, 0:Tc], ALU.mult, [rsn, rbi], [rB_])
                vtt("dve", A_, A_, B_, ALU.add, [rA_, rB_], [rA_])
                vtt("dve", C_, cs_, b_im[:, 0:Tc], ALU.mult, [rcs, rbi], [rC_])
                vtt("dve", D_, sn_, b_re[:, 0:Tc], ALU.mult, [rsn, rbr], [rD_])
                vtt("dve", C_, C_, D_, ALU.subtract, [rC_, rD_], [rC_])
                if k + 1 < len(units):
                    emit_bu(k + 1)
                if s_ == 0:
                    i_re, i_im = 0.0, 0.0
                else:
                    i_re, i_im = winit[:, gp, 0:1], winit[:, gp, 1:2]
                P.op("dve", lambda e, B_=B_, A_=A_, m=m, i_re=i_re: e.tensor_tensor_scan(
                    out=B_, data0=rT[:, m, :], data1=A_, initial=i_re, op0=ALU.mult, op1=ALU.add),
                    reads=[R("rT", m), rA_, R("winit", gp)], writes=[rB_])
                P.op("dve", lambda e, D_=D_, C_=C_, m=m, i_im=i_im: e.tensor_tensor_scan(
                    out=D_, data0=rT[:, m, :], data1=C_, initial=i_im, op0=ALU.mult, op1=ALU.add),
                    reads=[R("rT", m), rC_, R("winit", gp)], writes=[rD_])
                yield
                if s_ < NS - 1:
                    cT_, sT_ = cosT[:, m, Tc:Tc + 1], sinT[:, m, Tc:Tc + 1]
                    wl_re, wl_im = B_[:, Tc - 1:Tc], D_[:, Tc - 1:Tc]
                    ts("dve", winit[:, gp, 0:1], wl_re, cT_, ALU.mult, [rB_, rcs], [R("winit", gp)])
                    vtt("dve", sc_f[:, 0:1], wl_im, sT_, ALU.mult, [rD_, rsn], [R("sc_f")])
                    vtt("dve", winit[:, gp, 0:1], winit[:, gp, 0:1], sc_f[:, 0:1], ALU.subtract,
                        [R("sc_f"), R("winit", gp)], [R("winit", gp)])
                    ts("dve", winit[:, gp, 1:2], wl_re, sT_, ALU.mult, [rB_, rsn], [R("winit", gp)])
                    vtt("dve", sc_f[:, 0:1], wl_im, cT_, ALU.mult, [rD_, rcs], [R("sc_f")])
                    vtt("dve", winit[:, gp, 1:2], winit[:, gp, 1:2], sc_f[:, 0:1], ALU.add,
                        [R("sc_f"), R("winit", gp)], [R("winit", gp)])
                E_, F_ = tE[:, par, 0, :], tE[:, par, 1, :]
                rE_, rF_ = R("tE", par, 0), R("tE", par, 1)
                SR_, SI_ = sRI[:, par, 0, :], sRI[:, par, 1, :]
                rSR, rSI = R("sRI", par, 0), R("sRI", par, 1)
                E2_, F2_, rE2_, rF2_ = A_, C_, rA_, rC_
                vtt("dve", E_, cs_, B_, ALU.mult, [rcs, rB_], [rE_])
                vtt("pool", F_, sn_, D_, ALU.mult, [rsn, rD_], [rF_])
                vtt("pool", SR_, E_, F_, ALU.subtract, [rE_, rF_], [rSR])
                vtt("pool", E2_, sn_, B_, ALU.mult, [rsn, rB_], [rE2_])
                vtt("pool", F2_, cs_, D_, ALU.mult, [rcs, rD_], [rF2_])
                vtt("pool", SI_, E2_, F2_, ALU.add, [rE2_, rF2_], [rSI])
                P.op("pe", lambda e, gsl=gsl, SR_=SR_, m=m: e.matmul(PS[ybank][:, 0:Tc], ctb[:, 0, gsl], SR_, start=(m == 0), stop=False),
                     reads=[R("ctb"), rSR], writes=[R("ps", ybank)])
                P.op("pe", lambda e, gsl=gsl, SI_=SI_, m=m: e.matmul(PS[ybank][:, 0:Tc], ctb[:, 1, gsl], SI_, start=False, stop=(m == 3)),
                     reads=[R("ctb"), rSI], writes=[R("ps", ybank)])
                if m == 3:
                    un = k // 4
                    gt = gl_t[:, un % 2, :]
                    rg = R("gl_t", un % 2)
                    stt("dve", gt, uT[:, ct, cols], ssm_dg[:, ct:ct + 1], PS[ybank][:, 0:Tc], ALU.mult, ALU.add,
                        [R("uT", ct, s_), R("ssm_dg"), R("ps", ybank)], [rg])
                    act(uT[:, ct, cols], gt, AF.Gelu, [rg], [R("uT", ct, s_)])
                yield

        def att_gen():
            steps = [(qb, h) for qb in range(NT) for h in range(NH)]
            OB_ = 7

            def geom(qb):
                c0 = 0 if qb > 0 else 128
                kc0 = (qb - 1) * 128 if qb > 0 else 0
                kcols = slice(kc0, (qb + 1) * 128)
                s_cur = qb // TPS
                kres = [s_cur] if (qb == 0 or (qb - 1) // TPS == s_cur) else [s_cur - 1, s_cur]
                kbs = [0, 1] if qb > 0 else [1]
                return c0, kcols, kres, kbs

            def emit_S(i):
                qb, h = steps[i]
                c0, kcols, kres, kbs = geom(qb)
                kv, qt_, pb, par = h // 4, h // 2, 64 * (h % 2), h % 2
                so, SB_ = 0, 3 + par
                P.op("pe", lambda e: e.matmul(
                    PS[SB_][:, so + c0:so + 256], qT[pb:pb + 64, qt_, qb * 128:(qb + 1) * 128],
                    kT[pb:pb + 64, kv, kcols], start=True, stop=True),
                    reads=[R("qT", qt_, qb)] + [R("kT", kv, s_) for s_ in kres], writes=[R("ps", SB_)])

            def emit_st2(i):
                qb, h = steps[i]
                c0, kcols, kres, kbs = geom(qb)
                par = h % 2
                so, SB_ = 0, 3 + par
                rs, re_, rsm = R("sS", par), R("eS", par), R("sm", par)
                stt("dve", sS[:, par, c0:256], PS[SB_][:, so + c0:so + 256], HD ** -0.5, abias[:, h, c0:256],
                    ALU.mult, ALU.add, [R("ps", SB_), R("abias")], [rs])
                P.op("dve", lambda e: e.reduce_max(out=sm[:, par, 0:1], in_=sS[:, par, c0:256], axis=AX.X),
                     reads=[rs], writes=[rsm])
                vtt("dve", sm[:, par, 0:1], sm[:, par, 0:1], sinkb[:, h:h + 1], ALU.max, [rsm, R("sinkb")], [rsm])
                ts("dve", sm[:, par, 1:2], sm[:, par, 0:1], -1.0, ALU.mult, [rsm], [rsm])
                act(eS[:, par, c0:256], sS[:, par, c0:256], AF.Exp, [rs, rsm], [re_, rsm],
                    bias=sm[:, par, 1:2], accum=sm[:, par, 2:3])
                act(sm[:, par, 3:4], sinkb[:, h:h + 1], AF.Exp, [R("sinkb"), rsm], [rsm], bias=sm[:, par, 1:2])

            def emit_st3(i):
                qb, h = steps[i]
                c0, kcols, kres, kbs = geom(qb)
                par, qpar = h % 2, qb % 2
                to, TB_ = 0, 5 + par
                re_, rsm, rp = R("eS", par), R("sm", par), R("pT", par)
                vtt("dve", sm[:, par, 2:3], sm[:, par, 2:3], sm[:, par, 3:4], ALU.add, [rsm], [rsm])
                P.op("dve", lambda e: e.reciprocal(out=rden[:, qpar, h:h + 1], in_=sm[:, par, 2:3]),
                     reads=[rsm], writes=[R("rden", qpar)])
                for kb in kbs:
                    P.op("pe", lambda e, kb=kb: e.transpose(
                        out=PS[TB_][:, to + kb * 128:to + (kb + 1) * 128], in_=eS[:, par, kb * 128:(kb + 1) * 128],
                        identity=ident[:]), reads=[re_, R("ident")], writes=[R("ps", TB_)])
                k0 = kbs[0]
                P.op("act", lambda e: e.copy(
                    out=pT[:, par, k0:2, :], in_=PS[TB_][:, to + k0 * 128:to + 256].rearrange("p (a b) -> p a b", b=128)),
                    reads=[R("ps", TB_)], writes=[rp])

            def emit_PV(i):
                qb, h = steps[i]
                c0, kcols, kres, kbs = geom(qb)
                par, kv = h % 2, h // 4
                rp = R("pT", par)
                for kb in kbs:
                    vt = qb - 1 + kb
                    P.op("pe", lambda e, kb=kb, vt=vt: e.matmul(
                        PS[OB_][:, h * 64:(h + 1) * 64], pT[:, par, kb, :], vS[:, vt, kv * 64:(kv + 1) * 64],
                        start=(kb == kbs[0]), stop=(kb == 1)),
                        reads=[rp, R("vS", vt)], writes=[R("ps", OB_)])

            def emit_epi(qb):
                qpar = qb % 2
                ro = R("osb", qpar)
                P.op("dve", lambda e: e.tensor_tensor(
                    out=osb[:, qpar, :].rearrange("p (a b) -> p a b", b=64),
                    in0=PS[OB_][:, :].rearrange("p (a b) -> p a b", b=64),
                    in1=rden[:, qpar, :].unsqueeze(2).broadcast_to([128, NH, 64]), op=ALU.mult),
                    reads=[R("ps", OB_), R("rden", qpar)], writes=[ro])
                TB_ = 5
                tres = [R("ps", 5)]
                for ft in range(4):
                    P.op("pe", lambda e, ft=ft: e.transpose(
                        out=PS[TB_][:, ft * 128:(ft + 1) * 128], in_=osb[:, qpar, ft * 128:(ft + 1) * 128],
                        identity=ident[:]), reads=[ro, R("ident")], writes=tres)
                P.op("act", lambda e: e.copy(
                    out=qT[:, 0:4, qb * 128:(qb + 1) * 128], in_=PS[TB_][:, :].rearrange("p (a b) -> p a b", b=128)),
                    reads=tres, writes=[R("qT", ft, qb) for ft in range(4)])

            n = len(steps)
            emit_S(0)
            for i in range(n):
                if i + 1 < n:
                    emit_S(i + 1)
                if i >= 1:
                    emit_PV(i - 1)
                    if steps[i - 1][1] == NH - 1:
                        emit_epi(steps[i - 1][0])
                emit_st2(i)
                yield
                emit_st3(i)
                yield
            emit_PV(n - 1)
            emit_epi(steps[n - 1][0])
            yield

        g_ssm, g_att = ssm_gen(), att_gen()
        live = {"s": True, "a": True}

        def adv(g, k_):
            if live[k_]:
                try:
                    next(g)
                except StopIteration:
                    live[k_] = False

        while live["s"] or live["a"]:
            adv(g_att, "a")
            adv(g_ssm, "s")
            adv(g_att, "a")

        gbank = [0, 1, 2, 3]
        for s_ in range(NS):
            cols = slice(s_ * Tc, (s_ + 1) * Tc)
            yres = [R("uT", k, s_) for k in range(4)]
            for ft in range(4):
                for kc in range(4):
                    P.op("pe", lambda e, ft=ft, kc=kc, cols=cols: e.matmul(
                        PS[gbank[ft]][:, 0:Tc], wglu_bf[:, kc, ft * 128:(ft + 1) * 128], uT[:, kc, cols],
                        start=(kc == 0), stop=(kc == 3)),
                        reads=yres + [R("wglu_bf", kc)], writes=[R("ps", gbank[ft])])
            for ft in range(4):
                gt = gl_t[:, ft % 2, :]
                rg = R("gl_t", ft % 2)
                act(gt, PS[gbank[ft]][:, 0:Tc], AF.Sigmoid, [R("ps", gbank[ft]), R("ssm_dg")], [rg],
                    bias=ssm_dg[:, 4 + ft:5 + ft])
                vtt("dve", uT[:, ft, cols], uT[:, ft, cols], gt, ALU.mult, [rg, R("uT", ft, s_)], [R("uT", ft, s_)])
        if dbg:
            dd = dout("dbg_yssmT", [512, T])
            for o in range(4):
                tmp = sb("dbgys%d" % o, [128, T])
                P.op("dve", lambda e, tmp=tmp, o=o: e.tensor_copy(out=tmp[:], in_=uT[:, o, :]),
                     reads=[R("uT", o, s) for s in range(NS)], writes=[R("dbgys", o)])
                P.dma(lambda e, dd=dd, tmp=tmp, o=o: e.dma_start(out=dd[o * 128:(o + 1) * 128, :], in_=tmp[:]),
                      reads=[R("dbgys", o)])
        if dbg:
            dd = dout("dbg_yattT", [512, T])
            for o in range(4):
                tmp = sb("dbgya%d" % o, [128, T])
                P.op("dve", lambda e, tmp=tmp, o=o: e.tensor_copy(out=tmp[:], in_=qT[:, o, :]),
                     reads=[R("qT", o, b) for b in range(NT)], writes=[R("dbgya", o)])
                P.dma(lambda e, dd=dd, tmp=tmp, o=o: e.dma_start(out=dd[o * 128:(o + 1) * 128, :], in_=tmp[:]),
                      reads=[R("dbgya", o)])
        P.barrier()
        st_t.close()
        cur.pop()
        st_s.close()
        cur.pop()
        st_sw.close()
        cur.pop()
        st_kv.close()
        cur.pop()

        if stop == 4:
            return early_exit()
        st_5 = ExitStack()
        cur.append(st_5)
        xt = sb("xt5", [128, TPS, D], F32)
        hT = sb("hT5", [128, DC, TS], BF16)
        wgate_bf = sb("wgate_bf", [128, DC, 2 * D], BF16)
        wA_bf = sb("wA_bf", [128, 4, D], BF16)
        wB_bf = sb("wB_bf", [128, 4, D], BF16)
        wout_bf = sb("wout_bf", [128, DC, D], BF16)
        wst5 = sb("wst5", [128, 2, D], F32)
        bgate = sb("bgate", [128, 16])
        P.dma(lambda e: e.dma_start(out=bgate[:], in_=bgate_d[:, :]), writes=[R("bgate")])
        for hh in range(2):
            load_w_bf16(wgate_bf[:, :, hh * D:(hh + 1) * D], w_gate_d[:, hh * D:(hh + 1) * D], D, D, 0,
                        [(0, D, 0)], wst5, "wst5", "wgate_bf%d" % hh)
        load_w_bf16(wA_bf, w_bra_d, 512, D, None, [(0, D, 0)], wst5, "wst5", "wA_bf")
        load_w_bf16(wB_bf, w_brb_d, 512, D, None, [(0, D, 0)], wst5, "wst5", "wB_bf")
        load_w_bf16(wout_bf, w_out_d, D, D, None, [(0, D, 0)], wst5, "wst5", "wout_bf")
        wg_res = [R("wgate_bf0", c) for c in range(DC)] + [R("wgate_bf1", c) for c in range(DC)]
        gts = sb("gts", [128, 2, 2, TS])
        mT = sb("mT", [128, DC, TS], BF16)
        x1t = sb("x1t", [128, 2, D])
        for s in range(NS):
            emit_hT(s)
            hres = [R("hT", j) for j in range(TPS)]
            cols = slice(s * TS, (s + 1) * TS)
            for f in range(DC):
                fp = f % 2
                for br in range(2):
                    bank = 4 * fp + br
                    for c in range(DC):
                        P.op("pe", lambda e, bank=bank, c=c, f=f, br=br: e.matmul(
                            PS[bank][:, 0:TS], wgate_bf[:, c, br * D + f * 128:br * D + (f + 1) * 128], hT[:, c, :],
                            start=(c == 0), stop=(c == DC - 1)),
                            reads=hres + wg_res, writes=[R("ps", bank)])
                for br, (wbr, src, tag) in enumerate(((wA_bf, uT, "wA_bf"), (wB_bf, qT, "wB_bf"))):
                    bank = 4 * fp + 2 + br
                    if br == 0:
                        srcres = [R("uT", k, s) for k in range(4)]
                    else:
                        srcres = [R("qT", k, s * TPS + jj) for k in range(4) for jj in range(TPS)]
                    for k in range(4):
                        P.op("pe", lambda e, bank=bank, k=k, f=f, wbr=wbr, src=src, cols=cols: e.matmul(
                            PS[bank][:, 0:TS], wbr[:, k, f * 128:(f + 1) * 128], src[:, k, cols],
                            start=(k == 0), stop=(k == 3)),
                            reads=srcres + [R(tag, k) for k in range(4)], writes=[R("ps", bank)])
                for br in range(2):
                    bank = 4 * fp + br
                    rg = R("gts", fp, br)
                    act(gts[:, fp, br, :], PS[bank][:, 0:TS], AF.Sigmoid, [R("ps", bank), R("bgate")], [rg],
                        bias=bgate[:, br * 8 + f:br * 8 + f + 1])
                    vtt("dve", gts[:, fp, br, :], gts[:, fp, br, :], PS[4 * fp + 2 + br][:, 0:TS], ALU.mult,
                        [rg, R("ps", 4 * fp + 2 + br)], [rg])
                vtt("dve", mT[:, f, :], gts[:, fp, 0, :], gts[:, fp, 1, :], ALU.add,
                    [R("gts", fp, 0), R("gts", fp, 1)], [R("mT", f)])
            mres = [R("mT", f) for f in range(DC)]
            wo_res = [R("wout_bf", c) for c in range(DC)]
            for j in range(TPS):
                tix = s * TPS + j
                xb = tix % 2
                for hf in range(2):
                    bank = (2 * j + hf) % 8
                    for f in range(DC):
                        P.op("pe", lambda e, bank=bank, f=f, j=j, hf=hf: e.matmul(
                            PS[bank][:, :], mT[:, f, j * 128:(j + 1) * 128], wout_bf[:, f, hf * 512:(hf + 1) * 512],
                            start=(f == 0), stop=(f == DC - 1)),
                            reads=mres + wo_res, writes=[R("ps", bank)])
                    vtt("dve", x1t[:, xb, hf * 512:(hf + 1) * 512], PS[bank][:, :], xt[:, j, hf * 512:(hf + 1) * 512],
                        ALU.add, [R("ps", bank), R("xt", j)], [R("x1t", xb)])
                P.dma(lambda e, tix=tix, xb=xb: e.dma_start(out=x1_d[tix * 128:(tix + 1) * 128, :], in_=x1t[:, xb, :]),
                      reads=[R("x1t", xb)], writes=[R("x1d", tix)])
                if dbg:
                    if tix == 0:
                        dbg_x1 = dout("dbg_x1", [T, D])
                    P.dma(lambda e, tix=tix, xb=xb: e.dma_start(out=dbg_x1[tix * 128:(tix + 1) * 128, :], in_=x1t[:, xb, :]),
                          reads=[R("x1t", xb)])
        P.barrier()
        st_5.close()
        cur.pop()
        st_mix.close()
        cur.pop()

        if stop == 5:
            return early_exit()
        NHALF = 2 if T >= 1024 else 1
        TH = T // NHALF
        NTH = TH // 128
        TCH = min(512, TH)
        NCH = TH // TCH
        TPC = TCH // 128
        BIG = 1.0e4
        st_b = ExitStack()
        cur.append(st_b)
        acc = sb("acc", [128, NTH, D])
        h2T = sb("h2T", [128, DC, TH], BF16)
        wden = sb("wden", [128, NTH, NE])
        for half in range(NHALF):
            st_b0 = ExitStack()
            cur.append(st_b0)
            wr32 = sb("wr32_%d" % half, [128, DC, 36])
            brt = sb("brt_%d" % half, [128, 36])
            wrs = sb("wrs_%d" % half, [128, DC, 36])
            wr_hi = sb("wr_hi_%d" % half, [128, DC, 36], BF16)
            wr_lo = sb("wr_lo_%d" % half, [128, DC, 36], BF16)
            P.dma(lambda e: e.dma_start(out=brt[:], in_=b_rt_d[0:1, :].partition_broadcast(128)), writes=[R("brt")])
            P.dma(lambda e: e.dma_start(out=wr32[:], in_=w_rt_d.rearrange("(c p) n -> p c n", p=128)), writes=[R("wr32")])
            P.op("pool", lambda e: e.tensor_copy(out=wr_hi[:], in_=wr32[:]), reads=[R("wr32")], writes=[R("wr_hi")])
            P.op("pool", lambda e: e.tensor_copy(out=wrs[:], in_=wr_hi[:]), reads=[R("wr_hi")], writes=[R("wrs")])
            vtt("pool", wrs[:], wr32[:], wrs[:], ALU.subtract, [R("wr32"), R("wrs")], [R("wrs")])
            P.op("pool", lambda e: e.tensor_copy(out=wr_lo[:], in_=wrs[:]), reads=[R("wrs")], writes=[R("wr_lo")])
            load_gb(1)
            lgall = sb("lgall_%d" % half, [128, NTH, 36])
            hlo2 = sb("hlo2_%d" % half, [128, 2, DC, 128], BF16)
            pend = []

            def emit_lg(j, rbank):
                vtt("dve", lgall[:, j, :], PS[rbank][:, 0:36], brt[:], ALU.add, [R("ps", rbank), R("brt")], [R("lgall")])

            for j in range(NTH):
                tix = half * NTH + j
                P.dma(lambda e, j=j, tix=tix: e.dma_start(out=acc[:, j, :], in_=x1_d[tix * 128:(tix + 1) * 128, :]),
                      reads=[R("x1d", tix)], writes=[R("acc", j)])
                slot = j % 4
                rms_rstd(acc[:, j, :], R("acc", j), slot)
                b = j % 2
                stt("dve", xn[:, b, :], acc[:, j, :], rstd[:, slot:slot + 1], gb[:], ALU.mult, ALU.mult,
                    [R("acc", j), R("rstd", slot), R("gb")], [R("xn", b)])
                jc = slice(j * 128, (j + 1) * 128)
                for hf in range(2):
                    bank = hf
                    for cc in range(4):
                        c = hf * 4 + cc
                        P.op("pe", lambda e, b=b, c=c, cc=cc, bank=bank: e.transpose(
                            out=PS[bank][:, cc * 128:(cc + 1) * 128], in_=xn[:, b, c * 128:(c + 1) * 128],
                            identity=ident[:]), reads=[R("xn", b), R("ident")], writes=[R("ps", bank)])
                    inap = PS[bank][:, :].rearrange("p (a b) -> p a b", a=4)
                    hsl = slice(hf * 4, hf * 4 + 4)
                    P.op("dve", lambda e, hsl=hsl, jc=jc, inap=inap: e.tensor_copy(out=h2T[:, hsl, jc], in_=inap),
                         reads=[R("ps", bank)], writes=[R("h2T", j)])
                    P.op("dve", lambda e, hsl=hsl, jc=jc, inap=inap, b=b: e.tensor_tensor(
                        out=hlo2[:, b, hsl, :], in0=inap, in1=h2T[:, hsl, jc], op=ALU.subtract),
                        reads=[R("ps", bank), R("h2T", j)], writes=[R("hlo2", b)])
                rbank = 2 + (j % 2)
                k_ = 0
                for (ha, hres_, wa, wres_) in ((h2T[:, :, jc], R("h2T", j), wr_hi, R("wr_hi")),
                                               (h2T[:, :, jc], R("h2T", j), wr_lo, R("wr_lo")),
                                               (hlo2[:, b, :, :], R("hlo2", b), wr_hi, R("wr_hi"))):
                    for c in range(DC):
                        P.op("pe", lambda e, c=c, rbank=rbank, ha=ha, wa=wa, k_=k_: e.matmul(
                            PS[rbank][:, 0:36], ha[:, c, :], wa[:, c, :], start=(k_ == 0), stop=(k_ == 3 * DC - 1)),
                            reads=[hres_, wres_], writes=[R("ps", rbank)])
                        k_ += 1
                if pend:
                    emit_lg(*pend.pop())
                pend.append((j, rbank))
            emit_lg(*pend.pop())
            N_ = NTH
            r1 = sb("r1_%d" % half, [128, 8, N_])
            g4 = sb("g4_%d" % half, [128, 3, N_, 4])
            e32 = sb("e32_%d" % half, [128, 3, N_, NE])
            rL, r1r, rg4, re32 = R("lgall"), R("r1"), R("g4"), R("e32")
            G_ = lgall[:, :, 0:4]
            gm, gsum, gpr, m1, m2, ex_, w1, w2 = (r1[:, i, :] for i in range(8))

            def bc(v, n):
                return v.unsqueeze(2).broadcast_to([128, N_, n])

            P.op("dve", lambda e: e.reduce_max(out=gm, in_=G_, axis=AX.X), reads=[rL], writes=[r1r])
            vtt("dve", g4[:, 0, :, :], G_, bc(gm, 4), ALU.subtract, [rL, r1r], [rg4])
            ts("dve", g4[:, 1, :, :], g4[:, 0, :, :], 0.0, ALU.is_ge, [rg4], [rg4])
            act(g4[:, 0, :, :], g4[:, 0, :, :], AF.Exp, [rg4], [rg4])
            P.op("dve", lambda e: e.reduce_sum(out=gsum, in_=g4[:, 0, :, :], axis=AX.X), reads=[rg4], writes=[r1r])
            P.op("dve", lambda e: e.reciprocal(out=gpr, in_=gsum), reads=[r1r], writes=[r1r])
            ts("dve", g4[:, 2, :, :], g4[:, 1, :, :], BIG, ALU.mult, [rg4], [rg4], s2=-BIG, op1=ALU.add)
            em_, oh1, oh2 = e32[:, 0, :, :], e32[:, 1, :, :], e32[:, 2, :, :]
            P.op("dve", lambda e: e.tensor_copy(out=em_, in_=lgall[:, :, 4:36]), reads=[rL], writes=[re32])
            P.op("dve", lambda e: e.tensor_tensor(
                out=em_.rearrange("p n (a b) -> p (n a) b", b=8), in0=em_.rearrange("p n (a b) -> p (n a) b", b=8),
                in1=g4[:, 2, :, :].rearrange("p n a -> p (n a)").unsqueeze(2).broadcast_to([128, N_ * 4, 8]),
                op=ALU.add), reads=[rg4, re32], writes=[re32])
            P.op("dve", lambda e: e.reduce_max(out=m1, in_=em_, axis=AX.X), reads=[re32], writes=[r1r])
            vtt("dve", oh1, em_, bc(m1, NE), ALU.subtract, [re32, r1r], [re32])
            ts("dve", oh1, oh1, 0.0, ALU.is_ge, [re32], [re32])
            stt("dve", em_, oh1, -BIG, em_, ALU.mult, ALU.add, [re32], [re32])
            P.op("dve", lambda e: e.reduce_max(out=m2, in_=em_, axis=AX.X), reads=[re32], writes=[r1r])
            vtt("dve", oh2, em_, bc(m2, NE), ALU.subtract, [re32, r1r], [re32])
            ts("dve", oh2, oh2, 0.0, ALU.is_ge, [re32], [re32])
            vtt("dve", ex_, m2, m1, ALU.subtract, [r1r], [r1r])
            act(ex_, ex_, AF.Exp, [r1r], [r1r])
            ts("dve", w1, ex_, 1.0, ALU.add, [r1r], [r1r])
            P.op("dve", lambda e: e.reciprocal(out=w1, in_=w1), reads=[r1r], writes=[r1r])
            vtt("dve", w2, ex_, w1, ALU.mult, [r1r], [r1r])
            vtt("dve", w1, w1, gpr, ALU.mult, [r1r], [r1r])
            vtt("dve", w2, w2, gpr, ALU.mult, [r1r], [r1r])
            wres_all = [R("wden", j) for j in range(NTH)]
            vtt("dve", oh1, oh1, bc(w1, NE), ALU.mult, [re32, r1r], [re32])
            vtt("dve", oh2, oh2, bc(w2, NE), ALU.mult, [re32, r1r], [re32])
            vtt("dve", wden[:, :, :], oh1, oh2, ALU.add, [re32], wres_all)
            P.barrier()
            st_b0.close()
            cur.pop()
            if stop == 6:
                return early_exit()
            st_e = ExitStack()
            cur.append(st_e)
            wgu_bf = sb("wgu_bf%d" % half, [128, 2, 2, DC, DFF], BF16)
            wd_bf = sb("wd_bf%d" % half, [128, 2, 4, D], BF16)
            wste = sb("wste%d" % half, [128, 2, 4096])
            aT = sb("aT%d" % half, [128, 2, 4, TCH], BF16)
            sgt = sb("sgt%d" % half, [128, 2, TCH])
            h2res = [R("h2T", j) for j in range(NTH)]
            stg = [0]

            def load_expert(e_):
                bsel = e_ % 2
                for mi, wd_ in enumerate((w_eg_d, w_eu_d, w_ed_d)):
                    si = stg[0] % 2
                    stg[0] += 1
                    rs_ = R("wste", si)
                    if mi < 2:
                        P.dma(lambda e, wd_=wd_, si=si, e_=e_: e.dma_start(
                            out=wste[:, si, :].rearrange("p (c n) -> p c n", n=DFF),
                            in_=wd_[e_].rearrange("(c p) n -> p c n", p=128)), writes=[rs_])
                        P.op("act", lambda e, bsel=bsel, mi=mi, si=si: e.copy(
                            out=wgu_bf[:, bsel, mi, :, :], in_=wste[:, si, :].rearrange("p (c n) -> p c n", n=DFF)),
                            reads=[rs_], writes=[R("wgu", bsel, mi)])
                    else:
                        P.dma(lambda e, wd_=wd_, si=si, e_=e_: e.dma_start(
                            out=wste[:, si, :].rearrange("p (c n) -> p c n", n=D),
                            in_=wd_[e_].rearrange("(c p) n -> p c n", p=128)), writes=[rs_])
                        P.op("dve", lambda e, bsel=bsel, si=si: e.tensor_copy(
                            out=wd_bf[:, bsel, :, :], in_=wste[:, si, :].rearrange("p (c n) -> p c n", n=D)),
                            reads=[rs_], writes=[R("wd", bsel)])

            load_expert(0)
            for e_ in range(n_exp):
                if e_ + 1 < n_exp:
                    load_expert(e_ + 1)
                bsel = e_ % 2
                for ch in range(NCH):
                    ap_ = ch % 2
                    ccols = slice(ch * TCH, (ch + 1) * TCH)
                    hres_c = h2res[ch * TPC:(ch + 1) * TPC]
                    for f in range(4):
                        fp = f % 2
                        for mi in range(2):
                            bank = 2 * fp + mi
                            for c in range(DC):
                                P.op("pe", lambda e, bank=bank, bsel=bsel, mi=mi, c=c, f=f, ccols=ccols: e.matmul(
                                    PS[bank][:, 0:TCH], wgu_bf[:, bsel, mi, c, f * 128:(f + 1) * 128], h2T[:, c, ccols],
                                    start=(c == 0), stop=(c == DC - 1)),
                                    reads=hres_c + [R("wgu", bsel, mi)], writes=[R("ps", bank)])
                        act(sgt[:, fp, :], PS[2 * fp][:, 0:TCH], AF.Silu, [R("ps", 2 * fp)], [R("sgt", fp)])
                        vtt("dve", aT[:, ap_, f, :], sgt[:, fp, :], PS[2 * fp + 1][:, 0:TCH], ALU.mult,
                            [R("sgt", fp), R("ps", 2 * fp + 1)], [R("aT", ap_, f)])
                    ares = [R("aT", ap_, f) for f in range(4)]
                    for jj in range(TPC):
                        j = ch * TPC + jj
                        for hf in range(2):
                            bank = 4 + (2 * jj + hf) % 4
                            for f in range(4):
                                P.op("pe", lambda e, bank=bank, ap_=ap_, f=f, jj=jj, bsel=bsel, hf=hf: e.matmul(
                                    PS[bank][:, :], aT[:, ap_, f, jj * 128:(jj + 1) * 128],
                                    wd_bf[:, bsel, f, hf * 512:(hf + 1) * 512], start=(f == 0), stop=(f == 3)),
                                    reads=ares + [R("wd", bsel)], writes=[R("ps", bank)])
                            stt("dve", acc[:, j, hf * 512:(hf + 1) * 512], PS[bank][:, :], wden[:, j, e_:e_ + 1],
                                acc[:, j, hf * 512:(hf + 1) * 512], ALU.mult, ALU.add,
                                [R("ps", bank), R("wden", j), R("acc", j)], [R("acc", j)])
            P.barrier()
            st_e.close()
            cur.pop()
            if stop == 7:
                return early_exit()
            st_c = ExitStack()
            cur.append(st_c)
            wpg_bf = sb("wpg_bf%d" % half, [128, DC, D], BF16)
            wpp_bf = sb("wpp_bf%d" % half, [128, 2, D], BF16)
            wstc = sb("wstc%d" % half, [128, 2, D])
            load_gb(2)
            bpg_b = sb("bpg_b%d" % half, [128, D])
            gfin_b = sb("gfin_b%d" % half, [128, D])
            P.dma(lambda e: e.dma_start(out=bpg_b[:], in_=b_pg_d[0:1, :].partition_broadcast(128)), writes=[R("bpg_b")])
            P.dma(lambda e: e.dma_start(out=gfin_b[:], in_=g_fin_d[0:1, :].partition_broadcast(128)), writes=[R("gfin_b")])
            load_w_bf16(wpg_bf, w_pg_d, D, D, 2, [(0, D, 0)], wstc, "wstc", "wpg_bf")
            load_w_bf16(wpp_bf, w_pp_d, PLE, D, None, [(0, D, 0)], wstc, "wstc", "wpp_bf")
            wpg_res = [R("wpg_bf", c) for c in range(DC)]
            wpp_res = [R("wpp_bf", c) for c in range(2)]
            h3T = sb("h3T%d" % half, [128, 2, DC, 128], BF16)
            ptl = sb("ptl%d" % half, [128, 2, PLE])
            pT_ = sb("pTt%d" % half, [128, 2, 2, 128], BF16)
            gtm = sb("gtm%d" % half, [128, 2, D])
            x3t = sb("x3t%d" % half, [128, 2, D])
            def c_tile(j):
                tix = half * NTH + j
                b = j % 2
                slot = j % 4
                slot2 = (j + 2) % 4
                base = 4 * b
                rms_rstd(acc[:, j, :], R("acc", j), slot)
                P.dma(lambda e: e.dma_start(out=ptl[:, b, :], in_=p_d[tix * 128:(tix + 1) * 128, :]),
                      writes=[R("ptl", b)])
                yield
                stt("dve", xn[:, b, :], acc[:, j, :], rstd[:, slot:slot + 1], gb[:], ALU.mult, ALU.mult,
                    [R("acc", j), R("rstd", slot), R("gb")], [R("xn", b)])
                for hf in range(2):
                    bank = base + hf
                    for cc in range(4):
                        c = hf * 4 + cc
                        P.op("pe", lambda e, c=c, cc=cc, bank=bank: e.transpose(
                            out=PS[bank][:, cc * 128:(cc + 1) * 128], in_=xn[:, b, c * 128:(c + 1) * 128],
                            identity=ident[:]), reads=[R("xn", b), R("ident")], writes=[R("ps", bank)])
                for c2 in range(2):
                    P.op("pe", lambda e, c2=c2: e.transpose(
                        out=PS[base + 2][:, c2 * 128:(c2 + 1) * 128], in_=ptl[:, b, c2 * 128:(c2 + 1) * 128],
                        identity=ident[:]), reads=[R("ptl", b), R("ident")], writes=[R("ps", base + 2)])
                yield
                for hf in range(2):
                    bank = base + hf
                    inap = PS[bank][:, :].rearrange("p (a b) -> p a b", a=4)
                    if hf == 0:
                        P.op("dve", lambda e, hf=hf, inap=inap: e.tensor_copy(out=h3T[:, b, hf * 4:hf * 4 + 4, :], in_=inap),
                             reads=[R("ps", bank)], writes=[R("h3T", b)])
                    else:
                        P.op("act", lambda e, hf=hf, inap=inap: e.copy(out=h3T[:, b, hf * 4:hf * 4 + 4, :], in_=inap),
                             reads=[R("ps", bank)], writes=[R("h3T", b)])
                P.op("act", lambda e: e.copy(out=pT_[:, b, :, :],
                                             in_=PS[base + 2][:, 0:256].rearrange("p (a b) -> p a b", b=128)),
                     reads=[R("ps", base + 2)], writes=[R("pT_", b)])
                yield
                for hf in range(2):
                    gbk, pb_ = base + 2 + hf, base + hf
                    hs = slice(hf * 512, (hf + 1) * 512)
                    for c in range(DC):
                        P.op("pe", lambda e, gbk=gbk, c=c, hs=hs: e.matmul(
                            PS[gbk][:, :], h3T[:, b, c, :], wpg_bf[:, c, hs], start=(c == 0), stop=(c == DC - 1)),
                            reads=[R("h3T", b)] + wpg_res, writes=[R("ps", gbk)])
                    for c2 in range(2):
                        P.op("pe", lambda e, pb_=pb_, c2=c2, hs=hs: e.matmul(
                            PS[pb_][:, :], pT_[:, b, c2, :], wpp_bf[:, c2, hs], start=(c2 == 0), stop=(c2 == 1)),
                            reads=[R("pT_", b)] + wpp_res, writes=[R("ps", pb_)])
                yield
                for hf in range(2):
                    gbk = base + 2 + hf
                    hs = slice(hf * 512, (hf + 1) * 512)
                    rg = R("gtm", b, hf)
                    vtt("dve", gtm[:, b, hs], PS[gbk][:, :], bpg_b[:, hs], ALU.add, [R("ps", gbk), R("bpg_b")], [rg])
                    act(gtm[:, b, hs], gtm[:, b, hs], AF.Sigmoid, [rg], [rg])
                yield
                for hf in range(2):
                    pb_ = base + hf
                    hs = slice(hf * 512, (hf + 1) * 512)
                    rg = R("gtm", b, hf)
                    vtt("dve", gtm[:, b, hs], gtm[:, b, hs], PS[pb_][:, :], ALU.mult, [rg, R("ps", pb_)], [rg])
                    vtt("dve", x3t[:, b, hs], gtm[:, b, hs], acc[:, j, hs], ALU.add, [rg, R("acc", j)], [R("x3t", b, hf)])
                x3res = [R("x3t", b, 0), R("x3t", b, 1)]
                if dbg:
                    P.dma(lambda e: e.dma_start(out=dbg_c[0][tix * 128:(tix + 1) * 128, :], in_=acc[:, j, :]),
                          reads=[R("acc", j)])
                    P.dma(lambda e: e.dma_start(out=dbg_c[1][tix * 128:(tix + 1) * 128, :], in_=x3t[:, b, :]),
                          reads=x3res)
                P.op("act", lambda e: e.activation(out=junk[:], in_=x3t[:, b, :], func=AF.Square,
                                                   accum_out=ssq[:, slot2:slot2 + 1]),
                     reads=x3res, writes=[R("junk"), R("ssq", slot2)])
                P.op("act", lambda e: e.activation(out=ssq[:, slot2:slot2 + 1], in_=ssq[:, slot2:slot2 + 1],
                                                   func=AF.Sqrt, bias=epsc[:, 0:1], scale=1.0 / D),
                     reads=[R("ssq", slot2), R("epsc")], writes=[R("ssq", slot2)])
                yield
                P.op("dve", lambda e: e.reciprocal(out=rstd[:, slot2:slot2 + 1], in_=ssq[:, slot2:slot2 + 1]),
                     reads=[R("ssq", slot2)], writes=[R("rstd", slot2)])
                stt("dve", x3t[:, b, :], x3t[:, b, :], rstd[:, slot2:slot2 + 1], gfin_b[:], ALU.mult, ALU.mult,
                    x3res + [R("rstd", slot2), R("gfin_b")], x3res)
                P.dma(lambda e: e.dma_start(out=out_d[tix * 128:(tix + 1) * 128, :], in_=x3t[:, b, :]),
                      reads=x3res, writes=[R("x1d", tix)])
                yield

            if dbg and half == 0:
                dbg_c = [dout("dbg_x2", [T, D]), dout("dbg_x3", [T, D])]
            NSTEP, STAG = 7, 4
            gens = [c_tile(j) for j in range(NTH)]
            for t_ in range(STAG * (NTH - 1) + NSTEP):
                for j in range(NTH):
                    if 0 <= t_ - STAG * j < NSTEP:
                        next(gens[j])
            P.barrier()
            st_c.close()
            cur.pop()
        st_b.close()
        cur.pop()
        P.finish()
    return nc


def host_prep(inp, core):
    g = np.zeros((128, 4, DC), np.float32)
    g[:, 0, :] = inp["g_mix"][0].reshape(DC, 128).T
    g[:, 1, :] = inp["g_ffn"][0].reshape(DC, 128).T
    g[:, 2, :] = inp["g_ple"][0].reshape(DC, 128).T
    bt = np.zeros((128, 2, 16, 128), np.float32)
    ct_ = np.zeros((128, 2, 16, 128), np.float32)
    af = np.zeros((3, 16, 128), np.float32)
    ap = np.zeros((128, 3, 16), np.float32)
    are, aim, ldt = inp["ssm_a_re"][0], inp["ssm_a_im"][0], inp["ssm_log_dt"][0]
    for ri, (B_, C_) in enumerate(((inp["ssm_b_re"][0], inp["ssm_c_re"][0]), (inp["ssm_b_im"][0], inp["ssm_c_im"][0]))):
        for g_ in range(32):
            gp_, g2 = g_ // 2, g_ % 2
            g8 = g_ % 8
            bt[g8 * 16:(g8 + 1) * 16, ri, gp_, g2 * 64:(g2 + 1) * 64] = B_[g_].T
            ct_[g2 * 64:(g2 + 1) * 64, ri, gp_, g8 * 16:(g8 + 1) * 16] = C_[g_].T
    for g_ in range(32):
        gp_, g2 = g_ // 2, g_ % 2
        af[0, gp_, g2 * 64:(g2 + 1) * 64] = are[g_]
        af[1, gp_, g2 * 64:(g2 + 1) * 64] = aim[g_]
        af[2, gp_, g2 * 64:(g2 + 1) * 64] = ldt[g_]
        ap[g2 * 64:(g2 + 1) * 64, 0, gp_] = are[g_]
        ap[g2 * 64:(g2 + 1) * 64, 1, gp_] = aim[g_]
        ap[g2 * 64:(g2 + 1) * 64, 2, gp_] = ldt[g_]
    dg = np.zeros((128, 8), np.float32)
    dg[:, 0:4] = inp["ssm_d"][0].reshape(4, 128).T
    dg[:, 4:8] = inp["b_glu"][0].reshape(4, 128).T
    q_loc = np.arange(128)[:, None]
    c_loc = np.arange(256)[None, :]
    rel = q_loc + 128 - c_loc
    valid = (rel >= 0) & (rel < 128)
    bkt = t5_bucket_np(np.maximum(rel, 0))
    rb = inp["rel_bias"]
    ab = np.full((128, NH, 256), NEG, np.float32)
    for h in range(NH):
        ab[:, h, :] = np.where(valid, rb[bkt, h], np.float32(NEG))
    m = {
        "w_router": np.ascontiguousarray(np.concatenate([inp["w_router_group"][0], inp["w_router_expert"][0]], axis=1)),
        "b_router": np.ascontiguousarray(np.concatenate([inp["b_router_group"][0], inp["b_router_expert"][0]])[None, :]),
        "w_e_gate": np.ascontiguousarray(inp["w_e_gate"][0]),
        "w_e_up": np.ascontiguousarray(inp["w_e_up"][0]),
        "w_e_down": np.ascontiguousarray(inp["w_e_down"][0]),
        "w_ple_gate": np.ascontiguousarray(inp["w_ple_gate"][0]),
        "b_ple_gate": np.ascontiguousarray(inp["b_ple_gate"][0][None, :]),
        "w_ple_proj": np.ascontiguousarray(inp["w_ple_proj"][0]),
        "g_final": np.ascontiguousarray(inp["g_final"][None, :]),
        "attn_bias": ab,
        "sinks_b": np.ascontiguousarray(np.broadcast_to(inp["sinks"][0][None, :], (128, NH))).astype(np.float32),
        "w_gate": np.ascontiguousarray(inp["w_gate"][0]),
        "b_gate_l": np.ascontiguousarray(inp["b_gate"][0].reshape(16, 128).T),
        "w_br_ssm": np.ascontiguousarray(inp["w_br_ssm"][0]),
        "w_br_attn": np.ascontiguousarray(inp["w_br_attn"][0]),
        "w_out": np.ascontiguousarray(inp["w_out"][0]),
        "ssm_bt": bt.reshape(128, 2, 2048), "ssm_ct": ct_.reshape(128, 2, 2048),
        "ssm_af": af.reshape(1, 3 * 2048), "ssm_ap": ap, "ssm_dg": dg,
        "w_glu": np.ascontiguousarray(inp["w_glu"][0]),
        "x": np.ascontiguousarray(inp["x"][core]),
        "p": np.ascontiguousarray(inp["p"][0, core]),
        "w_in": np.ascontiguousarray(inp["w_in"][0]),
        "gvec": g,
        "grow": np.ascontiguousarray(np.stack([inp["g_mix"][0], inp["g_ffn"][0], inp["g_ple"][0]], axis=0)).astype(np.float32),
    }
    return m


def kernel(**inputs):
    inp = {k: np.asarray(v) for k, v in inputs.items()}
    T = inp["x"].shape[1]
    nc = build(T)
    in_maps = [host_prep(inp, c) for c in range(8)]
    res = run_bass_kernel_spmd(nc, in_maps, core_ids=list(range(8)))
    return np.stack([r["out"] for r in res.results], axis=0).astype(np.float32)
```
